# Optimizing a Trainium2 kernel written in Bass

```python
import jax
import jax.numpy as jnp
from jax import lax
import numpy as np

D_MODEL = 1024
BATCH = 4
SEQ = 8192
DEPTH = 2

GRID_W = 64
CTX_LEN = 256
HEAD_DIM = 64
NA_HEADS = 6
NA_WIN_R = 8
NA_WIN_C = 16
RET_HEADS = 6
RET_CHUNK = 128
POOL_WINDOWS = (2, 4, 8, 16)
POOL_GROUP = 64
NA_WIDTH = NA_HEADS * HEAD_DIM
RET_WIDTH = RET_HEADS * HEAD_DIM
POOL_WIDTH = POOL_GROUP * len(POOL_WINDOWS)
D_MIX = NA_WIDTH + RET_WIDTH + POOL_WIDTH
O_NA_Q = 0
O_NA_K = O_NA_Q + NA_WIDTH
O_NA_V = O_NA_K + NA_WIDTH
O_RET_Q = O_NA_V + NA_WIDTH
O_RET_K = O_RET_Q + RET_WIDTH
O_RET_V = O_RET_K + RET_WIDTH
O_RET_G = O_RET_V + RET_WIDTH
O_POOL = O_RET_G + RET_WIDTH
D_PROJ = O_POOL + POOL_WIDTH
ROPE_BASE = 10000.0
ROPE_PAIRS = HEAD_DIM // 4
PEER_HEADS = 8
PEER_NKEYS = 128
PEER_KEY_DIM = 128
PEER_TOPK = 16
PEER_BLOCK = 128
N_EXPERTS = PEER_NKEYS * PEER_NKEYS
NORM_EPS = 1e-6
NEG_INF = -1e30

kernel_name = 'hybrid_na_retention_pool_peer_dit'


def rms_norm(x, w):
    xf = x.astype(jnp.float32)
    y = xf * lax.rsqrt(jnp.mean(xf * xf, axis=-1, keepdims=True) + NORM_EPS)
    return (y * w.astype(jnp.float32)).astype(x.dtype)


def modulate(h, shift, scale):
    return h * (1 + scale) + shift


def split_heads(t):
    B, T, _ = t.shape
    return t.reshape(B, T, -1, HEAD_DIM).transpose(0, 2, 1, 3)


def merge_heads(t):
    B, H, T, d = t.shape
    return t.transpose(0, 2, 1, 3).reshape(B, T, H * d)


def rope_2d_tables(S):
    t = jnp.arange(S)
    row = (t // GRID_W).astype(jnp.float32)
    col = (t % GRID_W).astype(jnp.float32)
    inv = ROPE_BASE ** (-jnp.arange(ROPE_PAIRS, dtype=jnp.float32) / ROPE_PAIRS)
    ang = jnp.concatenate([row[:, None] * inv, col[:, None] * inv], axis=-1)
    return jnp.cos(ang), jnp.sin(ang)


def _rotate(xh, cos, sin):
    x1, x2 = xh[..., :ROPE_PAIRS], xh[..., ROPE_PAIRS:]
    return jnp.concatenate([x1 * cos - x2 * sin, x1 * sin + x2 * cos], axis=-1)


def apply_rope_2d(x, cos, sin):
    cos = cos.astype(x.dtype)
    sin = sin.astype(x.dtype)
    half = 2 * ROPE_PAIRS
    return jnp.concatenate([
        _rotate(x[..., :half], cos[:, :ROPE_PAIRS], sin[:, :ROPE_PAIRS]),
        _rotate(x[..., half:], cos[:, ROPE_PAIRS:], sin[:, ROPE_PAIRS:])], axis=-1)


def neighbourhood_attention(q, k, v, k_ctx, v_ctx, rpb):
    B, H, S, d = q.shape
    rows = S // GRID_W
    wr = min(NA_WIN_R, rows)
    qg = q.reshape(B, H, rows, GRID_W, d)
    kg = k.reshape(B, H, rows, GRID_W, d)
    vg = v.reshape(B, H, rows, GRID_W, d)
    r = jnp.arange(rows)
    r_start = jnp.clip(r - wr // 2, 0, rows - wr)
    ridx = r_start[:, None] + jnp.arange(wr)[None, :]
    k_blk = kg[:, :, ridx]
    v_blk = vg[:, :, ridx]
    cidx = jnp.arange(GRID_W)
    c_start = jnp.clip(cidx - NA_WIN_C // 2, 0, GRID_W - NA_WIN_C)
    col_ok = (cidx[None, :] >= c_start[:, None]) & (cidx[None, :] < c_start[:, None] + NA_WIN_C)
    roff = ridx - r[:, None] + (NA_WIN_R - 1)
    coff = jnp.clip(cidx[None, :] - cidx[:, None] + (NA_WIN_C - 1), 0, 2 * NA_WIN_C - 2)
    bias = rpb[:, roff[:, None, :, None], coff[None, :, None, :]].astype(jnp.float32)
    scale = HEAD_DIM ** -0.5
    s_win = jnp.einsum('bhrqd,bhrjkd->bhrqjk', qg, k_blk).astype(jnp.float32) * scale + bias
    s_win = jnp.where(col_ok[:, None, :], s_win, NEG_INF)
    s_ctx = jnp.einsum('bhrqd,bhcd->bhrqc', qg, k_ctx).astype(jnp.float32) * scale
    n_win = wr * GRID_W
    s = jnp.concatenate([s_win.reshape(B, H, rows, GRID_W, n_win), s_ctx], axis=-1)
    p = jax.nn.softmax(s, axis=-1).astype(v.dtype)
    p_win = p[..., :n_win].reshape(B, H, rows, GRID_W, wr, GRID_W)
    p_ctx = p[..., n_win:]
    out = (jnp.einsum('bhrqjk,bhrjkd->bhrqd', p_win, v_blk)
           + jnp.einsum('bhrqc,bhcd->bhrqd', p_ctx, v_ctx))
    return out.reshape(B, H, S, d)


def context_attention(q, k, v):
    s = jnp.einsum('bhqd,bhkd->bhqk', q, k).astype(jnp.float32) * HEAD_DIM ** -0.5
    p = jax.nn.softmax(s, axis=-1).astype(v.dtype)
    return jnp.einsum('bhqk,bhkd->bhqd', p, v)


def retention_scan(q, k, v, log_gamma, state0):
    B, H, T, d = q.shape
    nc = T // RET_CHUNK

    def chunks(t):
        return t.astype(jnp.float32).reshape(B, H, nc, RET_CHUNK, d).transpose(2, 0, 1, 3, 4)

    pos = jnp.arange(RET_CHUNK, dtype=jnp.float32)
    diff = pos[:, None] - pos[None, :]
    decay = jnp.where(diff[None] >= 0,
                      jnp.exp(jnp.maximum(diff, 0.0)[None] * log_gamma[:, None, None]), 0.0)
    xi = jnp.exp((pos + 1.0)[None, :] * log_gamma[:, None])[..., None]
    zeta = jnp.exp((RET_CHUNK - 1.0 - pos)[None, :] * log_gamma[:, None])[..., None]
    chunk_decay = jnp.exp(RET_CHUNK * log_gamma)[:, None, None]

    def step(R, qkv):
        qc, kc, vc = qkv
        inner = jnp.einsum('bhnm,bhmd->bhnd', jnp.einsum('bhnd,bhmd->bhnm', qc, kc) * decay, vc)
        cross = jnp.einsum('bhnd,bhde->bhne', qc * xi, R)
        R = chunk_decay * R + jnp.einsum('bhmd,bhme->bhde', kc * zeta, vc)
        return R, inner + cross

    _, y = lax.scan(step, state0, (chunks(q), chunks(k), chunks(v)))
    return y.transpose(1, 2, 0, 3, 4).reshape(B, H, T, d)


def retention_bidir(q, k, v, lg_fwd, lg_bwd, state_fwd, state_bwd):
    y_f = retention_scan(q, k, v, lg_fwd, state_fwd)
    y_b = retention_scan(jnp.flip(q, 2), jnp.flip(k, 2), jnp.flip(v, 2), lg_bwd, state_bwd)
    return y_f + jnp.flip(y_b, 2)


def context_states(k, v, lg_fwd, lg_bwd):
    C = k.shape[2]
    m = jnp.arange(C, dtype=jnp.float32)
    w_f = jnp.exp((C - 1.0 - m)[None, :] * lg_fwd[:, None])
    w_b = jnp.exp(m[None, :] * lg_bwd[:, None])
    kf = k.astype(jnp.float32)
    vf = v.astype(jnp.float32)
    R_f = jnp.einsum('bhmd,hm,bhme->bhde', kf, w_f, vf)
    R_b = jnp.einsum('bhmd,hm,bhme->bhde', kf, w_b, vf)
    return R_f, R_b


def retention_output(y, gate, gn_w):
    mu = jnp.mean(y, axis=-1, keepdims=True)
    var = jnp.mean(jnp.square(y - mu), axis=-1, keepdims=True)
    yn = merge_heads((y - mu) * lax.rsqrt(var + NORM_EPS)) * gn_w.astype(jnp.float32)
    return (yn * jax.nn.silu(gate.astype(jnp.float32))).astype(gate.dtype)


def multiscale_pool(p, w_pool, scale):
    B, T, _ = p.shape
    t = jnp.arange(T)
    pf = p.astype(jnp.float32)
    outs = []
    for g, w in enumerate(POOL_WINDOWS):
        xg = pf[..., g * POOL_GROUP:(g + 1) * POOL_GROUP]
        cs = jnp.concatenate([jnp.zeros((B, 1, POOL_GROUP), jnp.float32), jnp.cumsum(xg, axis=1)], axis=1)
        lo = jnp.clip(t - w // 2, 0, T)
        hi = jnp.clip(t + w // 2, 0, T)
        mean = (cs[:, hi] - cs[:, lo]) / (hi - lo).astype(jnp.float32)[:, None]
        outs.append((mean - xg).astype(p.dtype) @ w_pool[g])
    return jnp.concatenate(outs, axis=-1) * scale


def peer_ffn(h, wq, keys, u, v):
    B, T, D = h.shape
    xb = h.reshape(-1, PEER_BLOCK, D)

    def block(xt):
        q = (xt @ wq).reshape(PEER_BLOCK, PEER_HEADS, 2, PEER_KEY_DIM)
        s_a = jnp.einsum('thd,hkd->thk', q[:, :, 0], keys[0]).astype(jnp.float32)
        s_b = jnp.einsum('thd,hkd->thk', q[:, :, 1], keys[1]).astype(jnp.float32)
        va, ia = lax.top_k(s_a, PEER_TOPK)
        vb, ib = lax.top_k(s_b, PEER_TOPK)
        cand = (va[..., :, None] + vb[..., None, :]).reshape(PEER_BLOCK, PEER_HEADS, PEER_TOPK * PEER_TOPK)
        sc, ci = lax.top_k(cand, PEER_TOPK)
        idx = (jnp.take_along_axis(ia, ci // PEER_TOPK, axis=-1) * PEER_NKEYS
               + jnp.take_along_axis(ib, ci % PEER_TOPK, axis=-1))
        gate = jax.nn.softmax(sc, axis=-1).astype(xt.dtype)
        act = jax.nn.gelu(jnp.einsum('td,thkd->thk', xt, u[idx]), approximate=False)
        return jnp.einsum('thk,thkd->td', gate * act, v[idx])

    return lax.map(block, xb).reshape(B, T, D)


def setup_inputs(seed: int = 0) -> dict:
    key = jax.random.key(seed)
    ks = jax.random.split(key, 21)
    f32 = jnp.float32

    def nrm(k, shape, scale):
        return jax.random.normal(k, shape, f32) * scale

    base_logit = jnp.log(2.0 ** (5.0 + jnp.arange(RET_HEADS, dtype=f32)) - 1.0)
    return {
        'x': nrm(ks[0], (BATCH, SEQ, D_MODEL), 1.0),
        'c': nrm(ks[1], (BATCH, D_MODEL), 1.0),
        'ctx': nrm(ks[2], (BATCH, CTX_LEN, D_MODEL), 1.0),
        'c_ctx': nrm(ks[3], (D_MODEL,), 1.0),
        'norm1_w': 1.0 + nrm(ks[4], (DEPTH, D_MODEL), 0.02),
        'norm2_w': 1.0 + nrm(ks[5], (DEPTH, D_MODEL), 0.02),
        'w_ada': nrm(ks[6], (DEPTH, D_MODEL, 6 * D_MODEL), 0.5 * D_MODEL ** -0.5),
        'b_ada': nrm(ks[7], (DEPTH, 6 * D_MODEL), 0.01),
        'w_in': nrm(ks[8], (DEPTH, D_MODEL, D_PROJ), D_MODEL ** -0.5),
        'w_out': nrm(ks[9], (DEPTH, D_MIX, D_MODEL), D_MIX ** -0.5),
        'na_rpb': nrm(ks[10], (DEPTH, NA_HEADS, 2 * NA_WIN_R - 1, 2 * NA_WIN_C - 1), 0.02),
        'ret_decay_fwd': base_logit + nrm(ks[11], (DEPTH, RET_HEADS), 0.01),
        'ret_decay_bwd': base_logit + nrm(ks[12], (DEPTH, RET_HEADS), 0.01),
        'ret_gn_w': 1.0 + nrm(ks[13], (DEPTH, RET_WIDTH), 0.02),
        'pool_w': nrm(ks[14], (DEPTH, len(POOL_WINDOWS), POOL_GROUP, POOL_GROUP), POOL_GROUP ** -0.5),
        'pool_scale': 1.0 + nrm(ks[15], (DEPTH, POOL_WIDTH), 0.02),
        'peer_wq': nrm(ks[16], (DEPTH, D_MODEL, PEER_HEADS * 2 * PEER_KEY_DIM), D_MODEL ** -0.5),
        'peer_keys': nrm(ks[17], (DEPTH, 2, PEER_HEADS, PEER_NKEYS, PEER_KEY_DIM), PEER_KEY_DIM ** -0.5),
        'peer_u': nrm(ks[18], (DEPTH, N_EXPERTS, D_MODEL), D_MODEL ** -0.5),
        'peer_v': nrm(ks[19], (DEPTH, N_EXPERTS, D_MODEL), 0.5),
        'final_norm_w': 1.0 + nrm(ks[20], (D_MODEL,), 0.02),
    }


def reference(x, c, ctx, c_ctx, norm1_w, norm2_w, w_ada, b_ada, w_in, w_out, na_rpb,
              ret_decay_fwd, ret_decay_bwd, ret_gn_w, pool_w, pool_scale,
              peer_wq, peer_keys, peer_u, peer_v, final_norm_w):
    B, S, _ = x.shape
    cos, sin = rope_2d_tables(S)
    c_act = jax.nn.silu(c)
    cc_act = jax.nn.silu(c_ctx)
    for l in range(DEPTH):
        last = l == DEPTH - 1
        wi = w_in[l]
        mod_x = (c_act @ w_ada[l] + b_ada[l])[:, None, :]
        mod_c = cc_act @ w_ada[l] + b_ada[l]
        sh1, sc1, g1, sh2, sc2, g2 = jnp.split(mod_x, 6, axis=-1)
        csh1, csc1, cg1, csh2, csc2, cg2 = jnp.split(mod_c, 6, axis=-1)
        lg_f = jax.nn.log_sigmoid(ret_decay_fwd[l].astype(jnp.float32))
        lg_b = jax.nn.log_sigmoid(ret_decay_bwd[l].astype(jnp.float32))

        hc = modulate(rms_norm(ctx, norm1_w[l]), csh1, csc1)
        if last:
            kv_na_c = hc @ wi[:, O_NA_K:O_RET_Q]
            kv_ret_c = hc @ wi[:, O_RET_K:O_RET_G]
        else:
            pc = hc @ wi
            kv_na_c = pc[..., O_NA_K:O_RET_Q]
            kv_ret_c = pc[..., O_RET_K:O_RET_G]
        kc_na = split_heads(kv_na_c[..., :NA_WIDTH])
        vc_na = split_heads(kv_na_c[..., NA_WIDTH:])
        kc_ret = split_heads(kv_ret_c[..., :RET_WIDTH]) * HEAD_DIM ** -0.5
        vc_ret = split_heads(kv_ret_c[..., RET_WIDTH:])
        R_f, R_b = context_states(kc_ret, vc_ret, lg_f, lg_b)

        hx = modulate(rms_norm(x, norm1_w[l]), sh1, sc1)
        px = hx @ wi
        q_na = split_heads(px[..., O_NA_Q:O_NA_K])
        k_na = split_heads(px[..., O_NA_K:O_NA_V])
        v_na = split_heads(px[..., O_NA_V:O_RET_Q])
        q_ret = apply_rope_2d(split_heads(px[..., O_RET_Q:O_RET_K]), cos, sin)
        k_ret = apply_rope_2d(split_heads(px[..., O_RET_K:O_RET_V]), cos, sin) * HEAD_DIM ** -0.5
        v_ret = split_heads(px[..., O_RET_V:O_RET_G])
        na_out = neighbourhood_attention(q_na, k_na, v_na, kc_na, vc_na, na_rpb[l])
        ret_out = retention_output(retention_bidir(q_ret, k_ret, v_ret, lg_f, lg_b, R_f, R_b),
                                   px[..., O_RET_G:O_POOL], ret_gn_w[l])
        pool_out = multiscale_pool(px[..., O_POOL:], pool_w[l], pool_scale[l])
        mix = jnp.concatenate([merge_heads(na_out), ret_out, pool_out], axis=-1) @ w_out[l]
        x = x + g1 * mix
        x = x + g2 * peer_ffn(modulate(rms_norm(x, norm2_w[l]), sh2, sc2),
                              peer_wq[l], peer_keys[l], peer_u[l], peer_v[l])

        if not last:
            zero_state = jnp.zeros((B, RET_HEADS, HEAD_DIM, HEAD_DIM), jnp.float32)
            na_c = context_attention(split_heads(pc[..., O_NA_Q:O_NA_K]), kc_na, vc_na)
            ret_c = retention_output(
                retention_bidir(split_heads(pc[..., O_RET_Q:O_RET_K]), kc_ret, vc_ret,
                                lg_f, lg_b, zero_state, zero_state),
                pc[..., O_RET_G:O_POOL], ret_gn_w[l])
            pool_c = multiscale_pool(pc[..., O_POOL:], pool_w[l], pool_scale[l])
            mix_c = jnp.concatenate([merge_heads(na_c), ret_c, pool_c], axis=-1) @ w_out[l]
            ctx = ctx + cg1 * mix_c
            ctx = ctx + cg2 * peer_ffn(modulate(rms_norm(ctx, norm2_w[l]), csh2, csc2),
                                       peer_wq[l], peer_keys[l], peer_u[l], peer_v[l])
    return rms_norm(x, final_norm_w)
```

```python
D = 1024
DPROJ = 2944
NTOK_TM = 1920
NRING = 16
import math
import numpy as np
import ml_dtypes
from contextlib import ExitStack

import concourse.bass as bass
import concourse.mybir as mybir

F32 = mybir.dt.float32
BF16 = mybir.dt.bfloat16
U32 = mybir.dt.uint32
I32 = mybir.dt.int32
AF = mybir.ActivationFunctionType
ALU = mybir.AluOpType
AX = mybir.AxisListType

ENGS = ("pe", "dve", "act", "pool", "sp")
NDSEM = {"sp": 16, "pool": 20, "act": 8}


class Prog:
    def __init__(self, name="k"):
        self.nc = bass.Bass("TRN2", target_bir_lowering=False)
        self.es = ExitStack()
        self.stream = {e: [] for e in ENGS}
        self.cnt = {e: 0 for e in ENGS}
        self.sem = {}
        for e in ENGS:
            self.sem["E" + e] = self.es.enter_context(self.nc.semaphore("s_" + e))
        self.dsem_use = {}
        self.dsem_rr = {}
        for q, n in NDSEM.items():
            for j in range(n):
                key = "D%s%d" % (q, j)
                self.sem[key] = self.es.enter_context(self.nc.semaphore("d_%s%d" % (q, j)))
                self.dsem_use[key] = 0
            self.dsem_rr[q] = 0
        self.known = {e: {} for e in ENGS}
        self.targets = {"E" + e: set() for e in ENGS}
        self.pes = None
        self.nalloc = 0
        self.ncc = 0
        self.ccev = []
        self.rank = {}
        self.sigcount = {"E" + e: 0 for e in ENGS}
        self.emitted = {"E" + e: 0 for e in ENGS}
        self.last_w = {}
        self.readers = {}
        self.uid = 0

    def dram(self, name, shape, dtype, kind):
        return self.nc.dram_tensor(name, list(shape), dtype, kind=kind).ap()

    def sb(self, name, shape, dtype=F32):
        es = self.pes if self.pes is not None else self.es
        self.nalloc += 1
        return es.enter_context(self.nc.sbuf_tensor("%s_%d" % (name, self.nalloc), list(shape), dtype))

    def ps(self, name, shape, dtype=F32):
        es = self.pes if self.pes is not None else self.es
        self.nalloc += 1
        return es.enter_context(self.nc.psum_tensor("%s_%d" % (name, self.nalloc), list(shape), dtype))

    def scratch(self, name, shape, dtype=F32):
        return self.nc.dram_tensor(name, list(shape), dtype)

    def begin_phase(self):
        self.pes = ExitStack()
        self.last_w = {}
        self.readers = {}

    def cc(self, kind, groups, in_t, out_t, reads=(), writes=()):
        key = "CC%d" % self.ncc
        self.ncc += 1
        self.sem[key] = self.es.enter_context(self.nc.semaphore(key.lower()))
        deps = self._deps(reads, writes)
        waits = self._waits("pool", deps)
        fn = lambda e: e.collective_compute(kind, ALU.bypass, replica_groups=groups, ins=[in_t.ap().opt()], outs=[out_t.ap().opt()])
        self.stream["pool"].append((waits, fn, key, "cc"))
        ev = (key, 1)
        self.ccev.append(ev)
        self._commit(ev, reads, writes)
        return ev

    def end_phase(self):
        for e in ENGS:
            deps = {}
            for o in ENGS:
                if self.cnt[o] > 0 and o != "sp":
                    deps["E" + o] = self.cnt[o]
            for key, n in self.dsem_use.items():
                if n > 0:
                    deps[key] = 16 * n
            for key, v in self.ccev:
                deps[key] = v
            waits = self._waits(e, deps)
            if e == "pe" and self.cnt["pe"] > 0:
                self.known["pe"]["Epe"] = self.cnt["pe"]
            self.stream[e].append((waits, None, None, None))
        self._emit()
        if self.pes is not None:
            self.pes.close()
            self.pes = None

    def _deps(self, reads, writes):
        deps = {}

        def add(ev):
            if ev is None:
                return
            s, v = ev
            if deps.get(s, 0) < v:
                deps[s] = v

        for r in reads:
            add(self.last_w.get(r))
        for w in writes:
            add(self.last_w.get(w))
            for ev in self.readers.get(w, ()):
                add(ev)
        return deps

    def _commit(self, ev, reads, writes):
        for r in reads:
            self.readers.setdefault(r, []).append(ev)
        for w in writes:
            self.last_w[w] = ev
            self.readers[w] = []

    def _waits(self, eng, deps):
        out = []
        kn = self.known[eng]
        for s, v in deps.items():
            if eng == "pe" and s == "Epe":
                continue
            if kn.get(s, 0) >= v:
                continue
            kn[s] = v
            out.append((s, v))
            if s[0] == "E":
                self.targets[s].add(v)
        return out

    def ins(self, eng, fn, reads=(), writes=()):
        deps = self._deps(reads, writes)
        waits = self._waits(eng, deps)
        self.cnt[eng] += 1
        ev = ("E" + eng, self.cnt[eng])
        self.stream[eng].append((waits, fn, ev[0], self.cnt[eng]))
        self._commit(ev, reads, writes)
        return ev

    def op(self, eng, method, reads=(), writes=(), **kw):
        return self.ins(eng, lambda e: getattr(e, method)(**kw), reads, writes)

    def dop(self, q, reads=(), writes=(), method="dma_start", **kw):
        return self.dma(q, lambda e: getattr(e, method)(**kw), reads, writes)

    def dma(self, q, fn, reads=(), writes=()):
        deps = self._deps(reads, writes)
        j = self.dsem_rr[q]
        self.dsem_rr[q] = (j + 1) % NDSEM[q]
        key = "D%s%d" % (q, j)
        prev = self.dsem_use[key]
        if prev > 0:
            if deps.get(key, 0) < 16 * prev:
                deps[key] = 16 * prev
        waits = self._waits(q, deps)
        self.dsem_use[key] = prev + 1
        ev = (key, 16 * (prev + 1))
        self.stream[q].append((waits, fn, key, None))
        self._commit(ev, reads, writes)
        return ev

    def _emit(self):
        rank = self.rank
        for skey, tg in self.targets.items():
            new = sorted(i for i in tg if i > self.emitted[skey])
            assert all((skey, i) in rank for i in tg if i <= self.emitted[skey]), "wait on an already-emitted, unsignalled instruction"
            for i in new:
                self.sigcount[skey] += 1
                rank[(skey, i)] = self.sigcount[skey]
        nc = self.nc
        sem = self.sem
        stream = self.stream

        def wval(s, v):
            return rank[(s, v)] if s[0] == "E" else v

        with nc.Block() as block:
            def emit(e, handle):
                for waits, fn, skey, idx in stream[e]:
                    for s, v in waits:
                        handle.wait_ge(sem[s], wval(s, v))
                    if fn is None:
                        continue
                    ins = fn(handle)
                    if idx is None:
                        ins.then_inc(sem[skey], 16)
                    elif idx == "cc":
                        ins.then_inc(sem[skey])
                    elif (skey, idx) in rank:
                        ins.then_inc(sem[skey], 1)

            @block.tensor
            def _(h):
                emit("pe", h)

            @block.vector
            def _(h):
                emit("dve", h)

            @block.scalar
            def _(h):
                emit("act", h)

            @block.gpsimd
            def _(h):
                emit("pool", h)

            @block.sync
            def _(h):
                emit("sp", h)
        for e in ENGS:
            self.emitted["E" + e] = self.cnt[e]
            self.stream[e] = []

    def finish(self):
        self.end_phase()
        self.es.close()
        return self.nc


def phase_A(P, IO, NT, NC):
    NTT = NT + NC
    T = NTT * 128
    P.begin_phase()
    x = IO["x_src"]; ctx = IO["ctx_src"]; cvT = IO["cvT"]; w_ada = IO["w_ada"]; b_ada = IO["b_ada"]; n1w = IO["n1w"]
    w_in = IO["w_in"]; ropec = IO["ropec"]; ropes = IO["ropes"]; dec = IO["dec"]; posf = IO["posf"]; posb = IO["posb"]
    ident_d = IO["ident"]
    mod = IO["d_mod"]; o_qT = IO["d_qT"]; o_kx = IO["d_kx"]; o_kTc = IO["d_kTc"]; o_vx = IO["d_vx"]; o_vaugc = IO["d_vaugc"]
    o_qrT = IO["d_qrT"]; o_krT = IO["d_krT"]; o_kr = IO["d_kr"]; o_vr = IO["d_vr"]; o_gr = IO["d_gr"]
    o_px = IO["d_px"]; o_pxc = IO["d_pxc"]; o_stpp = IO["d_stpp"]; xb = IO["d_xb"]; xf = IO["d_xf"]
    dbg = 0

    identf = P.sb("identf", [128, 128], F32)
    identb = P.sb("identb", [128, 128], BF16)
    cvt = P.sb("cvt", [128, 8, 2], F32)
    scv = P.sb("scv", [128, 8, 2], F32)
    wada = [P.sb("wada%d" % i, [128, 8, 512], F32) for i in range(2)]
    bada = P.sb("bada", [2, 6 * D], F32)
    modrow = P.sb("modrow", [2, 6 * D], F32)
    colsrc = P.sb("colsrc", [40, 128], F32)
    cols = P.sb("cols", [128, 40], F32)
    w1x = P.sb("w1x", [128, 8], F32)
    w1c = P.sb("w1c", [128, 8], F32)
    wstg = [P.sb("wstg%d" % i, [128, DPROJ], F32) for i in range(2)]
    wb = P.sb("wb", [128, 8, DPROJ], BF16)
    dect = P.sb("dect", [128, 12], F32)
    lg = P.sb("lg", [128, 12], F32)
    tmp12 = P.sb("tmp12", [128, 12], F32)
    eps12 = P.sb("eps12", [128, 12], F32)
    posft = P.sb("posft", [128, 64], F32)
    posbt = P.sb("posbt", [128, 64], F32)

    xt = [P.sb("xt%d" % i, [128, D], F32) for i in range(2)]
    rc = [P.sb("rc%d" % i, [128, 32], F32) for i in range(2)]
    rs = [P.sb("rs%d" % i, [128, 32], F32) for i in range(2)]
    junk = P.sb("junk", [128, D], F32)
    ssq = P.sb("ssq", [128, 1], F32)
    rstd = P.sb("rstd", [128, 1], F32)
    xn = P.sb("xn", [128, D], BF16)
    hxT = P.sb("hxT", [128, 8, 128], BF16)
    tm = P.sb("tm", [128, NTOK_TM], F32)
    ra = P.sb("ra", [128, 384], F32)
    rb_ = P.sb("rb", [128, 384], F32)
    qk = P.sb("qk", [128, 768], F32)
    qkb = P.sb("qkb", [128, 768], BF16)
    wF = P.sb("wF", [128, 12], F32)
    kw = P.sb("kw", [128, 768], BF16)
    acc = P.sb("acc", [64, 4, 384], F32)

    s_qT = [P.sb("s_qT%d" % i, [128, 3, 128], BF16) for i in range(2)]
    s_kT = [P.sb("s_kT%d" % i, [128, 3, 128], BF16) for i in range(2)]
    s_v = [P.sb("s_v%d" % i, [128, 6, 65], BF16) for i in range(2)]
    s_qrT = [P.sb("s_qrT%d" % i, [128, 3, 128], BF16) for i in range(2)]
    s_krT = [P.sb("s_krT%d" % i, [128, 3, 128], BF16) for i in range(2)]
    s_vr = [P.sb("s_vr%d" % i, [128, 384], BF16) for i in range(2)]
    s_gr = [P.sb("s_gr%d" % i, [128, 384], F32) for i in range(2)]
    s_pT = [P.sb("s_pT%d" % i, [128, 2, 128], F32) for i in range(2)]

    pT = P.ps("pT", [128, 8, 128], BF16)
    pfm = P.ps("pfm", [128, 8, 128], F32)
    ptm = P.ps("ptm", [128, 2, 512], F32)
    pst = P.ps("pst", [64, 2, 512], F32)
    pmisc = P.ps("pmisc", [128, 512], F32)

    P.dma("sp", lambda e: e.dma_start(out=identf[:], in_=ident_d), writes=["identf"])
    P.dma("sp", lambda e: e.dma_start(out=cvt[:], in_=cvT), writes=["cvt"])
    P.dma("sp", lambda e: e.dma_start(out=bada[0:1, :], in_=b_ada), writes=["bada0"])
    P.dma("sp", lambda e: e.dma_start(out=bada[1:2, :], in_=b_ada), writes=["bada1"])
    P.dma("sp", lambda e: e.dma_start(out=dect[:], in_=dec), writes=["dect"])
    P.dma("sp", lambda e: e.dma_start(out=posft[:, 0:NTT], in_=posf), writes=["posft"])
    P.dma("sp", lambda e: e.dma_start(out=posbt[:, 0:NTT], in_=posb), writes=["posbt"])
    P.dma("sp", lambda e: e.dma_start(out=colsrc[32:40, :], in_=n1w), writes=["colsrc_n"])
    P.ins("dve", lambda e: e.tensor_copy(out=identb[:], in_=identf[:]), reads=["identf"], writes=["identb"])
    P.ins("act", lambda e: e.activation(out=scv[:], in_=cvt[:], func=AF.Silu), reads=["cvt"], writes=["scv"])
    P.ins("dve", lambda e: e.memset(acc[:], 0.0), writes=["acc"])
    for i_ in range(2):
        P.op("dve", "memset", [], ["s_v%d" % i_], ap=s_v[i_][:], constant=1.0)

    for nb in range(12):
        wa = wada[nb % 2]
        wtok = "wada%d" % (nb % 2)
        P.dma("sp", lambda e, wa=wa, nb=nb: e.dma_start(
            out=wa[:], in_=w_ada[:, nb * 512:(nb + 1) * 512].rearrange("(c p) n -> p c n", p=128)),
            writes=[wtok])
        for c in range(8):
            P.ins("pe", lambda e, wa=wa, c=c: e.matmul(out=pmisc[0:2, :], lhsT=scv[:, c, :], rhs=wa[:, c, :],
                                                       start=(c == 0), stop=(c == 7)),
                  reads=[wtok, "scv"], writes=["pmisc"])
        P.ins("dve", lambda e, nb=nb: e.tensor_tensor(out=modrow[:, nb * 512:(nb + 1) * 512], in0=pmisc[0:2, :],
                                                      in1=bada[:, nb * 512:(nb + 1) * 512], op=ALU.add),
              reads=["pmisc", "bada0", "bada1"], writes=["modrow"])
    P.dma("sp", lambda e: e.dma_start(out=mod, in_=modrow[:]), reads=["modrow"], writes=["mod_dram"])
    for v, (r, off) in enumerate([(0, 0), (0, D), (1, 0), (1, D)]):
        P.dma("sp", lambda e, v=v, r=r, off=off: e.dma_start(
            out=colsrc[v * 8:(v + 1) * 8, :],
            in_=mod[r:r + 1, off:off + D].rearrange("o (c p) -> (o c) p", p=128)),
            reads=["mod_dram"], writes=["colsrc%d" % v])
    P.ins("pe", lambda e: e.transpose(out=pmisc[:, 0:40], in_=colsrc[:, :], identity=identf[0:40, 0:40]),
          reads=["colsrc_n", "colsrc0", "colsrc1", "colsrc2", "colsrc3", "identf", "modrow"], writes=["pmisc"])
    P.ins("dve", lambda e: e.tensor_copy(out=cols[:], in_=pmisc[:, 0:40]), reads=["pmisc"], writes=["cols"])
    P.ins("dve", lambda e: e.scalar_tensor_tensor(out=w1x[:], in0=cols[:, 8:16], scalar=1.0, in1=cols[:, 32:40],
                                                  op0=ALU.add, op1=ALU.mult), reads=["cols"], writes=["w1x"])
    P.ins("dve", lambda e: e.scalar_tensor_tensor(out=w1c[:], in0=cols[:, 24:32], scalar=1.0, in1=cols[:, 32:40],
                                                  op0=ALU.add, op1=ALU.mult), reads=["cols"], writes=["w1c"])
    P.ins("act", lambda e: e.activation(out=eps12[:], in_=dect[:], func=AF.Exp, scale=-1.0), reads=["dect"], writes=["eps12"])
    P.ins("dve", lambda e: e.tensor_scalar(out=tmp12[:], in0=eps12[:], scalar1=-0.25, scalar2=1.0 / 3, op0=ALU.mult, op1=ALU.add),
          reads=["eps12"], writes=["tmp12"])
    P.ins("dve", lambda e: e.tensor_tensor(out=tmp12[:], in0=tmp12[:], in1=eps12[:], op=ALU.mult), reads=["tmp12", "eps12"], writes=["tmp12"])
    P.ins("dve", lambda e: e.tensor_scalar(out=tmp12[:], in0=tmp12[:], scalar1=-1.0, scalar2=0.5, op0=ALU.mult, op1=ALU.add),
          reads=["tmp12"], writes=["tmp12"])
    P.ins("dve", lambda e: e.tensor_tensor(out=tmp12[:], in0=tmp12[:], in1=eps12[:], op=ALU.mult), reads=["tmp12", "eps12"], writes=["tmp12"])
    P.ins("dve", lambda e: e.tensor_scalar(out=tmp12[:], in0=tmp12[:], scalar1=-1.0, scalar2=1.0, op0=ALU.mult, op1=ALU.add),
          reads=["tmp12"], writes=["tmp12"])
    P.ins("dve", lambda e: e.scalar_tensor_tensor(out=lg[:], in0=tmp12[:], scalar=-1.0, in1=eps12[:], op0=ALU.mult, op1=ALU.mult),
          reads=["tmp12", "eps12"], writes=["lg"])

    for c in range(8):
        ws = wstg[c % 2]
        wtok = "wstg%d" % (c % 2)
        P.dma("sp", lambda e, ws=ws, c=c: e.dma_start(out=ws[:], in_=w_in[c * 128:(c + 1) * 128, :]), writes=[wtok])
        eng = "dve"
        P.ins(eng, lambda e, ws=ws, c=c: e.tensor_copy(out=wb[:, c, :], in_=ws[:]), reads=[wtok], writes=["wb%d" % c])
    WB = ["wb%d" % c for c in range(8)]

    fm_cols = [0, 128, 256, 384, 512, 640, 2688, 2816]
    for it in range(NTT):
        b = it % 2
        is_ctx = it >= NT
        src = ctx[(it - NT) * 128:(it - NT + 1) * 128, :] if is_ctx else x[it * 128:(it + 1) * 128, :]
        w1 = w1c if is_ctx else w1x
        shc = 16 if is_ctx else 0
        t0 = it * 128
        X, RC, RS = xt[b], rc[b], rs[b]
        xtok, rctok, rstok = "xt%d" % b, "rc%d" % b, "rs%d" % b
        P.dma("sp", lambda e, X=X, src=src: e.dma_start(out=X[:], in_=src), writes=[xtok])
        P.dma("sp", lambda e, RC=RC, t0=t0: e.dma_start(out=RC[:], in_=ropec[t0:t0 + 128, :]), writes=[rctok])
        P.dma("sp", lambda e, RS=RS, t0=t0: e.dma_start(out=RS[:], in_=ropes[t0:t0 + 128, :]), writes=[rstok])
        P.ins("act", lambda e, X=X: e.activation(out=junk[:], in_=X[:], func=AF.Square, accum_out=ssq[:]),
              reads=[xtok], writes=["junk", "ssq"])
        P.ins("act", lambda e: e.activation(out=rstd[:], in_=ssq[:], func=AF.Sqrt, scale=1.0 / D, bias=1e-6),
              reads=["ssq"], writes=["rstd"])
        P.ins("dve", lambda e: e.reciprocal(out=rstd[:], in_=rstd[:]), reads=["rstd"], writes=["rstd"])
        P.ins("dve", lambda e, X=X: e.tensor_scalar(out=xn[:], in0=X[:], scalar1=rstd[:, 0:1], scalar2=None, op0=ALU.mult),
              reads=[xtok, "rstd"], writes=["xn"])
        for c in range(8):
            P.ins("pe", lambda e, c=c: e.transpose(out=pT[:, c, :], in_=xn[:, c * 128:(c + 1) * 128], identity=identb[:]),
                  reads=["xn", "identb"], writes=["pT"])
        for c in range(8):
            P.ins("act", lambda e, c=c, w1=w1, shc=shc: e.activation(
                out=hxT[:, c, :], in_=pT[:, c, :], func=AF.Identity,
                scale=w1[:, c:c + 1], bias=cols[:, shc + c:shc + c + 1]),
                reads=["pT", "w1x", "w1c", "cols"], writes=["hxT"])
        for g, col in enumerate(fm_cols):
            for c in range(8):
                P.ins("pe", lambda e, g=g, col=col, c=c: e.matmul(
                    out=pfm[:, g, :], lhsT=wb[:, c, col:col + 128], rhs=hxT[:, c, :], start=(c == 0), stop=(c == 7)),
                    reads=["hxT"] + WB, writes=["pfm"])
        P.ins("act", lambda e, b=b: e.copy(out=s_qT[b][:], in_=pfm[:, 0:3, :]), reads=["pfm"], writes=["s_qT%d" % b])
        P.ins("act", lambda e, b=b: e.copy(out=s_kT[b][:], in_=pfm[:, 3:6, :]), reads=["pfm"], writes=["s_kT%d" % b])
        P.ins("act", lambda e, b=b: e.copy(out=s_pT[b][:], in_=pfm[:, 6:8, :]), reads=["pfm"], writes=["s_pT%d" % b])
        P.dop("sp", reads=["s_qT%d" % b], out=o_qT[:, it], in_=s_qT[b][:])
        if is_ctx:
            P.dop("sp", reads=["s_kT%d" % b], out=o_kTc[:, :, (it - NT) * 128:(it - NT + 1) * 128], in_=s_kT[b][:])
        else:
            P.dop("sp", reads=["s_kT%d" % b], out=o_kx[:, :, (2 + it) * 128:(3 + it) * 128], in_=s_kT[b][:])
            if it < 2:
                P.dop("sp", reads=["s_kT%d" % b], out=xb[:, 0:768].rearrange("p (c t) -> p c t", c=3)[:, :, it * 128:(it + 1) * 128], in_=s_kT[b][:])
            if it >= NT - 2:
                j_ = it - (NT - 2)
                P.dop("sp", reads=["s_kT%d" % b], out=xb[:, 768:1536].rearrange("p (c t) -> p c t", c=3)[:, :, j_ * 128:(j_ + 1) * 128], in_=s_kT[b][:])
        if is_ctx:
            P.dop("sp", reads=["s_pT%d" % b], out=o_pxc[:, :, 8 + (it - NT) * 128:8 + (it - NT + 1) * 128], in_=s_pT[b][:])
        else:
            P.dop("sp", reads=["s_pT%d" % b], out=o_px[:, :, 8 + it * 128:8 + (it + 1) * 128], in_=s_pT[b][:])
            if it == 0:
                P.dop("sp", reads=["s_pT%d" % b], out=xf[:, 0:16].rearrange("p (c t) -> p c t", c=2), in_=s_pT[b][:, :, 0:8])
            if it == NT - 1:
                P.dop("sp", reads=["s_pT%d" % b], out=xf[:, 16:32].rearrange("p (c t) -> p c t", c=2), in_=s_pT[b][:, :, 120:128])
        for hf in range(2):
            for j in range(2):
                col = 768 + (hf * 2 + j) * 480
                for c in range(8):
                    P.ins("pe", lambda e, j=j, col=col, c=c: e.matmul(
                        out=ptm[:, j, 0:480], lhsT=hxT[:, c, :], rhs=wb[:, c, col:col + 480], start=(c == 0), stop=(c == 7)),
                        reads=["hxT"] + WB, writes=["ptm"])
            P.ins("act", lambda e, hf=hf: e.copy(
                out=tm[:, hf * 960:(hf + 1) * 960].rearrange("p (j n) -> p j n", j=2), in_=ptm[:, :, 0:480]),
                reads=["ptm"], writes=["tm"])
        P.ins("act", lambda e, b=b: e.copy(out=s_v[b][:, :, 0:64], in_=tm[:, 0:384].rearrange("p (h e) -> p h e", h=6)), reads=["tm"], writes=["s_v%d" % b])
        P.ins("act", lambda e, b=b: e.copy(out=s_vr[b][:], in_=tm[:, 1152:1536]), reads=["tm"], writes=["s_vr%d" % b])
        P.ins("act", lambda e, b=b: e.copy(out=s_gr[b][:], in_=tm[:, 1536:1920]), reads=["tm"], writes=["s_gr%d" % b])
        if is_ctx:
            P.dop("sp", reads=["s_v%d" % b], out=o_vaugc[:, it - NT], in_=s_v[b][:])
        else:
            P.dop("sp", reads=["s_v%d" % b], out=o_vx[:, 2 + it], in_=s_v[b][:])
            if it < 2:
                P.dop("sp", reads=["s_v%d" % b], out=xb[:, 1536:2316].rearrange("p (k h e) -> p k h e", k=2, h=6)[:, it], in_=s_v[b][:])
            if it >= NT - 2:
                P.dop("sp", reads=["s_v%d" % b], out=xb[:, 2316:3096].rearrange("p (k h e) -> p k h e", k=2, h=6)[:, it - (NT - 2)], in_=s_v[b][:])
        P.dop("sp", reads=["s_vr%d" % b], out=o_vr[:, it, :], in_=s_vr[b][:])
        P.dop("sp", reads=["s_gr%d" % b], out=o_gr[:, it, :], in_=s_gr[b][:])
        src5 = tm[:, 384:1152].rearrange("p (h f s i) -> p h f s i", h=12, f=2, s=2, i=16)
        dst5 = qk[:].rearrange("p (h f s i) -> p h f s i", h=12, f=2, s=2, i=16)
        ra4 = ra[:].rearrange("p (h f i) -> p h f i", h=12, f=2, i=16)
        rb4 = rb_[:].rearrange("p (h f i) -> p h f i", h=12, f=2, i=16)
        cosb = RC[:].rearrange("p (f i) -> p f i", f=2).unsqueeze(1).to_broadcast([128, 12, 2, 16])
        sinb = RS[:].rearrange("p (f i) -> p f i", f=2).unsqueeze(1).to_broadcast([128, 12, 2, 16])
        A_ = src5[:, :, :, 0, :]
        B_ = src5[:, :, :, 1, :]
        P.ins("dve", lambda e, A_=A_, cosb=cosb: e.tensor_tensor(out=ra4, in0=A_, in1=cosb, op=ALU.mult), reads=["tm", rctok], writes=["ra"])
        P.ins("dve", lambda e, B_=B_, sinb=sinb: e.tensor_tensor(out=rb4, in0=B_, in1=sinb, op=ALU.mult), reads=["tm", rstok], writes=["rb"])
        P.ins("dve", lambda e, dst5=dst5: e.tensor_tensor(out=dst5[:, :, :, 0, :], in0=ra4, in1=rb4, op=ALU.subtract), reads=["ra", "rb"], writes=["qk_a"])
        P.ins("dve", lambda e, A_=A_, sinb=sinb: e.tensor_tensor(out=ra4, in0=A_, in1=sinb, op=ALU.mult), reads=["tm", rstok], writes=["ra"])
        P.ins("dve", lambda e, B_=B_, cosb=cosb: e.tensor_tensor(out=rb4, in0=B_, in1=cosb, op=ALU.mult), reads=["tm", rctok], writes=["rb"])
        P.ins("dve", lambda e, dst5=dst5: e.tensor_tensor(out=dst5[:, :, :, 1, :], in0=ra4, in1=rb4, op=ALU.add), reads=["ra", "rb"], writes=["qk_b"])
        P.ins("act", lambda e: e.copy(out=qkb[:], in_=qk[:]), reads=["qk_a", "qk_b"], writes=["qkb"])
        P.dop("sp", reads=["qkb"], out=o_kr[:, it, :], in_=qkb[:, 384:768])
        for c in range(6):
            P.ins("pe", lambda e, c=c: e.transpose(out=pT[:, c, :], in_=qkb[:, c * 128:(c + 1) * 128], identity=identb[:]),
                  reads=["qkb", "identb"], writes=["pT"])
        P.ins("act", lambda e, b=b: e.copy(out=s_qrT[b][:], in_=pT[:, 0:3, :]), reads=["pT"], writes=["s_qrT%d" % b])
        P.ins("act", lambda e, b=b: e.copy(out=s_krT[b][:], in_=pT[:, 3:6, :]), reads=["pT"], writes=["s_krT%d" % b])
        P.dop("sp", reads=["s_qrT%d" % b], out=o_qrT[:, it], in_=s_qrT[b][:])
        P.dop("sp", reads=["s_krT%d" % b], out=o_krT[:, it], in_=s_krT[b][:])
        P.ins("act", lambda e, it=it: e.activation(out=wF[:, 0:6], in_=lg[:, 0:6], func=AF.Exp, scale=posft[:, it:it + 1],
                                                   bias=math.log(0.125)), reads=["lg", "posft"], writes=["wF0"])
        P.ins("act", lambda e, it=it: e.activation(out=wF[:, 6:12], in_=lg[:, 6:12], func=AF.Exp, scale=posbt[:, it:it + 1],
                                                   bias=math.log(0.125)), reads=["lg", "posbt"], writes=["wF1"])
        for d_ in range(2):
            P.ins("dve", lambda e, d_=d_: e.tensor_tensor(
                out=kw[:, d_ * 384:(d_ + 1) * 384].rearrange("p (h e) -> p h e", h=6),
                in0=qk[:, 384:768].rearrange("p (h e) -> p h e", h=6),
                in1=wF[:, d_ * 6:(d_ + 1) * 6].unsqueeze(2).to_broadcast([128, 6, 64]), op=ALU.mult),
                reads=["qk_a", "qk_b", "wF0", "wF1"], writes=["kw%d" % d_])
        for d_ in range(2):
            for h in range(6):
                P.ins("pe", lambda e, d_=d_, h=h, b=b: e.matmul(
                    out=pst[:, d_, h * 64:(h + 1) * 64], lhsT=kw[:, d_ * 384 + h * 64:d_ * 384 + (h + 1) * 64],
                    rhs=s_vr[b][:, h * 64:(h + 1) * 64], start=True, stop=True),
                    reads=["kw%d" % d_, "s_vr%d" % b], writes=["pst"])
        so = 2 if is_ctx else 0
        for d_ in range(2):
            P.ins("dve", lambda e, d_=d_, so=so: e.tensor_tensor(out=acc[:, so + d_, :], in0=acc[:, so + d_, :],
                                                                  in1=pst[:, d_, 0:384], op=ALU.add),
                  reads=["pst", "acc"], writes=["acc"])

    acc5 = acc[:].rearrange("d s (c j e) -> d s c j e", j=2, e=64)
    for j_ in range(2):
        P.dop("sp", reads=["acc"], out=o_stpp[j_ * 64:(j_ + 1) * 64], in_=acc5[:, :, :, j_, :])
        P.dop("sp", reads=["acc"], out=xf[j_ * 64:(j_ + 1) * 64, 32:416].rearrange("p (s c e) -> p s c e", s=2, c=3), in_=acc5[:, 0:2, :, j_, :])
    P.end_phase()


def phase_X(P, IO, NT, ncores=8):
    P.begin_phase()
    xb_t = IO["t_xb"]; xf_t = IO["t_xf"]; gb_t = IO["t_gb"]; gf_t = IO["t_gf"]
    gb = IO["d_gb"]; gf = IO["d_gf"]; kx = IO["d_kx"]; vx = IO["d_vx"]; px = IO["d_px"]; pxc = IO["d_pxc"]; sel = IO["sel"]
    groups = [[2 * i, 2 * i + 1] for i in range(ncores // 2)]
    P.cc("AllGather", groups, xb_t, gb_t, writes=["gb"])
    P.cc("AllGather", groups, xf_t, gf_t, writes=["gf"])
    kb = P.sb("kb", [128, 2, 768], BF16)
    vb = P.sb("vb", [128, 2, 780], BF16)
    hal = P.sb("hal", [128, 2, 16]); sels = P.sb("sels", [128, 4]); zer = P.sb("zer", [128, 16])
    P.dop("sp", writes=["sels"], out=sels[:], in_=sel)
    P.op("dve", "memset", [], ["zer"], ap=zer[:], constant=0.0)
    P.dop("sp", reads=["gb"], writes=["kb0"], out=kb[:, 0, :], in_=gb[0:128, 768:1536])
    P.dop("sp", reads=["gb"], writes=["kb1"], out=kb[:, 1, :], in_=gb[128:256, 0:768])
    P.dop("sp", reads=["gb"], writes=["vb0"], out=vb[:, 0, :], in_=gb[0:128, 2316:3096])
    P.dop("sp", reads=["gb"], writes=["vb1"], out=vb[:, 1, :], in_=gb[128:256, 1536:2316])
    P.dop("sp", reads=["kb0"], out=kx[:, :, 0:256], in_=kb[:, 0, :].rearrange("p (c t) -> p c t", c=3))
    P.dop("sp", reads=["kb1"], out=kx[:, :, (NT + 2) * 128:(NT + 4) * 128], in_=kb[:, 1, :].rearrange("p (c t) -> p c t", c=3))
    P.dop("sp", reads=["vb0"], out=vx[:, 0:2], in_=vb[:, 0, :].rearrange("p (k h e) -> p k h e", k=2, h=6))
    P.dop("sp", reads=["vb1"], out=vx[:, NT + 2:NT + 4], in_=vb[:, 1, :].rearrange("p (k h e) -> p k h e", k=2, h=6))
    P.dop("sp", reads=["gf"], writes=["hal0"], out=hal[:, 0, :], in_=gf[0:128, 16:32])
    P.dop("sp", reads=["gf"], writes=["hal1"], out=hal[:, 1, :], in_=gf[128:256, 0:16])
    P.op("dve", "tensor_scalar", ["hal0", "sels"], ["hal0"], out=hal[:, 0, :], in0=hal[:, 0, :], scalar1=sels[:, 0:1], scalar2=None, op0=ALU.mult)
    P.op("dve", "tensor_scalar", ["hal1", "sels"], ["hal1"], out=hal[:, 1, :], in0=hal[:, 1, :], scalar1=sels[:, 1:2], scalar2=None, op0=ALU.mult)
    P.dop("sp", reads=["hal0"], out=px[:, :, 0:8], in_=hal[:, 0, :].rearrange("p (c t) -> p c t", c=2))
    P.dop("sp", reads=["hal1"], out=px[:, :, 8 + NT * 128:16 + NT * 128], in_=hal[:, 1, :].rearrange("p (c t) -> p c t", c=2))
    P.dop("sp", reads=["zer"], out=pxc[:, :, 0:8], in_=zer[:].rearrange("p (c t) -> p c t", c=2))
    P.dop("sp", reads=["zer"], out=pxc[:, :, 264:272], in_=zer[:].rearrange("p (c t) -> p c t", c=2))
    P.end_phase()


def phase_B1(P, IO, NT, NC):
    NTT = NT + NC
    T = NTT * 128
    stages = 15
    P.begin_phase()
    x = IO["x_src"]; mod = IO["d_mod"]; qT = IO["d_qT"]; kx = IO["d_kx"]; vx = IO["d_vx"]; kTc = IO["d_kTc"]; vaugc = IO["d_vaugc"]
    G = IO["G"]; M01 = IO["M01"]; qrT = IO["d_qrT"]; krT = IO["d_krT"]; kr = IO["d_kr"]; vr = IO["d_vr"]; gr = IO["d_gr"]
    st_own = IO["d_stpp"]; gf = IO["d_gf"]; npow = IO["npow"]; sel = IO["sel"]; dec_row = IO["dec_row"]; dec_col = IO["dec_col"]
    cst = IO["cst"]; pm = IO["pm"]; ident_d = IO["ident"]; gnw = IO["gnw"]; px = IO["d_px"]; pxc = IO["d_pxc"]; invc = IO["invc"]
    wpool = IO["wpool"]; pscale = IO["pscale"]; w_out = IO["w_out"]; x1 = IO["x1_dst"]

    identf = P.sb("identf", [128, 128]); identb = P.sb("identb", [128, 128], BF16)
    wostg = [P.sb("wostg%d" % i, [128, 512]) for i in range(2)]
    wo = P.sb("wo", [128, 8, D], BF16)
    Gs = [P.sb("Gs%d" % i, [128, 896]) for i in range(2)]
    m01 = P.sb("m01", [128, 896])
    EB = P.sb("EB", [128, 5, 6, 896], BF16)
    kTc_s = P.sb("kTc_s", [128, 3, 256], BF16)
    vaugc_s = P.sb("vaugc_s", [128, 2, 6, 65], BF16)
    decr = P.sb("decr", [128, 12]); decc = P.sb("decc", [128, 6])
    lgr = P.sb("lgr", [128, 12]); lgc = P.sb("lgc", [128, 6])
    t12 = P.sb("t12", [128, 12]); e12 = P.sb("e12", [128, 12])
    cs = P.sb("cs", [128, 6, 128])
    pms = P.sb("pms", [128, 2])
    DTf = P.sb("DTf", [128, 6, 128]); DTb = P.sb("DTb", [128, 6, 128])
    XIf = P.sb("XIf", [128, 3, 128]); XIb = P.sb("XIb", [128, 3, 128])
    ZF = P.sb("ZF", [128, 6]); ZB = P.sb("ZB", [128, 6])
    cdrow = P.sb("cdrow", [128, 12])
    npw = P.sb("npw", [128, 2])
    sto = P.sb("sto", [128, 4, 3, 64]); stt = P.sb("stt", [128, 2, 3, 64]); sels = P.sb("sels", [128, 4]); hal = P.sb("hal", [128, 2, 16])
    scl = P.sb("scl", [128, 6])
    Rf = P.sb("Rf", [128, 3, 64]); Rb = P.sb("Rb", [128, 3, 64]); Rtmp = P.sb("Rtmp", [128, 3, 64])
    Rfb = P.sb("Rfb", [128, 3, 64], BF16)
    Rbs = P.sb("Rbs", [128, NTT, 3, 64], BF16)
    g1x = P.sb("g1x", [128, D]); g1c = P.sb("g1c", [128, D])
    gnwb = P.sb("gnwb", [128, 384])
    invcs = P.sb("invcs", [128, 5, 2, 128])
    wpf = P.sb("wpf", [128, 2, 128]); wpb = P.sb("wpb", [128, 2, 128], BF16)
    psc = P.sb("psc", [128, 2])

    xt = [P.sb("xt%d" % i, [128, D]) for i in range(2)]
    qTt = [P.sb("qTt%d" % i, [128, 3, 128], BF16) for i in range(2)]
    kwt = [P.sb("kwt%d" % i, [128, 3, 896], BF16) for i in range(2)]
    vwt = [P.sb("vwt%d" % i, [128, 7, 6, 65], BF16) for i in range(2)]
    qrt = [P.sb("qrt%d" % i, [128, 3, 128], BF16) for i in range(2)]
    krt = [P.sb("krt%d" % i, [128, 3, 128], BF16) for i in range(2)]
    krm = [P.sb("krm%d" % i, [128, 384], BF16) for i in range(2)]
    vrm = [P.sb("vrm%d" % i, [128, 384], BF16) for i in range(2)]
    grm = [P.sb("grm%d" % i, [128, 384]) for i in range(2)]
    ppd = [P.sb("ppd%d" % i, [128, 2, 144]) for i in range(2)]
    krm2 = [P.sb("krm2%d" % i, [128, 384], BF16) for i in range(2)]
    vrm2 = [P.sb("vrm2%d" % i, [128, 384], BF16) for i in range(2)]
    kz = P.sb("kz", [128, 384], BF16)
    pexp = P.sb("pexp", [128, 9, 128], BF16)
    rcp = P.sb("rcp", [128, 6])
    mixtok = P.sb("mixtok", [128, 768], BF16)
    mixT = P.sb("mixT", [128, 8, 128], BF16)
    SDf = P.sb("SDf", [128, 6, 128], BF16); SDb = P.sb("SDb", [128, 6, 128], BF16)
    qxf = P.sb("qxf", [128, 3, 128], BF16); qxb = P.sb("qxb", [128, 3, 128], BF16)
    ysum = P.sb("ysum", [128, 6]); yd = P.sb("yd", [128, 384]); yq = P.sb("yq", [128, 384])
    yv = P.sb("yv", [128, 6]); sg = P.sb("sg", [128, 384])
    a2 = P.sb("a2", [128, 2, 143]); a4 = P.sb("a4", [128, 2, 141]); a8 = P.sb("a8", [128, 2, 137]); a16 = P.sb("a16", [128, 2, 128])
    pdm = P.sb("pdm", [128, 2, 128])
    pdf = P.sb("pdf", [128, 2, 128], BF16)
    otmp = P.sb("otmp", [128, D])

    pS = P.ps("pS", [128, 8, 128])
    po = P.ps("po", [128, 6, 65])
    py = P.ps("py", [128, 6, 64])
    pmi = P.ps("pmi", [128, 512])
    ptr = P.ps("ptr", [128, 8, 128], BF16)
    pout = P.ps("pout", [128, 2, 512])

    ld = lambda out, in_, w, r=(): P.dop("sp", reads=list(r), writes=[w], out=out, in_=in_)

    if stages != 15:
        P.op("dve", "memset", [], ["mixtok_a", "mixtok_b"], ap=mixtok[:], constant=0.0)
        P.op("dve", "memset", [], ["mixT_p", "mixT_t"], ap=mixT[:], constant=0.0)
        for it_ in range(NTT):
            P.op("dve", "memset", [], ["Rbs%d" % it_], ap=Rbs[:, it_, :, :], constant=0.0)
    ld(identf[:], ident_d, "identf")
    P.op("dve", "tensor_copy", ["identf"], ["identb"], out=identb[:], in_=identf[:])
    ld(kTc_s[:], kTc, "kTc_s"); ld(vaugc_s[:], vaugc, "vaugc_s")
    ld(decr[:], dec_row, "decr"); ld(decc[:], dec_col, "decc"); ld(cs[:], cst, "cs"); ld(pms[:], pm, "pms")
    ld(npw[:], npow, "npw"); ld(sto[:], st_own, "sto"); ld(sels[:], sel, "sels")
    ld(stt[:, 0], gf[0:128, 32:224].rearrange("p (c e) -> p c e", c=3), "stt0")
    ld(stt[:, 1], gf[128:256, 224:416].rearrange("p (c e) -> p c e", c=3), "stt1")
    ld(g1x[:], mod[0:1, 2 * D:3 * D].partition_broadcast(128), "g1x")
    ld(g1c[:], mod[1:2, 2 * D:3 * D].partition_broadcast(128), "g1c")
    ld(gnwb[:], gnw.partition_broadcast(128), "gnwb")
    ld(invcs[:], invc, "invcs"); ld(wpf[:], wpool, "wpf"); ld(psc[:], pscale, "psc")
    P.op("dve", "tensor_copy", ["wpf"], ["wpb"], out=wpb[:], in_=wpf[:])
    for c in range(8):
        for hf in range(2):
            ld(wostg[hf][:], w_out[c * 128:(c + 1) * 128, hf * 512:(hf + 1) * 512], "wostg%d" % hf)
            P.op("dve" if hf == 0 else "act", "tensor_copy" if hf == 0 else "copy", ["wostg%d" % hf], ["wo%d" % c], out=wo[:, c, hf * 512:(hf + 1) * 512], in_=wostg[hf][:])
    WO = ["wo%d" % c for c in range(8)]
    for s in range(5):
        ld(m01[:], M01[:, s, :], "m01")
        for h in range(6):
            i = (s * 6 + h) % 2
            ld(Gs[i][:], G[:, s, h, :], "Gs%d" % i)
            P.op("act", "activation", ["Gs%d" % i], ["Gs%d" % i], out=Gs[i][:], in_=Gs[i][:], func=AF.Exp)
            P.op("dve", "tensor_tensor", ["Gs%d" % i, "m01"], ["EB"], out=EB[:, s, h, :], in0=Gs[i][:], in1=m01[:], op=ALU.mult)

    def logsig(dst, src, n, stok, dtok):
        P.op("act", "activation", [stok], ["e12"], out=e12[:, 0:n], in_=src, func=AF.Exp, scale=-1.0)
        P.op("dve", "tensor_scalar", ["e12"], ["t12"], out=t12[:, 0:n], in0=e12[:, 0:n], scalar1=-0.25, scalar2=1.0 / 3, op0=ALU.mult, op1=ALU.add)
        P.op("dve", "tensor_tensor", ["t12", "e12"], ["t12"], out=t12[:, 0:n], in0=t12[:, 0:n], in1=e12[:, 0:n], op=ALU.mult)
        P.op("dve", "tensor_scalar", ["t12"], ["t12"], out=t12[:, 0:n], in0=t12[:, 0:n], scalar1=-1.0, scalar2=0.5, op0=ALU.mult, op1=ALU.add)
        P.op("dve", "tensor_tensor", ["t12", "e12"], ["t12"], out=t12[:, 0:n], in0=t12[:, 0:n], in1=e12[:, 0:n], op=ALU.mult)
        P.op("dve", "tensor_scalar", ["t12"], ["t12"], out=t12[:, 0:n], in0=t12[:, 0:n], scalar1=-1.0, scalar2=1.0, op0=ALU.mult, op1=ALU.add)
        P.op("dve", "scalar_tensor_tensor", ["t12", "e12"], [dtok], out=dst, in0=t12[:, 0:n], scalar=-1.0, in1=e12[:, 0:n], op0=ALU.mult, op1=ALU.mult)
    logsig(lgr[:], decr[:], 12, "decr", "lgr")
    logsig(lgc[:], decc[:], 6, "decc", "lgc")
    sidx = lambda h: (h % 2) * 3 + h // 2
    for h in range(6):
        P.op("act", "activation", ["lgr", "cs"], ["DTf%d" % h], out=DTf[:, sidx(h), :], in_=cs[:, 0, :], func=AF.Exp, scale=lgr[:, h:h + 1])
        P.op("dve", "tensor_tensor", ["DTf%d" % h, "cs"], ["DTf%d" % h], out=DTf[:, sidx(h), :], in0=DTf[:, sidx(h), :], in1=cs[:, 1, :], op=ALU.mult)
        P.op("act", "activation", ["lgr", "cs"], ["DTb%d" % h], out=DTb[:, sidx(h), :], in_=cs[:, 2, :], func=AF.Exp, scale=lgr[:, 6 + h:7 + h])
        P.op("dve", "tensor_tensor", ["DTb%d" % h, "cs"], ["DTb%d" % h], out=DTb[:, sidx(h), :], in0=DTb[:, sidx(h), :], in1=cs[:, 3, :], op=ALU.mult)
    DT = ["DTf%d" % h for h in range(6)] + ["DTb%d" % h for h in range(6)]
    for c in range(3):
        P.op("act", "activation", ["lgc", "cs"], ["XI"], out=XIf[:, c, :], in_=cs[:, 4, :], func=AF.Exp, scale=lgc[:, c:c + 1])
        P.op("act", "activation", ["lgc", "cs"], ["XI"], out=XIb[:, c, :], in_=cs[:, 5, :], func=AF.Exp, scale=lgc[:, 3 + c:4 + c])
    P.op("act", "activation", ["lgr", "pms"], ["ZF"], out=ZF[:], in_=lgr[:, 0:6], func=AF.Exp, scale=pms[:, 0:1], bias=math.log(0.125))
    P.op("act", "activation", ["lgr", "pms"], ["ZB"], out=ZB[:], in_=lgr[:, 6:12], func=AF.Exp, scale=pms[:, 1:2], bias=math.log(0.125))
    P.op("act", "activation", ["lgr"], ["cdrow"], out=cdrow[:], in_=lgr[:], func=AF.Exp, scale=128.0)
    P.op("act", "activation", ["lgc", "npw"], ["scl"], out=scl[:, 0:3], in_=lgc[:, 0:3], func=AF.Exp, scale=npw[:, 0:1])
    P.op("act", "activation", ["lgc", "npw"], ["scl"], out=scl[:, 3:6], in_=lgc[:, 3:6], func=AF.Exp, scale=npw[:, 1:2])
    P.op("dve", "tensor_tensor", ["sto", "scl"], ["Rf"], out=Rf[:], in0=sto[:, 2, :, :], in1=scl[:, 0:3].unsqueeze(2).to_broadcast([128, 3, 64]), op=ALU.mult)
    P.op("dve", "scalar_tensor_tensor", ["Rf", "stt0", "sels"], ["Rf"], out=Rf[:].rearrange("p c e -> p (c e)"), in0=stt[:, 0, :, :].rearrange("p c e -> p (c e)"), scalar=sels[:, 2:3], in1=Rf[:].rearrange("p c e -> p (c e)"), op0=ALU.mult, op1=ALU.add)
    P.op("dve", "tensor_tensor", ["sto", "scl"], ["Rb"], out=Rb[:], in0=sto[:, 3, :, :], in1=scl[:, 3:6].unsqueeze(2).to_broadcast([128, 3, 64]), op=ALU.mult)
    P.op("dve", "scalar_tensor_tensor", ["Rb", "stt1", "sels"], ["Rb"], out=Rb[:].rearrange("p c e -> p (c e)"), in0=stt[:, 1, :, :].rearrange("p c e -> p (c e)"), scalar=sels[:, 3:4], in1=Rb[:].rearrange("p c e -> p (c e)"), op0=ALU.mult, op1=ALU.add)

    cdc = P.sb("cdc", [128, 6])
    P.op("act", "activation", ["lgc"], ["cdc"], out=cdc[:], in_=lgc[:], func=AF.Exp, scale=128.0)

    def state_update(Rm, KZ, V, vtok, dirn, it):
        for h in range(6):
            c, j = h // 2, h % 2
            P.op("pe", "matmul", ["kz", vtok], ["pmi_s"], out=pmi[j * 64:(j + 1) * 64, c * 64:(c + 1) * 64],
                 lhsT=KZ[:, h * 64:(h + 1) * 64], rhs=V[:, h * 64:(h + 1) * 64], start=True, stop=True)
        rtok = "Rf" if dirn == 0 else "Rb"
        P.op("dve", "tensor_tensor", [rtok, "cdc"], ["Rtmp"], out=Rtmp[:], in0=Rm[:],
             in1=cdc[:, dirn * 3:dirn * 3 + 3].unsqueeze(2).to_broadcast([128, 3, 64]), op=ALU.mult)
        P.op("dve", "tensor_tensor", ["Rtmp", "pmi_s"], [rtok], out=Rm[:], in0=Rtmp[:],
             in1=pmi[:, 0:192].rearrange("p (c e) -> p c e", c=3), op=ALU.add)

    order = list(range(NT - 1, -1, -1)) + list(range(NTT - 1, NT - 1, -1))
    for i, it in enumerate(order if stages & 8 else []):
        b = i % 2
        if it == NTT - 1 and NC > 0:
            P.op("dve", "memset", [], ["Rb"], ap=Rb[:], constant=0.0)
        P.op("act", "copy", ["Rb"], ["Rbs%d" % it], out=Rbs[:, it, :, :], in_=Rb[:])
        last = (it == 0) or (it == NT)
        if last:
            continue
        ld(krm2[b][:], kr[:, it, :], "krm2%d" % b); ld(vrm2[b][:], vr[:, it, :], "vrm2%d" % b)
        P.op("dve", "tensor_tensor", ["krm2%d" % b, "ZB"], ["kz"], out=kz[:].rearrange("p (h e) -> p h e", h=6),
             in0=krm2[b][:].rearrange("p (h e) -> p h e", h=6), in1=ZB[:].unsqueeze(2).to_broadcast([128, 6, 64]), op=ALU.mult)
        state_update(Rb, kz, vrm2[b], "vrm2%d" % b, 1, it)

    for it in range(NTT):
        b = it % 2
        is_ctx = it >= NT
        t0 = it * 128
        S = lambda n: "%s%d" % (n, b)
        ld(xt[b][:], x[t0:t0 + 128, :], S("xt"))
        ld(qTt[b][:], qT[:, it], S("qTt"))
        special = (not is_ctx) and (it < 2 or it >= NT - 2)
        nwb = 7 if special else 5
        ext0 = (0 if it < 2 else NT - 3) if special else it
        if not is_ctx:
            ld(kwt[b][:, :, 0:nwb * 128], kx[:, :, ext0 * 128:(ext0 + nwb) * 128], S("kwt")); ld(vwt[b][:, 0:nwb], vx[:, ext0:ext0 + nwb], S("vwt"))
        ld(qrt[b][:], qrT[:, it], S("qrt")); ld(krt[b][:], krT[:, it], S("krt"))
        ld(krm[b][:], kr[:, it, :], S("krm")); ld(vrm[b][:], vr[:, it, :], S("vrm")); ld(grm[b][:], gr[:, it, :], S("grm"))
        if is_ctx:
            ld(ppd[b][:], pxc[:, :, (it - NT) * 128:(it - NT) * 128 + 144], S("ppd"))
        else:
            ld(ppd[b][:], px[:, :, it * 128:it * 128 + 144], S("ppd"))
        if stages & 1:
            if is_ctx:
                slot = None
            elif it == 0:
                slot = 0
            elif it == 1:
                slot = 1
            elif it == NT - 2:
                slot = 3
            elif it == NT - 1:
                slot = 4
            else:
                slot = 2
            for h in range(6):
                c, j = h // 2, h % 2
                pr = slice(j * 64, (j + 1) * 64)
                blocks = []
                if not is_ctx:
                    for bl in range(nwb):
                        blocks.append((kwt[b][pr, c, bl * 128:(bl + 1) * 128], vwt[b][:, bl, h, :], [S("kwt")], [S("vwt")]))
                for bl in range(2):
                    blocks.append((kTc_s[pr, c, bl * 128:(bl + 1) * 128], vaugc_s[:, bl, h, :], ["kTc_s"], ["vaugc_s"]))
                nb = len(blocks)
                for g0 in range(0, nb, 8):
                    g1 = min(nb, g0 + 8)
                    for bi in range(g0, g1):
                        kap, vap, kt_, vt_ = blocks[bi]
                        P.op("pe", "matmul", kt_ + [S("qTt")], ["pS"], out=pS[:, bi - g0, :], lhsT=kap, rhs=qTt[b][pr, c, :], start=True, stop=True)
                    P.op("act", "activation", ["pS"], ["pexp"], out=pexp[:, g0:g1, :], in_=pS[:, 0:g1 - g0, :], func=AF.Exp, scale=0.125)
                if not is_ctx:
                    P.op("dve", "tensor_tensor", ["pexp", "EB"], ["pexp"], out=pexp[:, 0:nwb, :], in0=pexp[:, 0:nwb, :],
                         in1=EB[:, slot, h, 0:nwb * 128].rearrange("p (k q) -> p k q", k=nwb), op=ALU.mult)
                for bi, (kap, vap, kt_, vt_) in enumerate(blocks):
                    P.op("pe", "matmul", vt_ + ["pexp"], ["po"], out=po[:, h, :], lhsT=pexp[:, bi, :], rhs=vap, start=(bi == 0), stop=(bi == nb - 1))
            P.op("dve", "reciprocal", ["po"], ["rcp"], out=rcp[:], in_=po[:, :, 64])
            P.op("dve", "tensor_tensor", ["po", "rcp"], ["mixtok_a"], out=mixtok[:, 0:384].rearrange("p (h e) -> p h e", h=6),
                 in0=po[:, :, 0:64], in1=rcp[:].unsqueeze(2).to_broadcast([128, 6, 64]), op=ALU.mult)
        if stages & 2:
            if it == NT and NC > 0:
                P.op("dve", "memset", [], ["Rf"], ap=Rf[:], constant=0.0)
            P.op("act", "copy", ["Rf"], ["Rfb"], out=Rfb[:], in_=Rf[:])
            for h in range(6):
                c, j = h // 2, h % 2
                pr = slice(j * 64, (j + 1) * 64)
                P.op("pe", "matmul", [S("krt"), S("qrt")], ["pS"], out=pS[:, j * 4 + c, :], lhsT=krt[b][pr, c, :], rhs=qrt[b][pr, c, :], start=True, stop=True)
            for j_ in range(2):
                P.op("dve", "tensor_tensor", ["pS"] + DT, ["SDf"], out=SDf[:, j_ * 3:j_ * 3 + 3, :], in0=pS[:, j_ * 4:j_ * 4 + 3, :], in1=DTf[:, j_ * 3:j_ * 3 + 3, :], op=ALU.mult)
                P.op("dve", "tensor_tensor", ["pS"] + DT, ["SDb"], out=SDb[:, j_ * 3:j_ * 3 + 3, :], in0=pS[:, j_ * 4:j_ * 4 + 3, :], in1=DTb[:, j_ * 3:j_ * 3 + 3, :], op=ALU.mult)
            P.op("dve", "tensor_tensor", [S("qrt"), "XI"], ["qxf"], out=qxf[:], in0=qrt[b][:], in1=XIf[:], op=ALU.mult)
            P.op("dve", "tensor_tensor", [S("qrt"), "XI"], ["qxb"], out=qxb[:], in0=qrt[b][:], in1=XIb[:], op=ALU.mult)
            for h in range(6):
                c, j = h // 2, h % 2
                pr = slice(j * 64, (j + 1) * 64)
                vh = vrm[b][:, h * 64:(h + 1) * 64]
                P.op("pe", "matmul", ["SDf", S("vrm")], ["py"], out=py[:, h, :], lhsT=SDf[:, sidx(h), :], rhs=vh, start=True, stop=False)
                P.op("pe", "matmul", ["SDb", S("vrm")], ["py"], out=py[:, h, :], lhsT=SDb[:, sidx(h), :], rhs=vh, start=False, stop=False)
                P.op("pe", "matmul", ["qxf", "Rfb"], ["py"], out=py[:, h, :], lhsT=qxf[pr, c, :], rhs=Rfb[pr, c, :], start=False, stop=False)
                P.op("pe", "matmul", ["qxb", "Rbs%d" % it], ["py"], out=py[:, h, :], lhsT=qxb[pr, c, :], rhs=Rbs[pr, it, c, :], start=False, stop=True)
            if it != NT - 1 and it != NTT - 1:
                P.op("dve", "tensor_tensor", [S("krm"), "ZF"], ["kz"], out=kz[:].rearrange("p (h e) -> p h e", h=6),
                     in0=krm[b][:].rearrange("p (h e) -> p h e", h=6), in1=ZF[:].unsqueeze(2).to_broadcast([128, 6, 64]), op=ALU.mult)
                state_update(Rf, kz, vrm[b], S("vrm"), 0, it)
            y3 = py[:, :, :]
            P.op("dve", "tensor_reduce", ["py"], ["ysum"], out=ysum[:], in_=y3, axis=AX.X, op=ALU.add)
            P.op("dve", "tensor_scalar", ["ysum"], ["ysum"], out=ysum[:], in0=ysum[:], scalar1=1.0 / 64, scalar2=None, op0=ALU.mult)
            P.op("dve", "tensor_tensor", ["py", "ysum"], ["yd"], out=yd[:].rearrange("p (h e) -> p h e", h=6), in0=y3,
                 in1=ysum[:].unsqueeze(2).to_broadcast([128, 6, 64]), op=ALU.subtract)
            P.op("dve", "tensor_tensor", ["yd"], ["yq"], out=yq[:], in0=yd[:], in1=yd[:], op=ALU.mult)
            P.op("dve", "tensor_reduce", ["yq"], ["yv"], out=yv[:], in_=yq[:].rearrange("p (h e) -> p h e", h=6), axis=AX.X, op=ALU.add)
            P.op("act", "activation", ["yv"], ["yv"], out=yv[:], in_=yv[:], func=AF.Sqrt, scale=1.0 / 64, bias=1e-6)
            P.op("dve", "reciprocal", ["yv"], ["yv"], out=yv[:], in_=yv[:])
            P.op("dve", "tensor_tensor", ["yd", "yv"], ["yd"], out=yd[:].rearrange("p (h e) -> p h e", h=6),
                 in0=yd[:].rearrange("p (h e) -> p h e", h=6), in1=yv[:].unsqueeze(2).to_broadcast([128, 6, 64]), op=ALU.mult)
            P.op("dve", "tensor_tensor", ["yd", "gnwb"], ["yd"], out=yd[:], in0=yd[:], in1=gnwb[:], op=ALU.mult)
            P.op("act", "activation", [S("grm")], ["sg"], out=sg[:], in_=grm[b][:], func=AF.Silu)
            P.op("dve", "tensor_tensor", ["yd", "sg"], ["mixtok_b"], out=mixtok[:, 384:768], in0=yd[:], in1=sg[:], op=ALU.mult)
        if stages & 4:
            pp = ppd[b]
            P.op("dve", "tensor_tensor", [S("ppd")], ["a2"], out=a2[:], in0=pp[:, :, 0:143], in1=pp[:, :, 1:144], op=ALU.add)
            P.op("dve", "tensor_tensor", ["a2"], ["a4"], out=a4[:], in0=a2[:, :, 0:141], in1=a2[:, :, 2:143], op=ALU.add)
            P.op("dve", "tensor_tensor", ["a4"], ["a8"], out=a8[:], in0=a4[:, :, 0:137], in1=a4[:, :, 4:141], op=ALU.add)
            P.op("dve", "tensor_tensor", ["a8"], ["a16"], out=a16[:], in0=a8[:, :, 0:128], in1=a8[:, :, 8:136], op=ALU.add)
            if is_ctx:
                isl = 3 + (it - NT)
            elif it == 0:
                isl = 0
            elif it == NT - 1:
                isl = 2
            else:
                isl = 1
            srcs = [(slice(0, 64), 0, a2[0:64, 0, 7:135]), (slice(64, 128), 0, a4[64:128, 0, 6:134]),
                    (slice(0, 64), 1, a8[0:64, 1, 4:132]), (slice(64, 128), 1, a16[64:128, 1, :])]
            for pr, c, wap in srcs:
                P.op("dve", "tensor_tensor", ["a2", "a4", "a8", "a16", "invcs"], ["pdm"], out=pdm[pr, c, :], in0=wap,
                     in1=invcs[pr, isl, c, :], op=ALU.mult)
                P.op("dve", "tensor_tensor", ["pdm", S("ppd")], ["pdf"], out=pdf[pr, c, :], in0=pdm[pr, c, :],
                     in1=pp[pr, c, 8:136], op=ALU.subtract)
            for c in range(2):
                P.op("pe", "matmul", ["pdf", "wpb"], ["pmi_p"], out=pmi[:, 256 + c * 128:256 + (c + 1) * 128], lhsT=wpb[:, c, :], rhs=pdf[:, c, :], start=True, stop=True)
                P.op("act", "activation", ["pmi_p", "psc"], ["mixT_p"], out=mixT[:, 6 + c, :], in_=pmi[:, 256 + c * 128:256 + (c + 1) * 128],
                     func=AF.Identity, scale=psc[:, c:c + 1])
        for c in range(6):
            P.op("pe", "transpose", ["mixtok_a", "mixtok_b", "identb"], ["ptr"], out=ptr[:, c, :], in_=mixtok[:, c * 128:(c + 1) * 128], identity=identb[:])
        P.op("act", "copy", ["ptr"], ["mixT_t"], out=mixT[:, 0:6, :], in_=ptr[:, 0:6, :])
        for hf in range(2):
            for c in range(8):
                P.op("pe", "matmul", ["mixT_t", "mixT_p"] + WO, ["pout"], out=pout[:, hf, :], lhsT=mixT[:, c, :], rhs=wo[:, c, hf * 512:(hf + 1) * 512],
                     start=(c == 0), stop=(c == 7))
        gg = g1c if is_ctx else g1x
        P.op("dve", "tensor_tensor", ["pout", "g1x", "g1c"], ["otmp"], out=otmp[:].rearrange("p (a n) -> p a n", a=2), in0=pout[:],
             in1=gg[:].rearrange("p (a n) -> p a n", a=2), op=ALU.mult)
        P.op("dve", "tensor_tensor", ["otmp", S("xt")], ["otmp"], out=otmp[:], in0=otmp[:], in1=xt[b][:], op=ALU.add)
        P.dop("sp", reads=["otmp"], out=x1[t0:t0 + 128, :], in_=otmp[:])
    P.end_phase()


def phase_B2(P, IO, NT, NC, final):
    NTT = NT + NC
    P.begin_phase()
    x1 = IO["x1_src"]; mod = IO["d_mod"]; n2w = IO["n2w"]; fnw = IO["fnw"]; wq = IO["wq"]; keysT = IO["keysT"]
    pu = IO["pu"]; pv = IO["pv"]; iota16 = IO["iota16"]; ident_d = IO["ident"]; x2 = IO["x2_dst"]

    identf = P.sb("identf", [128, 128]); identb = P.sb("identb", [128, 128], BF16)
    wqs = [P.sb("wqs%d" % i, [128, 1024]) for i in range(2)]
    wqb = P.sb("wqb", [128, 8, 2048], BF16)
    kTf = P.sb("kTf", [128, 16, 128]); kTb = P.sb("kTb", [128, 16, 128], BF16)
    io16 = P.sb("io16", [128, 16])
    w2x = P.sb("w2x", [128, D]); sh2x = P.sb("sh2x", [128, D]); g2x = P.sb("g2x", [128, D])
    if NC > 0:
        w2c = P.sb("w2c", [128, D]); sh2c = P.sb("sh2c", [128, D]); g2c = P.sb("g2c", [128, D])
    n2b = P.sb("n2b", [128, D])
    if final:
        fnb = P.sb("fnb", [128, D])

    xt = [P.sb("xt%d" % i, [128, D]) for i in range(2)]
    junk = P.sb("junk", [128, D]); ssq = P.sb("ssq", [128, 1]); rstd = P.sb("rstd", [128, 1])
    h2 = P.sb("h2", [128, D]); h2b2 = [P.sb("h2b%d" % i, [128, D], BF16) for i in range(2)]
    h2T = P.sb("h2T", [128, 8, 128], BF16)
    qTs = P.sb("qTs", [128, 16, 128], BF16)
    ssb = P.sb("ssb", [128, 16, 128]); wk = P.sb("wk", [128, 16, 128])
    va = P.sb("va", [128, 16, 16]); ia = P.sb("ia", [128, 16, 16], U32); iaf = P.sb("iaf", [128, 16, 16])
    cand = P.sb("cand", [128, 8, 256])
    sc = P.sb("sc", [128, 8, 16]); ci = P.sb("ci", [128, 8, 16], U32)
    rk = P.sb("rk", [128, 8, 16], U32); ck = P.sb("ck", [128, 8, 16], U32)
    rkf = P.sb("rkf", [128, 8, 16]); ckf = P.sb("ckf", [128, 8, 16])
    oh = P.sb("oh", [128, 8, 16, 16])
    iak = P.sb("iak", [128, 8, 16]); ibk = P.sb("ibk", [128, 8, 16])
    idxf = P.sb("idxf", [128, 128]); idx2 = [P.sb("idx%d" % i, [128, 128], I32) for i in range(2)]
    ex = P.sb("ex", [128, 8, 16]); zs = P.sb("zs", [128, 8]); gate2 = [P.sb("gate%d" % i, [128, 128]) for i in range(2)]
    aa = P.sb("aa", [128, 128]); coef = P.sb("coef", [128, 128])
    ring = [P.sb("ring%d" % i, [128, D], BF16) for i in range(NRING)]
    otmp = P.sb("otmp", [128, D]); dg = [P.sb("dg%d" % i, [128, 128], BF16) for i in range(4)]
    xo = [P.sb("xo%d" % i, [128, D]) for i in range(2)]

    pT = P.ps("pT", [128, 8, 128], BF16)
    pq = P.ps("pq", [128, 16, 128])
    pacc = P.ps("pacc", [128, 2, 512])

    ld = lambda out, in_, w, r=(): P.dop("sp", reads=list(r), writes=[w], out=out, in_=in_)
    ld(identf[:], ident_d, "identf")
    P.op("dve", "tensor_copy", ["identf"], ["identb"], out=identb[:], in_=identf[:])
    ld(kTf[:], keysT, "kTf"); P.op("dve", "tensor_copy", ["kTf"], ["kTb"], out=kTb[:], in_=kTf[:])
    ld(io16[:], iota16, "io16")
    ld(n2b[:], n2w.partition_broadcast(128), "n2b")
    rows = [(0, w2x, sh2x, g2x, "x")]
    if NC > 0:
        rows.append((1, w2c, sh2c, g2c, "c"))
    for r, w2_, sh2_, g2_, nm in rows:
        ld(sh2_[:], mod[r:r + 1, 3 * D:4 * D].partition_broadcast(128), "sh2" + nm)
        ld(w2_[:], mod[r:r + 1, 4 * D:5 * D].partition_broadcast(128), "w2" + nm)
        ld(g2_[:], mod[r:r + 1, 5 * D:6 * D].partition_broadcast(128), "g2" + nm)
        P.op("dve", "scalar_tensor_tensor", ["w2" + nm, "n2b"], ["w2" + nm], out=w2_[:], in0=w2_[:], scalar=1.0, in1=n2b[:], op0=ALU.add, op1=ALU.mult)
    if final:
        ld(fnb[:], fnw.partition_broadcast(128), "fnb")
    for c in range(8):
        for hf in range(2):
            ld(wqs[hf][:], wq[c * 128:(c + 1) * 128, hf * 1024:(hf + 1) * 1024], "wqs%d" % hf)
            P.op("dve", "tensor_copy", ["wqs%d" % hf], ["wqb%d" % c], out=wqb[:, c, hf * 1024:(hf + 1) * 1024], in_=wqs[hf][:])
    WQ = ["wqb%d" % c for c in range(8)]

    def top16(vals_tok, vals, work, outv, outi, wtok):
        P.op("dve", "max", [vals_tok], [wtok + "v"], out=outv[:, 0:8], in_=vals)
        P.op("dve", "max_index", [vals_tok, wtok + "v"], [wtok + "i"], out=outi[:, 0:8], in_max=outv[:, 0:8], in_values=vals)
        P.op("dve", "match_replace", [vals_tok, wtok + "v"], [wtok + "w"], out=work, in_to_replace=outv[:, 0:8], in_values=vals, imm_value=-1e30)
        P.op("dve", "max", [wtok + "w"], [wtok + "v"], out=outv[:, 8:16], in_=work)
        P.op("dve", "max_index", [wtok + "w", wtok + "v"], [wtok + "i"], out=outi[:, 8:16], in_max=outv[:, 8:16], in_values=work)

    ring_i = 0
    def prologue1(it):
            b = it % 2
            is_ctx = it >= NT
            t0 = it * 128
            S = lambda n: "%s%d" % (n, b)
            w2_, sh2_, g2_, nm = (w2c, sh2c, g2c, "c") if is_ctx else (w2x, sh2x, g2x, "x")
            ld(xt[b][:], x1[t0:t0 + 128, :], S("xt"))
            P.op("act", "activation", [S("xt")], ["junk", "ssq"], out=junk[:], in_=xt[b][:], func=AF.Square, accum_out=ssq[:])
            P.op("act", "activation", ["ssq"], ["rstd"], out=rstd[:], in_=ssq[:], func=AF.Sqrt, scale=1.0 / D, bias=1e-6)
            P.op("dve", "reciprocal", ["rstd"], ["rstd"], out=rstd[:], in_=rstd[:])
            P.op("dve", "scalar_tensor_tensor", [S("xt"), "rstd", "w2" + nm], ["h2"], out=h2[:], in0=xt[b][:], scalar=rstd[:, 0:1], in1=w2_[:], op0=ALU.mult, op1=ALU.mult)
            P.op("dve", "tensor_tensor", ["h2", "sh2" + nm], ["h2"], out=h2[:], in0=h2[:], in1=sh2_[:], op=ALU.add)
            P.op("act", "copy", ["h2"], [S("h2b")], out=h2b2[b][:], in_=h2[:])
            for c in range(8):
                P.op("pe", "transpose", [S("h2b"), "identb"], ["pT"], out=pT[:, c, :], in_=h2b2[b][:, c * 128:(c + 1) * 128], identity=identb[:])
            P.op("act", "copy", ["pT"], ["h2T"], out=h2T[:], in_=pT[:])
            for n in range(16):
                for c in range(8):
                    P.op("pe", "matmul", ["h2T"] + WQ, ["pq"], out=pq[:, n, :], lhsT=wqb[:, c, n * 128:(n + 1) * 128], rhs=h2T[:, c, :], start=(c == 0), stop=(c == 7))
            P.op("act", "copy", ["pq"], ["qTs"], out=qTs[:], in_=pq[:])
            for n in range(16):
                P.op("pe", "matmul", ["qTs", "kTb"], ["pq"], out=pq[:, n, :], lhsT=qTs[:, n, :], rhs=kTb[:, n, :], start=True, stop=True)
            P.op("act", "copy", ["pq"], ["ssb"], out=ssb[:], in_=pq[:])

    def prologue2(it):
            b = it % 2
            is_ctx = it >= NT
            t0 = it * 128
            S = lambda n: "%s%d" % (n, b)
            w2_, sh2_, g2_, nm = (w2c, sh2c, g2c, "c") if is_ctx else (w2x, sh2x, g2x, "x")
            for g in range(16):
                top16("ssb", ssb[:, g, :], wk[:, g, :], va[:, g, :], ia[:, g, :], "t%d" % g)
            TV = ["t%dv" % g for g in range(16)]
            TI = ["t%di" % g for g in range(16)]
            va4 = va[:].rearrange("p (h s) r -> p h s r", s=2)
            P.op("dve", "tensor_tensor", TV + TI, ["cand"], out=cand[:].rearrange("p h (r c) -> p h r c", r=16),
                 in0=va4[:, :, 0, :].unsqueeze(3).to_broadcast([128, 8, 16, 16]),
                 in1=va4[:, :, 1, :].unsqueeze(2).to_broadcast([128, 8, 16, 16]), op=ALU.add)
            for h in range(8):
                top16("cand", cand[:, h, :], wk[:, 2 * h:2 * h + 2, :].rearrange("p a n -> p (a n)"), sc[:, h, :], ci[:, h, :], "c%d" % h)
            CV = ["c%dv" % h for h in range(8)]
            CI = ["c%di" % h for h in range(8)]
            P.op("dve", "tensor_single_scalar", CI, ["rk"], out=rk[:], in_=ci[:], scalar=4, op=ALU.logical_shift_right)
            P.op("dve", "tensor_single_scalar", CI, ["ck"], out=ck[:], in_=ci[:], scalar=15, op=ALU.bitwise_and)
            P.op("dve", "tensor_copy", ["rk"], ["rkf"], out=rkf[:], in_=rk[:])
            P.op("dve", "tensor_copy", ["ck"], ["ckf"], out=ckf[:], in_=ck[:])
            P.op("dve", "tensor_copy", TI, ["iaf"], out=iaf[:], in_=ia[:])
            iaf4 = iaf[:].rearrange("p (h s) r -> p h s r", s=2)
            io_b = io16[:].unsqueeze(1).unsqueeze(1).to_broadcast([128, 8, 16, 16])
            for side, (kf, ktok, dst, dtok) in enumerate([(rkf, "rkf", iak, "iak"), (ckf, "ckf", ibk, "ibk")]):
                P.op("dve", "tensor_tensor", [ktok, "io16"], ["oh"], out=oh[:], in0=io_b,
                     in1=kf[:].unsqueeze(3).to_broadcast([128, 8, 16, 16]), op=ALU.is_equal)
                P.op("dve", "tensor_tensor", ["oh", "iaf"], ["oh"], out=oh[:], in0=oh[:],
                     in1=iaf4[:, :, side, :].unsqueeze(2).to_broadcast([128, 8, 16, 16]), op=ALU.mult)
                P.op("dve", "tensor_reduce", ["oh"], [dtok], out=dst[:], in_=oh[:], axis=AX.X, op=ALU.add)
            P.op("dve", "scalar_tensor_tensor", ["iak", "ibk"], ["idxf"], out=idxf[:], in0=iak[:].rearrange("p h k -> p (h k)"), scalar=128.0,
                 in1=ibk[:].rearrange("p h k -> p (h k)"), op0=ALU.mult, op1=ALU.add)
            P.op("dve", "tensor_copy", ["idxf"], [S("idx")], out=idx2[b][:], in_=idxf[:])
            P.op("dve", "tensor_tensor", CV, ["ex"], out=ex[:], in0=sc[:], in1=sc[:, :, 0:1].to_broadcast([128, 8, 16]), op=ALU.subtract)
            P.op("act", "activation", ["ex"], ["ex"], out=ex[:], in_=ex[:], func=AF.Exp)
            P.op("dve", "tensor_reduce", ["ex"], ["zs"], out=zs[:], in_=ex[:], axis=AX.X, op=ALU.add)
            P.op("dve", "reciprocal", ["zs"], ["zs"], out=zs[:], in_=zs[:])
            P.op("dve", "tensor_tensor", ["ex", "zs"], [S("gate")], out=gate2[b][:].rearrange("p (h k) -> p h k", h=8), in0=ex[:],
                 in1=zs[:].unsqueeze(2).to_broadcast([128, 8, 16]), op=ALU.mult)


    def uphase(it):
            nonlocal ring_i
            b = it % 2
            is_ctx = it >= NT
            t0 = it * 128
            S = lambda n: "%s%d" % (n, b)
            w2_, sh2_, g2_, nm = (w2c, sh2c, g2c, "c") if is_ctx else (w2x, sh2x, g2x, "x")
            for kk in range(128):
                rg = ring[ring_i % NRING]; rtok = "ring%d" % (ring_i % NRING); ring_i += 1
                P.dop("pool", reads=[S("idx")], writes=[rtok], method="indirect_dma_start", out=rg[:], out_offset=None, in_=pu,
                      in_offset=bass.IndirectOffsetOnAxis(ap=idx2[b][:, kk:kk + 1], axis=0))
                P.op("dve", "tensor_tensor", [rtok, S("h2b")], [rtok], out=rg[:], in0=rg[:], in1=h2b2[b][:], op=ALU.mult)
                P.op("act", "activation", [rtok], [rtok, "aa%d" % kk], out=rg[:], in_=rg[:], func=AF.Identity, accum_out=aa[:, kk:kk + 1])
            P.op("act", "activation", ["aa%d" % kk for kk in range(128)], ["coef"], out=coef[:], in_=aa[:], func=AF.Gelu)
            P.op("dve", "tensor_tensor", ["coef", S("gate")], ["coef"], out=coef[:], in0=coef[:], in1=gate2[b][:], op=ALU.mult)

    def vphase(it):
            nonlocal ring_i
            b = it % 2
            is_ctx = it >= NT
            t0 = it * 128
            S = lambda n: "%s%d" % (n, b)
            w2_, sh2_, g2_, nm = (w2c, sh2c, g2c, "c") if is_ctx else (w2x, sh2x, g2x, "x")
            for kk in range(128):
                rg = ring[ring_i % NRING]; rtok = "ring%d" % (ring_i % NRING); ring_i += 1
                P.dop("pool", reads=[S("idx")], writes=[rtok], method="indirect_dma_start", out=rg[:], out_offset=None, in_=pv,
                      in_offset=bass.IndirectOffsetOnAxis(ap=idx2[b][:, kk:kk + 1], axis=0))
                dgi = kk % 4
                P.op("act", "activation", ["coef", "identb"], ["dg%d" % dgi], out=dg[dgi][:], in_=identb[:], func=AF.Identity, scale=coef[:, kk:kk + 1])
                for hf in range(2):
                    P.op("pe", "matmul", ["dg%d" % dgi, rtok], ["pacc"], out=pacc[:, hf, :], lhsT=dg[dgi][:], rhs=rg[:, hf * 512:(hf + 1) * 512],
                         start=(kk == 0), stop=(kk == 127))

    def epilogue(it):
            b = it % 2
            is_ctx = it >= NT
            t0 = it * 128
            S = lambda n: "%s%d" % (n, b)
            w2_, sh2_, g2_, nm = (w2c, sh2c, g2c, "c") if is_ctx else (w2x, sh2x, g2x, "x")
            P.op("dve", "tensor_tensor", ["pacc", "g2" + nm], ["otmp"], out=otmp[:].rearrange("p (a n) -> p a n", a=2), in0=pacc[:],
                 in1=g2_[:].rearrange("p (a n) -> p a n", a=2), op=ALU.mult)
            P.op("dve", "tensor_tensor", ["otmp", S("xt")], [S("xo")], out=xo[b][:], in0=otmp[:], in1=xt[b][:], op=ALU.add)
            if final and not is_ctx:
                P.op("act", "activation", [S("xo")], ["junk", "ssq"], out=junk[:], in_=xo[b][:], func=AF.Square, accum_out=ssq[:])
                P.op("act", "activation", ["ssq"], ["rstd"], out=rstd[:], in_=ssq[:], func=AF.Sqrt, scale=1.0 / D, bias=1e-6)
                P.op("dve", "reciprocal", ["rstd"], ["rstd"], out=rstd[:], in_=rstd[:])
                P.op("dve", "scalar_tensor_tensor", [S("xo"), "rstd", "fnb"], [S("xo")], out=xo[b][:], in0=xo[b][:], scalar=rstd[:, 0:1], in1=fnb[:],
                     op0=ALU.mult, op1=ALU.mult)
            P.dop("sp", reads=[S("xo")], out=x2[t0:t0 + 128, :], in_=xo[b][:])


    prologue1(0)
    prologue2(0)
    for it in range(NTT):
        uphase(it)
        if it + 1 < NTT:
            prologue1(it + 1)
        vphase(it)
        if it + 1 < NTT:
            prologue2(it + 1)
        epilogue(it)
    P.end_phase()


import numpy as np
import ml_dtypes

BF = ml_dtypes.bfloat16
D = 1024
GRID_W = 64
POOL_W = (2, 4, 8, 16)


def rope_tables(S):
    t = np.arange(S)
    row = (t // GRID_W).astype(np.float32)
    col = (t % GRID_W).astype(np.float32)
    inv = (10000.0 ** (-np.arange(16, dtype=np.float32) / 16)).astype(np.float32)
    ang = np.concatenate([row[:, None] * inv, col[:, None] * inv], axis=-1)
    return np.cos(ang).astype(np.float32), np.sin(ang).astype(np.float32)


def na_slot_tables(rpb, half, NT):
    NB = 2 * NT
    rows = 2 * NB
    locs = [0, 1, 2, NT - 2, NT - 1]
    G = np.zeros((128, 5, 6, 640), np.float32)
    M = np.zeros((128, 5, 640), np.float32)
    k = np.arange(128)[:, None, None]
    blk = np.arange(5)[None, :, None]
    q = np.arange(128)[None, None, :]
    for s, ml in enumerate(locs):
        m = half * NT + ml
        bs = int(np.clip(m - 2, 0, NB - 5))
        kr = 2 * (bs + blk) + k // 64
        ck = k % 64
        qr = 2 * m + q // 64
        cq = q % 64
        r_start = np.clip(qr - 4, 0, rows - 8)
        valid_r = (kr >= r_start) & (kr < r_start + 8)
        roff = np.clip(kr - qr + 7, 0, 14)
        c_start = np.clip(cq - 8, 0, GRID_W - 16)
        valid_c = (ck >= c_start) & (ck < c_start + 16)
        coff = np.clip(ck - cq + 15, 0, 30)
        valid = np.broadcast_to(valid_r & valid_c, (128, 5, 128))
        roff_b = np.broadcast_to(roff, (128, 5, 128))
        coff_b = np.broadcast_to(coff, (128, 5, 128))
        M[:, s, :] = valid.reshape(128, 640)
        for h in range(6):
            g = rpb[h][roff_b, coff_b]
            G[:, s, h, :] = np.where(valid, g, np.float32(0)).reshape(128, 640)
    return G, M


def window_start(m, NT):
    return int(np.clip(m - 2, 0, 2 * NT - 5))


def ret_consts():
    m = np.arange(128)[:, None]
    n = np.arange(128)[None, :]
    cst = np.zeros((128, 6, 128), np.float32)
    cst[:, 0] = np.maximum(n - m, 0)
    cst[:, 1] = 0.125 * (n >= m)
    cst[:, 2] = np.maximum(m - n, 0)
    cst[:, 3] = 0.125 * (m >= n)
    cst[:, 4] = np.broadcast_to(n + 1, (128, 128))
    cst[:, 5] = np.broadcast_to(128 - n, (128, 128))
    pm = np.stack([127 - np.arange(128), np.arange(128)], 1).astype(np.float32)
    return cst, pm


def inv_counts(half, NT):
    S = 2 * NT * 128
    out = np.zeros((128, 5, 2, 128), np.float32)
    specs = [(half * NT * 128, S), (half * NT * 128 + 128, S), (half * NT * 128 + (NT - 1) * 128, S), (0, 256), (128, 256)]
    for s, (tstart, Tseq) in enumerate(specs):
        t = tstart + np.arange(128)
        for c in range(2):
            for gi in range(2):
                w = POOL_W[2 * c + gi]
                lo = np.clip(t - w // 2, 0, Tseq)
                hi = np.clip(t + w // 2, 0, Tseq)
                out[gi * 64:(gi + 1) * 64, s, c, :] = (1.0 / (hi - lo).astype(np.float32))[None, :]
    return out


def pair_pack(st):
    a = st.reshape(64, 4, 3, 2, 64)
    return np.ascontiguousarray(a.transpose(3, 0, 1, 2, 4).reshape(128, 4, 3, 64))


def phase_C(P, tables):
    P.begin_phase()
    stg = [P.sb("cstg%d" % i, [128, 4096]) for i in range(4)]
    stb = [P.sb("cstb%d" % i, [128, 4096], BF16) for i in range(4)]
    n = 0
    for src, dst in tables:
        sv = src.rearrange("(p j) d -> p (j d)", p=128)
        dv = dst.rearrange("(p j) d -> p (j d)", p=128)
        for ch in range(32):
            i = n % 4
            n += 1
            P.dop("sp", writes=["cstg%d" % i], out=stg[i][:], in_=sv[:, ch * 4096:(ch + 1) * 4096])
            if i % 2 == 0:
                P.op("dve", "tensor_copy", ["cstg%d" % i], ["cstb%d" % i], out=stb[i][:], in_=stg[i][:])
            else:
                P.op("act", "copy", ["cstg%d" % i], ["cstb%d" % i], out=stb[i][:], in_=stg[i][:])
            P.dop("sp", reads=["cstb%d" % i], out=dv[:, ch * 4096:(ch + 1) * 4096], in_=stb[i][:])
    P.end_phase()


def build_fused(NT, ncores=8):
    NC = 2
    NTT = NT + NC
    P = Prog()
    di = lambda n, s, dt=F32: P.dram(n, s, dt, "ExternalInput")
    xcat = di("xcat", [NTT * 128, D]); cvT = di("cvT", [128, 8, 2])
    w_ada = di("w_ada", [2, D, 6 * D]); b_ada = di("b_ada", [2, 1, 6 * D]); n1w = di("n1w", [2, 8, 128]); w_in = di("w_in", [2, D, DPROJ])
    ropec = di("ropec", [NTT * 128, 32]); ropes = di("ropes", [NTT * 128, 32]); dec = di("dec", [2, 128, 12])
    posf = di("posf", [128, NTT]); posb = di("posb", [128, NTT]); ident = di("ident", [128, 128])
    G = di("G", [2, 128, 5, 6, 896]); M01 = di("M01", [128, 5, 896]); npow = di("npow", [128, 2]); sel = di("sel", [128, 4])
    dec_col = di("dec_col", [2, 128, 6]); cst = di("cst", [128, 6, 128]); pm = di("pm", [128, 2]); gnw = di("gnw", [2, 1, 384])
    invc = di("invc", [128, 5, 2, 128]); wpool = di("wpool", [2, 128, 2, 128]); pscale = di("pscale", [2, 128, 2]); w_out = di("w_out", [2, D, D])
    n2w = di("n2w", [2, 1, D]); fnw = di("fnw", [1, D]); wq = di("wq", [2, D, 2048]); keysT = di("keysT", [2, 128, 16, 128])
    pu = [di("pu%d" % l_, [16384, D]) for l_ in range(2)]; pv = [di("pv%d" % l_, [16384, D]) for l_ in range(2)]; iota16 = di("iota16", [128, 16])
    out = P.dram("out", [NT * 128, D], F32, "ExternalOutput")

    sc = {}
    def mk(name, shape, dt=F32):
        sc["t_" + name] = P.scratch("s_" + name, shape, dt)
        sc["d_" + name] = sc["t_" + name].ap()
    mk("mod", [2, 6 * D]); mk("qT", [128, NTT, 3, 128], BF16); mk("qrT", [128, NTT, 3, 128], BF16); mk("krT", [128, NTT, 3, 128], BF16)
    mk("kx", [128, 3, (NT + 4) * 128], BF16); mk("kTc", [128, 3, 256], BF16); mk("vx", [128, NT + 4, 6, 65], BF16); mk("vaugc", [128, 2, 6, 65], BF16)
    mk("kr", [128, NTT, 384], BF16); mk("vr", [128, NTT, 384], BF16); mk("gr", [128, NTT, 384]); mk("px", [128, 2, NT * 128 + 16]); mk("pxc", [128, 2, 272])
    mk("stpp", [128, 4, 3, 64]); mk("xb", [128, 3096], BF16); mk("xf", [128, 416]); mk("gb", [256, 3096], BF16); mk("gf", [256, 416])
    mk("x1", [NTT * 128, D]); mk("x2", [NTT * 128, D])
    for l_ in range(2):
        mk("pub%d" % l_, [16384, D], BF16); mk("pvb%d" % l_, [16384, D], BF16)
    phase_C(P, [(pu[0], sc["d_pub0"]), (pv[0], sc["d_pvb0"]), (pu[1], sc["d_pub1"]), (pv[1], sc["d_pvb1"])])

    for l in range(2):
        last = l == 1
        NCB = 0 if last else NC
        src = xcat if l == 0 else sc["d_x2"]
        IO = dict(sc)
        IO.update(x_src=src[0:NT * 128, :], ctx_src=src[NT * 128:NTT * 128, :], cvT=cvT, w_ada=w_ada[l], b_ada=b_ada[l], n1w=n1w[l], w_in=w_in[l],
                  ropec=ropec, ropes=ropes, dec=dec[l], posf=posf, posb=posb, ident=ident)
        phase_A(P, IO, NT, NC)
        IO = dict(sc); IO.update(sel=sel)
        phase_X(P, IO, NT, ncores)
        IO = dict(sc)
        IO.update(x_src=src, G=G[l], M01=M01, npow=npow, sel=sel, dec_row=dec[l], dec_col=dec_col[l], cst=cst, pm=pm, ident=ident, gnw=gnw[l],
                  invc=invc, wpool=wpool[l], pscale=pscale[l], w_out=w_out[l], x1_dst=sc["d_x1"])
        phase_B1(P, IO, NT, NCB)
        IO = dict(sc)
        IO.update(x1_src=sc["d_x1"], n2w=n2w[l], fnw=fnw, wq=wq[l], keysT=keysT[l], pu=sc["d_pub%d" % l], pv=sc["d_pvb%d" % l], iota16=iota16, ident=ident,
                  x2_dst=out if last else sc["d_x2"])
        phase_B2(P, IO, NT, NCB, last)
    P.es.close()
    return P.nc


def na_slot_tables_fused(rpb, half, NT):
    NB = 2 * NT
    rows = 2 * NB
    specs = [(0, 0, 7), (1, 0, 7), (2, 2, 5), (NT - 2, NT - 3, 7), (NT - 1, NT - 3, 7)]
    G = np.zeros((128, 5, 6, 896), np.float32)
    M = np.zeros((128, 5, 896), np.float32)
    k = np.arange(128)[:, None, None]
    q = np.arange(128)[None, None, :]
    for s, (ml, ext0, nb) in enumerate(specs):
        m = half * NT + ml
        e = np.arange(nb)[None, :, None]
        g = half * NT + ext0 + e - 2
        inseq = (g >= 0) & (g < NB)
        kr = 2 * g + k // 64
        ck = k % 64
        qr = 2 * m + q // 64
        cq = q % 64
        r_start = np.clip(qr - 4, 0, rows - 8)
        valid_r = (kr >= r_start) & (kr < r_start + 8) & inseq
        roff = np.clip(kr - qr + 7, 0, 14)
        c_start = np.clip(cq - 8, 0, GRID_W - 16)
        valid_c = (ck >= c_start) & (ck < c_start + 16)
        coff = np.clip(ck - cq + 15, 0, 30)
        valid = np.broadcast_to(valid_r & valid_c, (128, nb, 128))
        roff_b = np.broadcast_to(roff, (128, nb, 128))
        coff_b = np.broadcast_to(coff, (128, nb, 128))
        M[:, s, :nb * 128] = valid.reshape(128, nb * 128)
        for h in range(6):
            gg = rpb[h][roff_b, coff_b]
            G[:, s, h, :nb * 128] = np.where(valid, gg, np.float32(0)).reshape(128, nb * 128)
    return G, M


def fused_inputs(inp, B, NT):
    S = 2 * NT * 128
    NTT = NT + 2
    own_T = NT * 128
    ident = np.eye(128, dtype=np.float32)
    cos, sin = rope_tables(S)
    cst, pm = ret_consts()
    iota16 = np.broadcast_to(np.arange(16, dtype=np.float32), (128, 16)).copy()
    DEPTH = 2
    dec = np.stack([np.tile(np.concatenate([inp["ret_decay_fwd"][l], inp["ret_decay_bwd"][l]])[None, :], (128, 1)) for l in range(DEPTH)]).astype(np.float32)
    dec_col = np.zeros((DEPTH, 128, 6), np.float32)
    wpool = np.zeros((DEPTH, 128, 2, 128), np.float32)
    for l in range(DEPTH):
        for cc in range(3):
            for j in range(2):
                dec_col[l, j * 64:(j + 1) * 64, cc] = inp["ret_decay_fwd"][l][2 * cc + j]
                dec_col[l, j * 64:(j + 1) * 64, 3 + cc] = inp["ret_decay_bwd"][l][2 * cc + j]
        for cc in range(2):
            for gi in range(2):
                wpool[l, gi * 64:(gi + 1) * 64, cc, gi * 64:(gi + 1) * 64] = inp["pool_w"][l][2 * cc + gi]
    pscale = np.ascontiguousarray(inp["pool_scale"].reshape(DEPTH, 2, 128).transpose(0, 2, 1))
    keysT = np.ascontiguousarray(inp["peer_keys"].transpose(0, 4, 2, 1, 3).reshape(DEPTH, 128, 16, 128))
    shared = {
        "w_ada": inp["w_ada"], "b_ada": inp["b_ada"][:, None, :], "n1w": inp["norm1_w"].reshape(DEPTH, 8, 128), "w_in": inp["w_in"],
        "dec": dec, "ident": ident, "dec_col": dec_col, "cst": cst, "pm": pm, "gnw": inp["ret_gn_w"][:, None, :], "wpool": wpool,
        "pscale": pscale, "w_out": inp["w_out"], "n2w": inp["norm2_w"][:, None, :], "fnw": inp["final_norm_w"][None, :], "wq": inp["peer_wq"],
        "keysT": keysT, "pu0": inp["peer_u"][0], "pu1": inp["peer_u"][1], "pv0": inp["peer_v"][0], "pv1": inp["peer_v"][1], "iota16": iota16}
    t = np.arange(NTT * 128)
    own = t < own_T
    posf = np.where(own, own_T - 1 - t, 255 - (t - own_T)).astype(np.float32)
    posb = np.where(own, t, t - own_T).astype(np.float32)
    posf = np.ascontiguousarray(posf.reshape(NTT, 128).T); posb = np.ascontiguousarray(posb.reshape(NTT, 128).T)
    tabs = {}
    for half in range(2):
        GM = [na_slot_tables_fused(inp["na_rpb"][l], half, NT) for l in range(DEPTH)]
        tabs[half] = (np.stack([g for g, _ in GM]), GM[0][1], inv_counts(half, NT))
    maps = []
    for c in range(2 * B):
        b, half = c // 2, c % 2
        cv = np.stack([inp["c"][b], inp["c_ctx"]], 0)
        m = dict(shared)
        m["xcat"] = np.concatenate([inp["x"][b, half * own_T:(half + 1) * own_T], inp["ctx"][b]], 0)
        m["cvT"] = np.ascontiguousarray(cv.reshape(2, 8, 128).transpose(2, 1, 0))
        m["ropec"] = np.concatenate([cos[half * own_T:(half + 1) * own_T], np.ones((256, 32), np.float32)], 0)
        m["ropes"] = np.concatenate([sin[half * own_T:(half + 1) * own_T], np.zeros((256, 32), np.float32)], 0)
        m["posf"] = posf; m["posb"] = posb
        m["G"], m["M01"], m["invc"] = tabs[half]
        npow = np.zeros((128, 2), np.float32); npow[:, 0] = own_T if half == 1 else 0; npow[:, 1] = own_T if half == 0 else 0
        m["npow"] = npow
        selv = np.zeros((128, 4), np.float32)
        selv[:, 0] = half; selv[:, 1] = 1 - half; selv[:, 2] = half; selv[:, 3] = 1 - half
        m["sel"] = selv
        maps.append(m)
    return maps


from concourse.bass_utils import run_bass_kernel_spmd


def kernel(**inputs):
    inp = {k: np.asarray(v) for k, v in inputs.items()}
    B, S, _ = inp["x"].shape
    NT = S // 256
    maps = fused_inputs(inp, B, NT)
    nc = build_fused(NT, len(maps))
    res = run_bass_kernel_spmd(nc, maps, core_ids=list(range(len(maps)))).results
    out = np.zeros((B, S, D), np.float32)
    for c in range(len(maps)):
        out[c // 2, (c % 2) * NT * 128:(c % 2 + 1) * NT * 128] = np.asarray(res[c]["out"])
    return out
```

```python
D = 1024
DPROJ = 2944
NTOK_TM = 1920
NRING = 16
import math
import numpy as np
import ml_dtypes
from contextlib import ExitStack

import concourse.bass as bass
import concourse.mybir as mybir

F32 = mybir.dt.float32
BF16 = mybir.dt.bfloat16
U32 = mybir.dt.uint32
I32 = mybir.dt.int32
AF = mybir.ActivationFunctionType
ALU = mybir.AluOpType
AX = mybir.AxisListType

ENGS = ("pe", "dve", "act", "pool", "sp")
NDSEM = {"sp": 16, "pool": 20, "act": 8}


class Prog:
    def __init__(self, name="k"):
        self.nc = bass.Bass("TRN2", target_bir_lowering=False)
        self.es = ExitStack()
        self.stream = {e: [] for e in ENGS}
        self.cnt = {e: 0 for e in ENGS}
        self.sem = {}
        for e in ENGS:
            self.sem["E" + e] = self.es.enter_context(self.nc.semaphore("s_" + e))
        self.dsem_use = {}
        self.dsem_rr = {}
        for q, n in NDSEM.items():
            for j in range(n):
                key = "D%s%d" % (q, j)
                self.sem[key] = self.es.enter_context(self.nc.semaphore("d_%s%d" % (q, j)))
                self.dsem_use[key] = 0
            self.dsem_rr[q] = 0
        self.known = {e: {} for e in ENGS}
        self.targets = {"E" + e: set() for e in ENGS}
        self.pes = None
        self.nalloc = 0
        self.ncc = 0
        self.ccev = []
        self.rank = {}
        self.sigcount = {"E" + e: 0 for e in ENGS}
        self.emitted = {"E" + e: 0 for e in ENGS}
        self.last_w = {}
        self.readers = {}
        self.uid = 0

    def dram(self, name, shape, dtype, kind):
        return self.nc.dram_tensor(name, list(shape), dtype, kind=kind).ap()

    def sb(self, name, shape, dtype=F32):
        es = self.pes if self.pes is not None else self.es
        self.nalloc += 1
        return es.enter_context(self.nc.sbuf_tensor("%s_%d" % (name, self.nalloc), list(shape), dtype))

    def ps(self, name, shape, dtype=F32):
        es = self.pes if self.pes is not None else self.es
        self.nalloc += 1
        return es.enter_context(self.nc.psum_tensor("%s_%d" % (name, self.nalloc), list(shape), dtype))

    def scratch(self, name, shape, dtype=F32):
        return self.nc.dram_tensor(name, list(shape), dtype)

    def begin_phase(self):
        self.pes = ExitStack()
        self.last_w = {}
        self.readers = {}

    def cc(self, kind, groups, in_t, out_t, reads=(), writes=()):
        key = "CC%d" % self.ncc
        self.ncc += 1
        self.sem[key] = self.es.enter_context(self.nc.semaphore(key.lower()))
        deps = self._deps(reads, writes)
        waits = self._waits("pool", deps)
        fn = lambda e: e.collective_compute(kind, ALU.bypass, replica_groups=groups, ins=[in_t.ap().opt()], outs=[out_t.ap().opt()])
        self.stream["pool"].append((waits, fn, key, "cc"))
        ev = (key, 1)
        self.ccev.append(ev)
        self._commit(ev, reads, writes)
        return ev

    def end_phase(self):
        for e in ENGS:
            deps = {}
            for o in ENGS:
                if self.cnt[o] > 0 and o != "sp":
                    deps["E" + o] = self.cnt[o]
            for key, n in self.dsem_use.items():
                if n > 0:
                    deps[key] = 16 * n
            for key, v in self.ccev:
                deps[key] = v
            waits = self._waits(e, deps)
            if e == "pe" and self.cnt["pe"] > 0:
                self.known["pe"]["Epe"] = self.cnt["pe"]
            self.stream[e].append((waits, None, None, None))
        self._emit()
        if self.pes is not None:
            self.pes.close()
            self.pes = None

    def _deps(self, reads, writes):
        deps = {}

        def add(ev):
            if ev is None:
                return
            s, v = ev
            if deps.get(s, 0) < v:
                deps[s] = v

        for r in reads:
            add(self.last_w.get(r))
        for w in writes:
            add(self.last_w.get(w))
            for ev in self.readers.get(w, ()):
                add(ev)
        return deps

    def _commit(self, ev, reads, writes):
        for r in reads:
            self.readers.setdefault(r, []).append(ev)
        for w in writes:
            self.last_w[w] = ev
            self.readers[w] = []

    def _waits(self, eng, deps):
        out = []
        kn = self.known[eng]
        for s, v in deps.items():
            if eng == "pe" and s == "Epe":
                continue
            if kn.get(s, 0) >= v:
                continue
            kn[s] = v
            out.append((s, v))
            if s[0] == "E":
                self.targets[s].add(v)
        return out

    def ins(self, eng, fn, reads=(), writes=()):
        deps = self._deps(reads, writes)
        waits = self._waits(eng, deps)
        self.cnt[eng] += 1
        ev = ("E" + eng, self.cnt[eng])
        self.stream[eng].append((waits, fn, ev[0], self.cnt[eng]))
        self._commit(ev, reads, writes)
        return ev

    def op(self, eng, method, reads=(), writes=(), **kw):
        return self.ins(eng, lambda e: getattr(e, method)(**kw), reads, writes)

    def dop(self, q, reads=(), writes=(), method="dma_start", **kw):
        return self.dma(q, lambda e: getattr(e, method)(**kw), reads, writes)

    def dma(self, q, fn, reads=(), writes=()):
        deps = self._deps(reads, writes)
        j = self.dsem_rr[q]
        self.dsem_rr[q] = (j + 1) % NDSEM[q]
        key = "D%s%d" % (q, j)
        prev = self.dsem_use[key]
        if prev > 0:
            if deps.get(key, 0) < 16 * prev:
                deps[key] = 16 * prev
        waits = self._waits(q, deps)
        self.dsem_use[key] = prev + 1
        ev = (key, 16 * (prev + 1))
        self.stream[q].append((waits, fn, key, None))
        self._commit(ev, reads, writes)
        return ev

    def _emit(self):
        rank = self.rank
        for skey, tg in self.targets.items():
            new = sorted(i for i in tg if i > self.emitted[skey])
            assert all((skey, i) in rank for i in tg if i <= self.emitted[skey]), "wait on an already-emitted, unsignalled instruction"
            for i in new:
                self.sigcount[skey] += 1
                rank[(skey, i)] = self.sigcount[skey]
        nc = self.nc
        sem = self.sem
        stream = self.stream

        def wval(s, v):
            return rank[(s, v)] if s[0] == "E" else v

        with nc.Block() as block:
            def emit(e, handle):
                for waits, fn, skey, idx in stream[e]:
                    for s, v in waits:
                        handle.wait_ge(sem[s], wval(s, v))
                    if fn is None:
                        continue
                    ins = fn(handle)
                    if idx is None:
                        ins.then_inc(sem[skey], 16)
                    elif idx == "cc":
                        ins.then_inc(sem[skey])
                    elif (skey, idx) in rank:
                        ins.then_inc(sem[skey], 1)

            @block.tensor
            def _(h):
                emit("pe", h)

            @block.vector
            def _(h):
                emit("dve", h)

            @block.scalar
            def _(h):
                emit("act", h)

            @block.gpsimd
            def _(h):
                emit("pool", h)

            @block.sync
            def _(h):
                emit("sp", h)
        for e in ENGS:
            self.emitted["E" + e] = self.cnt[e]
            self.stream[e] = []

    def finish(self):
        self.end_phase()
        self.es.close()
        return self.nc


def phase_A(P, IO, NT, NC):
    NTT = NT + NC
    T = NTT * 128
    P.begin_phase()
    x = IO["x_src"]; ctx = IO["ctx_src"]; cvT = IO["cvT"]; w_ada = IO["w_ada"]; b_ada = IO["b_ada"]; n1w = IO["n1w"]
    w_in = IO["w_in"]; ropec = IO["ropec"]; ropes = IO["ropes"]; dec = IO["dec"]; posf = IO["posf"]; posb = IO["posb"]
    ident_d = IO["ident"]
    mod = IO["d_mod"]; o_qT = IO["d_qT"]; o_kx = IO["d_kx"]; o_kTc = IO["d_kTc"]; o_vx = IO["d_vx"]; o_vaugc = IO["d_vaugc"]
    o_qrT = IO["d_qrT"]; o_krT = IO["d_krT"]; o_kr = IO["d_kr"]; o_vr = IO["d_vr"]; o_gr = IO["d_gr"]
    o_px = IO["d_px"]; o_pxc = IO["d_pxc"]; o_stpp = IO["d_stpp"]; xb = IO["d_xb"]; xf = IO["d_xf"]
    dbg = 0

    identf = P.sb("identf", [128, 128], F32)
    identb = P.sb("identb", [128, 128], BF16)
    cvt = P.sb("cvt", [128, 8, 2], F32)
    scv = P.sb("scv", [128, 8, 2], F32)
    wada = [P.sb("wada%d" % i, [128, 8, 512], F32) for i in range(2)]
    bada = P.sb("bada", [2, 6 * D], F32)
    modrow = P.sb("modrow", [2, 6 * D], F32)
    colsrc = P.sb("colsrc", [40, 128], F32)
    cols = P.sb("cols", [128, 40], F32)
    w1x = P.sb("w1x", [128, 8], F32)
    w1c = P.sb("w1c", [128, 8], F32)
    wstg = [P.sb("wstg%d" % i, [128, DPROJ], F32) for i in range(2)]
    wb = P.sb("wb", [128, 8, DPROJ], BF16)
    dect = P.sb("dect", [128, 12], F32)
    lg = P.sb("lg", [128, 12], F32)
    tmp12 = P.sb("tmp12", [128, 12], F32)
    eps12 = P.sb("eps12", [128, 12], F32)
    posft = P.sb("posft", [128, 64], F32)
    posbt = P.sb("posbt", [128, 64], F32)

    xt = [P.sb("xt%d" % i, [128, D], F32) for i in range(2)]
    rc = [P.sb("rc%d" % i, [128, 32], F32) for i in range(2)]
    rs = [P.sb("rs%d" % i, [128, 32], F32) for i in range(2)]
    junk = P.sb("junk", [128, D], F32)
    ssq = P.sb("ssq", [128, 1], F32)
    rstd = P.sb("rstd", [128, 1], F32)
    xn = P.sb("xn", [128, D], BF16)
    hxT = P.sb("hxT", [128, 8, 128], BF16)
    tm = P.sb("tm", [128, NTOK_TM], F32)
    ra = P.sb("ra", [128, 384], F32)
    rb_ = P.sb("rb", [128, 384], F32)
    qk = P.sb("qk", [128, 768], F32)
    qkb = P.sb("qkb", [128, 768], BF16)
    wF = P.sb("wF", [128, 12], F32)
    kw = P.sb("kw", [128, 768], BF16)
    acc = P.sb("acc", [64, 4, 384], F32)

    s_qT = [P.sb("s_qT%d" % i, [128, 3, 128], BF16) for i in range(2)]
    s_kT = [P.sb("s_kT%d" % i, [128, 3, 128], BF16) for i in range(2)]
    s_v = [P.sb("s_v%d" % i, [128, 6, 65], BF16) for i in range(2)]
    s_qrT = [P.sb("s_qrT%d" % i, [128, 3, 128], BF16) for i in range(2)]
    s_krT = [P.sb("s_krT%d" % i, [128, 3, 128], BF16) for i in range(2)]
    s_vr = [P.sb("s_vr%d" % i, [128, 384], BF16) for i in range(2)]
    s_gr = [P.sb("s_gr%d" % i, [128, 384], F32) for i in range(2)]
    s_pT = [P.sb("s_pT%d" % i, [128, 2, 128], F32) for i in range(2)]

    pT = P.ps("pT", [128, 8, 128], BF16)
    pfm = P.ps("pfm", [128, 8, 128], F32)
    ptm = P.ps("ptm", [128, 2, 512], F32)
    pst = P.ps("pst", [64, 2, 512], F32)
    pmisc = P.ps("pmisc", [128, 512], F32)

    P.dma("sp", lambda e: e.dma_start(out=identf[:], in_=ident_d), writes=["identf"])
    P.dma("sp", lambda e: e.dma_start(out=cvt[:], in_=cvT), writes=["cvt"])
    P.dma("sp", lambda e: e.dma_start(out=bada[0:1, :], in_=b_ada), writes=["bada0"])
    P.dma("sp", lambda e: e.dma_start(out=bada[1:2, :], in_=b_ada), writes=["bada1"])
    P.dma("sp", lambda e: e.dma_start(out=dect[:], in_=dec), writes=["dect"])
    P.dma("sp", lambda e: e.dma_start(out=posft[:, 0:NTT], in_=posf), writes=["posft"])
    P.dma("sp", lambda e: e.dma_start(out=posbt[:, 0:NTT], in_=posb), writes=["posbt"])
    P.dma("sp", lambda e: e.dma_start(out=colsrc[32:40, :], in_=n1w), writes=["colsrc_n"])
    P.ins("dve", lambda e: e.tensor_copy(out=identb[:], in_=identf[:]), reads=["identf"], writes=["identb"])
    P.ins("act", lambda e: e.activation(out=scv[:], in_=cvt[:], func=AF.Silu), reads=["cvt"], writes=["scv"])
    P.ins("dve", lambda e: e.memset(acc[:], 0.0), writes=["acc"])
    for i_ in range(2):
        P.op("dve", "memset", [], ["s_v%d" % i_], ap=s_v[i_][:], constant=1.0)

    for nb in range(12):
        wa = wada[nb % 2]
        wtok = "wada%d" % (nb % 2)
        P.dma("sp", lambda e, wa=wa, nb=nb: e.dma_start(
            out=wa[:], in_=w_ada[:, nb * 512:(nb + 1) * 512].rearrange("(c p) n -> p c n", p=128)),
            writes=[wtok])
        for c in range(8):
            P.ins("pe", lambda e, wa=wa, c=c: e.matmul(out=pmisc[0:2, :], lhsT=scv[:, c, :], rhs=wa[:, c, :],
                                                       start=(c == 0), stop=(c == 7)),
                  reads=[wtok, "scv"], writes=["pmisc"])
        P.ins("dve", lambda e, nb=nb: e.tensor_tensor(out=modrow[:, nb * 512:(nb + 1) * 512], in0=pmisc[0:2, :],
                                                      in1=bada[:, nb * 512:(nb + 1) * 512], op=ALU.add),
              reads=["pmisc", "bada0", "bada1"], writes=["modrow"])
    P.dma("sp", lambda e: e.dma_start(out=mod, in_=modrow[:]), reads=["modrow"], writes=["mod_dram"])
    for v, (r, off) in enumerate([(0, 0), (0, D), (1, 0), (1, D)]):
        P.dma("sp", lambda e, v=v, r=r, off=off: e.dma_start(
            out=colsrc[v * 8:(v + 1) * 8, :],
            in_=mod[r:r + 1, off:off + D].rearrange("o (c p) -> (o c) p", p=128)),
            reads=["mod_dram"], writes=["colsrc%d" % v])
    P.ins("pe", lambda e: e.transpose(out=pmisc[:, 0:40], in_=colsrc[:, :], identity=identf[0:40, 0:40]),
          reads=["colsrc_n", "colsrc0", "colsrc1", "colsrc2", "colsrc3", "identf", "modrow"], writes=["pmisc"])
    P.ins("dve", lambda e: e.tensor_copy(out=cols[:], in_=pmisc[:, 0:40]), reads=["pmisc"], writes=["cols"])
    P.ins("dve", lambda e: e.scalar_tensor_tensor(out=w1x[:], in0=cols[:, 8:16], scalar=1.0, in1=cols[:, 32:40],
                                                  op0=ALU.add, op1=ALU.mult), reads=["cols"], writes=["w1x"])
    P.ins("dve", lambda e: e.scalar_tensor_tensor(out=w1c[:], in0=cols[:, 24:32], scalar=1.0, in1=cols[:, 32:40],
                                                  op0=ALU.add, op1=ALU.mult), reads=["cols"], writes=["w1c"])
    P.ins("act", lambda e: e.activation(out=eps12[:], in_=dect[:], func=AF.Exp, scale=-1.0), reads=["dect"], writes=["eps12"])
    P.ins("dve", lambda e: e.tensor_scalar(out=tmp12[:], in0=eps12[:], scalar1=-0.25, scalar2=1.0 / 3, op0=ALU.mult, op1=ALU.add),
          reads=["eps12"], writes=["tmp12"])
    P.ins("dve", lambda e: e.tensor_tensor(out=tmp12[:], in0=tmp12[:], in1=eps12[:], op=ALU.mult), reads=["tmp12", "eps12"], writes=["tmp12"])
    P.ins("dve", lambda e: e.tensor_scalar(out=tmp12[:], in0=tmp12[:], scalar1=-1.0, scalar2=0.5, op0=ALU.mult, op1=ALU.add),
          reads=["tmp12"], writes=["tmp12"])
    P.ins("dve", lambda e: e.tensor_tensor(out=tmp12[:], in0=tmp12[:], in1=eps12[:], op=ALU.mult), reads=["tmp12", "eps12"], writes=["tmp12"])
    P.ins("dve", lambda e: e.tensor_scalar(out=tmp12[:], in0=tmp12[:], scalar1=-1.0, scalar2=1.0, op0=ALU.mult, op1=ALU.add),
          reads=["tmp12"], writes=["tmp12"])
    P.ins("dve", lambda e: e.scalar_tensor_tensor(out=lg[:], in0=tmp12[:], scalar=-1.0, in1=eps12[:], op0=ALU.mult, op1=ALU.mult),
          reads=["tmp12", "eps12"], writes=["lg"])

    for c in range(8):
        ws = wstg[c % 2]
        wtok = "wstg%d" % (c % 2)
        P.dma("sp", lambda e, ws=ws, c=c: e.dma_start(out=ws[:], in_=w_in[c * 128:(c + 1) * 128, :]), writes=[wtok])
        eng = "dve"
        P.ins(eng, lambda e, ws=ws, c=c: e.tensor_copy(out=wb[:, c, :], in_=ws[:]), reads=[wtok], writes=["wb%d" % c])
    WB = ["wb%d" % c for c in range(8)]

    fm_cols = [0, 128, 256, 384, 512, 640, 2688, 2816]
    for it in range(NTT):
        b = it % 2
        is_ctx = it >= NT
        src = ctx[(it - NT) * 128:(it - NT + 1) * 128, :] if is_ctx else x[it * 128:(it + 1) * 128, :]
        w1 = w1c if is_ctx else w1x
        shc = 16 if is_ctx else 0
        t0 = it * 128
        X, RC, RS = xt[b], rc[b], rs[b]
        xtok, rctok, rstok = "xt%d" % b, "rc%d" % b, "rs%d" % b
        P.dma("sp", lambda e, X=X, src=src: e.dma_start(out=X[:], in_=src), writes=[xtok])
        P.dma("sp", lambda e, RC=RC, t0=t0: e.dma_start(out=RC[:], in_=ropec[t0:t0 + 128, :]), writes=[rctok])
        P.dma("sp", lambda e, RS=RS, t0=t0: e.dma_start(out=RS[:], in_=ropes[t0:t0 + 128, :]), writes=[rstok])
        P.ins("act", lambda e, X=X: e.activation(out=junk[:], in_=X[:], func=AF.Square, accum_out=ssq[:]),
              reads=[xtok], writes=["junk", "ssq"])
        P.ins("act", lambda e: e.activation(out=rstd[:], in_=ssq[:], func=AF.Sqrt, scale=1.0 / D, bias=1e-6),
              reads=["ssq"], writes=["rstd"])
        P.ins("dve", lambda e: e.reciprocal(out=rstd[:], in_=rstd[:]), reads=["rstd"], writes=["rstd"])
        P.ins("dve", lambda e, X=X: e.tensor_scalar(out=xn[:], in0=X[:], scalar1=rstd[:, 0:1], scalar2=None, op0=ALU.mult),
              reads=[xtok, "rstd"], writes=["xn"])
        for c in range(8):
            P.ins("pe", lambda e, c=c: e.transpose(out=pT[:, c, :], in_=xn[:, c * 128:(c + 1) * 128], identity=identb[:]),
                  reads=["xn", "identb"], writes=["pT"])
        for c in range(8):
            P.ins("act", lambda e, c=c, w1=w1, shc=shc: e.activation(
                out=hxT[:, c, :], in_=pT[:, c, :], func=AF.Identity,
                scale=w1[:, c:c + 1], bias=cols[:, shc + c:shc + c + 1]),
                reads=["pT", "w1x", "w1c", "cols"], writes=["hxT"])
        for g, col in enumerate(fm_cols):
            for c in range(8):
                P.ins("pe", lambda e, g=g, col=col, c=c: e.matmul(
                    out=pfm[:, g, :], lhsT=wb[:, c, col:col + 128], rhs=hxT[:, c, :], start=(c == 0), stop=(c == 7)),
                    reads=["hxT"] + WB, writes=["pfm"])
        P.ins("act", lambda e, b=b: e.copy(out=s_qT[b][:], in_=pfm[:, 0:3, :]), reads=["pfm"], writes=["s_qT%d" % b])
        P.ins("act", lambda e, b=b: e.copy(out=s_kT[b][:], in_=pfm[:, 3:6, :]), reads=["pfm"], writes=["s_kT%d" % b])
        P.ins("act", lambda e, b=b: e.copy(out=s_pT[b][:], in_=pfm[:, 6:8, :]), reads=["pfm"], writes=["s_pT%d" % b])
        P.dop("sp", reads=["s_qT%d" % b], out=o_qT[:, it], in_=s_qT[b][:])
        if is_ctx:
            P.dop("sp", reads=["s_kT%d" % b], out=o_kTc[:, :, (it - NT) * 128:(it - NT + 1) * 128], in_=s_kT[b][:])
        else:
            P.dop("sp", reads=["s_kT%d" % b], out=o_kx[:, :, (2 + it) * 128:(3 + it) * 128], in_=s_kT[b][:])
            if it < 2:
                P.dop("sp", reads=["s_kT%d" % b], out=xb[:, 0:768].rearrange("p (c t) -> p c t", c=3)[:, :, it * 128:(it + 1) * 128], in_=s_kT[b][:])
            if it >= NT - 2:
                j_ = it - (NT - 2)
                P.dop("sp", reads=["s_kT%d" % b], out=xb[:, 768:1536].rearrange("p (c t) -> p c t", c=3)[:, :, j_ * 128:(j_ + 1) * 128], in_=s_kT[b][:])
        if is_ctx:
            P.dop("sp", reads=["s_pT%d" % b], out=o_pxc[:, :, 8 + (it - NT) * 128:8 + (it - NT + 1) * 128], in_=s_pT[b][:])
        else:
            P.dop("sp", reads=["s_pT%d" % b], out=o_px[:, :, 8 + it * 128:8 + (it + 1) * 128], in_=s_pT[b][:])
            if it == 0:
                P.dop("sp", reads=["s_pT%d" % b], out=xf[:, 0:16].rearrange("p (c t) -> p c t", c=2), in_=s_pT[b][:, :, 0:8])
            if it == NT - 1:
                P.dop("sp", reads=["s_pT%d" % b], out=xf[:, 16:32].rearrange("p (c t) -> p c t", c=2), in_=s_pT[b][:, :, 120:128])
        for hf in range(2):
            for j in range(2):
                col = 768 + (hf * 2 + j) * 480
                for c in range(8):
                    P.ins("pe", lambda e, j=j, col=col, c=c: e.matmul(
                        out=ptm[:, j, 0:480], lhsT=hxT[:, c, :], rhs=wb[:, c, col:col + 480], start=(c == 0), stop=(c == 7)),
                        reads=["hxT"] + WB, writes=["ptm"])
            P.ins("act", lambda e, hf=hf: e.copy(
                out=tm[:, hf * 960:(hf + 1) * 960].rearrange("p (j n) -> p j n", j=2), in_=ptm[:, :, 0:480]),
                reads=["ptm"], writes=["tm"])
        P.ins("act", lambda e, b=b: e.copy(out=s_v[b][:, :, 0:64], in_=tm[:, 0:384].rearrange("p (h e) -> p h e", h=6)), reads=["tm"], writes=["s_v%d" % b])
        P.ins("act", lambda e, b=b: e.copy(out=s_vr[b][:], in_=tm[:, 1152:1536]), reads=["tm"], writes=["s_vr%d" % b])
        P.ins("act", lambda e, b=b: e.copy(out=s_gr[b][:], in_=tm[:, 1536:1920]), reads=["tm"], writes=["s_gr%d" % b])
        if is_ctx:
            P.dop("sp", reads=["s_v%d" % b], out=o_vaugc[:, it - NT], in_=s_v[b][:])
        else:
            P.dop("sp", reads=["s_v%d" % b], out=o_vx[:, 2 + it], in_=s_v[b][:])
            if it < 2:
                P.dop("sp", reads=["s_v%d" % b], out=xb[:, 1536:2316].rearrange("p (k h e) -> p k h e", k=2, h=6)[:, it], in_=s_v[b][:])
            if it >= NT - 2:
                P.dop("sp", reads=["s_v%d" % b], out=xb[:, 2316:3096].rearrange("p (k h e) -> p k h e", k=2, h=6)[:, it - (NT - 2)], in_=s_v[b][:])
        P.dop("sp", reads=["s_vr%d" % b], out=o_vr[:, it, :], in_=s_vr[b][:])
        P.dop("sp", reads=["s_gr%d" % b], out=o_gr[:, it, :], in_=s_gr[b][:])
        src5 = tm[:, 384:1152].rearrange("p (h f s i) -> p h f s i", h=12, f=2, s=2, i=16)
        dst5 = qk[:].rearrange("p (h f s i) -> p h f s i", h=12, f=2, s=2, i=16)
        ra4 = ra[:].rearrange("p (h f i) -> p h f i", h=12, f=2, i=16)
        rb4 = rb_[:].rearrange("p (h f i) -> p h f i", h=12, f=2, i=16)
        cosb = RC[:].rearrange("p (f i) -> p f i", f=2).unsqueeze(1).to_broadcast([128, 12, 2, 16])
        sinb = RS[:].rearrange("p (f i) -> p f i", f=2).unsqueeze(1).to_broadcast([128, 12, 2, 16])
        A_ = src5[:, :, :, 0, :]
        B_ = src5[:, :, :, 1, :]
        P.ins("dve", lambda e, A_=A_, cosb=cosb: e.tensor_tensor(out=ra4, in0=A_, in1=cosb, op=ALU.mult), reads=["tm", rctok], writes=["ra"])
        P.ins("dve", lambda e, B_=B_, sinb=sinb: e.tensor_tensor(out=rb4, in0=B_, in1=sinb, op=ALU.mult), reads=["tm", rstok], writes=["rb"])
        P.ins("dve", lambda e, dst5=dst5: e.tensor_tensor(out=dst5[:, :, :, 0, :], in0=ra4, in1=rb4, op=ALU.subtract), reads=["ra", "rb"], writes=["qk_a"])
        P.ins("dve", lambda e, A_=A_, sinb=sinb: e.tensor_tensor(out=ra4, in0=A_, in1=sinb, op=ALU.mult), reads=["tm", rstok], writes=["ra"])
        P.ins("dve", lambda e, B_=B_, cosb=cosb: e.tensor_tensor(out=rb4, in0=B_, in1=cosb, op=ALU.mult), reads=["tm", rctok], writes=["rb"])
        P.ins("dve", lambda e, dst5=dst5: e.tensor_tensor(out=dst5[:, :, :, 1, :], in0=ra4, in1=rb4, op=ALU.add), reads=["ra", "rb"], writes=["qk_b"])
        P.ins("act", lambda e: e.copy(out=qkb[:], in_=qk[:]), reads=["qk_a", "qk_b"], writes=["qkb"])
        P.dop("sp", reads=["qkb"], out=o_kr[:, it, :], in_=qkb[:, 384:768])
        for c in range(6):
            P.ins("pe", lambda e, c=c: e.transpose(out=pT[:, c, :], in_=qkb[:, c * 128:(c + 1) * 128], identity=identb[:]),
                  reads=["qkb", "identb"], writes=["pT"])
        P.ins("act", lambda e, b=b: e.copy(out=s_qrT[b][:], in_=pT[:, 0:3, :]), reads=["pT"], writes=["s_qrT%d" % b])
        P.ins("act", lambda e, b=b: e.copy(out=s_krT[b][:], in_=pT[:, 3:6, :]), reads=["pT"], writes=["s_krT%d" % b])
        P.dop("sp", reads=["s_qrT%d" % b], out=o_qrT[:, it], in_=s_qrT[b][:])
        P.dop("sp", reads=["s_krT%d" % b], out=o_krT[:, it], in_=s_krT[b][:])
        P.ins("act", lambda e, it=it: e.activation(out=wF[:, 0:6], in_=lg[:, 0:6], func=AF.Exp, scale=posft[:, it:it + 1],
                                                   bias=math.log(0.125)), reads=["lg", "posft"], writes=["wF0"])
        P.ins("act", lambda e, it=it: e.activation(out=wF[:, 6:12], in_=lg[:, 6:12], func=AF.Exp, scale=posbt[:, it:it + 1],
                                                   bias=math.log(0.125)), reads=["lg", "posbt"], writes=["wF1"])
        for d_ in range(2):
            P.ins("dve", lambda e, d_=d_: e.tensor_tensor(
                out=kw[:, d_ * 384:(d_ + 1) * 384].rearrange("p (h e) -> p h e", h=6),
                in0=qk[:, 384:768].rearrange("p (h e) -> p h e", h=6),
                in1=wF[:, d_ * 6:(d_ + 1) * 6].unsqueeze(2).to_broadcast([128, 6, 64]), op=ALU.mult),
                reads=["qk_a", "qk_b", "wF0", "wF1"], writes=["kw%d" % d_])
        for d_ in range(2):
            for h in range(6):
                P.ins("pe", lambda e, d_=d_, h=h, b=b: e.matmul(
                    out=pst[:, d_, h * 64:(h + 1) * 64], lhsT=kw[:, d_ * 384 + h * 64:d_ * 384 + (h + 1) * 64],
                    rhs=s_vr[b][:, h * 64:(h + 1) * 64], start=True, stop=True),
                    reads=["kw%d" % d_, "s_vr%d" % b], writes=["pst"])
        so = 2 if is_ctx else 0
        for d_ in range(2):
            P.ins("dve", lambda e, d_=d_, so=so: e.tensor_tensor(out=acc[:, so + d_, :], in0=acc[:, so + d_, :],
                                                                  in1=pst[:, d_, 0:384], op=ALU.add),
                  reads=["pst", "acc"], writes=["acc"])

    acc5 = acc[:].rearrange("d s (c j e) -> d s c j e", j=2, e=64)
    for j_ in range(2):
        P.dop("sp", reads=["acc"], out=o_stpp[j_ * 64:(j_ + 1) * 64], in_=acc5[:, :, :, j_, :])
        P.dop("sp", reads=["acc"], out=xf[j_ * 64:(j_ + 1) * 64, 32:416].rearrange("p (s c e) -> p s c e", s=2, c=3), in_=acc5[:, 0:2, :, j_, :])
    P.end_phase()


def phase_X(P, IO, NT, ncores=8):
    P.begin_phase()
    xb_t = IO["t_xb"]; xf_t = IO["t_xf"]; gb_t = IO["t_gb"]; gf_t = IO["t_gf"]
    gb = IO["d_gb"]; gf = IO["d_gf"]; kx = IO["d_kx"]; vx = IO["d_vx"]; px = IO["d_px"]; pxc = IO["d_pxc"]; sel = IO["sel"]
    groups = [[2 * i, 2 * i + 1] for i in range(ncores // 2)]
    P.cc("AllGather", groups, xb_t, gb_t, writes=["gb"])
    P.cc("AllGather", groups, xf_t, gf_t, writes=["gf"])
    kb = P.sb("kb", [128, 2, 768], BF16)
    vb = P.sb("vb", [128, 2, 780], BF16)
    hal = P.sb("hal", [128, 2, 16]); sels = P.sb("sels", [128, 4]); zer = P.sb("zer", [128, 16])
    P.dop("sp", writes=["sels"], out=sels[:], in_=sel)
    P.op("dve", "memset", [], ["zer"], ap=zer[:], constant=0.0)
    P.dop("sp", reads=["gb"], writes=["kb0"], out=kb[:, 0, :], in_=gb[0:128, 768:1536])
    P.dop("sp", reads=["gb"], writes=["kb1"], out=kb[:, 1, :], in_=gb[128:256, 0:768])
    P.dop("sp", reads=["gb"], writes=["vb0"], out=vb[:, 0, :], in_=gb[0:128, 2316:3096])
    P.dop("sp", reads=["gb"], writes=["vb1"], out=vb[:, 1, :], in_=gb[128:256, 1536:2316])
    P.dop("sp", reads=["kb0"], out=kx[:, :, 0:256], in_=kb[:, 0, :].rearrange("p (c t) -> p c t", c=3))
    P.dop("sp", reads=["kb1"], out=kx[:, :, (NT + 2) * 128:(NT + 4) * 128], in_=kb[:, 1, :].rearrange("p (c t) -> p c t", c=3))
    P.dop("sp", reads=["vb0"], out=vx[:, 0:2], in_=vb[:, 0, :].rearrange("p (k h e) -> p k h e", k=2, h=6))
    P.dop("sp", reads=["vb1"], out=vx[:, NT + 2:NT + 4], in_=vb[:, 1, :].rearrange("p (k h e) -> p k h e", k=2, h=6))
    P.dop("sp", reads=["gf"], writes=["hal0"], out=hal[:, 0, :], in_=gf[0:128, 16:32])
    P.dop("sp", reads=["gf"], writes=["hal1"], out=hal[:, 1, :], in_=gf[128:256, 0:16])
    P.op("dve", "tensor_scalar", ["hal0", "sels"], ["hal0"], out=hal[:, 0, :], in0=hal[:, 0, :], scalar1=sels[:, 0:1], scalar2=None, op0=ALU.mult)
    P.op("dve", "tensor_scalar", ["hal1", "sels"], ["hal1"], out=hal[:, 1, :], in0=hal[:, 1, :], scalar1=sels[:, 1:2], scalar2=None, op0=ALU.mult)
    P.dop("sp", reads=["hal0"], out=px[:, :, 0:8], in_=hal[:, 0, :].rearrange("p (c t) -> p c t", c=2))
    P.dop("sp", reads=["hal1"], out=px[:, :, 8 + NT * 128:16 + NT * 128], in_=hal[:, 1, :].rearrange("p (c t) -> p c t", c=2))
    P.dop("sp", reads=["zer"], out=pxc[:, :, 0:8], in_=zer[:].rearrange("p (c t) -> p c t", c=2))
    P.dop("sp", reads=["zer"], out=pxc[:, :, 264:272], in_=zer[:].rearrange("p (c t) -> p c t", c=2))
    P.end_phase()


def phase_B1(P, IO, NT, NC):
    NTT = NT + NC
    T = NTT * 128
    stages = 15
    P.begin_phase()
    x = IO["x_src"]; mod = IO["d_mod"]; qT = IO["d_qT"]; kx = IO["d_kx"]; vx = IO["d_vx"]; kTc = IO["d_kTc"]; vaugc = IO["d_vaugc"]
    G = IO["G"]; M01 = IO["M01"]; qrT = IO["d_qrT"]; krT = IO["d_krT"]; kr = IO["d_kr"]; vr = IO["d_vr"]; gr = IO["d_gr"]
    st_own = IO["d_stpp"]; gf = IO["d_gf"]; npow = IO["npow"]; sel = IO["sel"]; dec_row = IO["dec_row"]; dec_col = IO["dec_col"]
    cst = IO["cst"]; pm = IO["pm"]; ident_d = IO["ident"]; gnw = IO["gnw"]; px = IO["d_px"]; pxc = IO["d_pxc"]; invc = IO["invc"]
    wpool = IO["wpool"]; pscale = IO["pscale"]; w_out = IO["w_out"]; x1 = IO["x1_dst"]

    identf = P.sb("identf", [128, 128]); identb = P.sb("identb", [128, 128], BF16)
    wostg = [P.sb("wostg%d" % i, [128, 512]) for i in range(2)]
    wo = P.sb("wo", [128, 8, D], BF16)
    Gs = [P.sb("Gs%d" % i, [128, 896]) for i in range(2)]
    m01 = P.sb("m01", [128, 896])
    EB = P.sb("EB", [128, 5, 6, 896], BF16)
    kTc_s = P.sb("kTc_s", [128, 3, 256], BF16)
    vaugc_s = P.sb("vaugc_s", [128, 2, 6, 65], BF16)
    decr = P.sb("decr", [128, 12]); decc = P.sb("decc", [128, 6])
    lgr = P.sb("lgr", [128, 12]); lgc = P.sb("lgc", [128, 6])
    t12 = P.sb("t12", [128, 12]); e12 = P.sb("e12", [128, 12])
    cs = P.sb("cs", [128, 6, 128])
    pms = P.sb("pms", [128, 2])
    DTf = P.sb("DTf", [128, 6, 128]); DTb = P.sb("DTb", [128, 6, 128])
    XIf = P.sb("XIf", [128, 3, 128]); XIb = P.sb("XIb", [128, 3, 128])
    ZF = P.sb("ZF", [128, 6]); ZB = P.sb("ZB", [128, 6])
    cdrow = P.sb("cdrow", [128, 12])
    npw = P.sb("npw", [128, 2])
    sto = P.sb("sto", [128, 4, 3, 64]); stt = P.sb("stt", [128, 2, 3, 64]); sels = P.sb("sels", [128, 4]); hal = P.sb("hal", [128, 2, 16])
    scl = P.sb("scl", [128, 6])
    Rf = P.sb("Rf", [128, 3, 64]); Rb = P.sb("Rb", [128, 3, 64]); Rtmp = P.sb("Rtmp", [128, 3, 64])
    Rfb = P.sb("Rfb", [128, 3, 64], BF16)
    Rbs = P.sb("Rbs", [128, NTT, 3, 64], BF16)
    g1x = P.sb("g1x", [128, D]); g1c = P.sb("g1c", [128, D])
    gnwb = P.sb("gnwb", [128, 384])
    invcs = P.sb("invcs", [128, 5, 2, 128])
    wpf = P.sb("wpf", [128, 2, 128]); wpb = P.sb("wpb", [128, 2, 128], BF16)
    psc = P.sb("psc", [128, 2])

    xt = [P.sb("xt%d" % i, [128, D]) for i in range(2)]
    qTt = [P.sb("qTt%d" % i, [128, 3, 128], BF16) for i in range(2)]
    kwt = [P.sb("kwt%d" % i, [128, 3, 896], BF16) for i in range(2)]
    vwt = [P.sb("vwt%d" % i, [128, 7, 6, 65], BF16) for i in range(2)]
    qrt = [P.sb("qrt%d" % i, [128, 3, 128], BF16) for i in range(2)]
    krt = [P.sb("krt%d" % i, [128, 3, 128], BF16) for i in range(2)]
    krm = [P.sb("krm%d" % i, [128, 384], BF16) for i in range(2)]
    vrm = [P.sb("vrm%d" % i, [128, 384], BF16) for i in range(2)]
    grm = [P.sb("grm%d" % i, [128, 384]) for i in range(2)]
    ppd = [P.sb("ppd%d" % i, [128, 2, 144]) for i in range(2)]
    krm2 = [P.sb("krm2%d" % i, [128, 384], BF16) for i in range(2)]
    vrm2 = [P.sb("vrm2%d" % i, [128, 384], BF16) for i in range(2)]
    kz = P.sb("kz", [128, 384], BF16)
    pexp = P.sb("pexp", [128, 9, 128], BF16)
    rcp = P.sb("rcp", [128, 6])
    mixtok = P.sb("mixtok", [128, 768], BF16)
    mixT = P.sb("mixT", [128, 8, 128], BF16)
    SDf = P.sb("SDf", [128, 6, 128], BF16); SDb = P.sb("SDb", [128, 6, 128], BF16)
    qxf = P.sb("qxf", [128, 3, 128], BF16); qxb = P.sb("qxb", [128, 3, 128], BF16)
    ysum = P.sb("ysum", [128, 6]); yd = P.sb("yd", [128, 384]); yq = P.sb("yq", [128, 384])
    yv = P.sb("yv", [128, 6]); sg = P.sb("sg", [128, 384])
    a2 = P.sb("a2", [128, 2, 143]); a4 = P.sb("a4", [128, 2, 141]); a8 = P.sb("a8", [128, 2, 137]); a16 = P.sb("a16", [128, 2, 128])
    pdm = P.sb("pdm", [128, 2, 128])
    pdf = P.sb("pdf", [128, 2, 128], BF16)
    otmp = P.sb("otmp", [128, D])

    pS = P.ps("pS", [128, 8, 128])
    po = P.ps("po", [128, 6, 65])
    py = P.ps("py", [128, 6, 64])
    pmi = P.ps("pmi", [128, 512])
    ptr = P.ps("ptr", [128, 8, 128], BF16)
    pout = P.ps("pout", [128, 2, 512])

    ld = lambda out, in_, w, r=(): P.dop("sp", reads=list(r), writes=[w], out=out, in_=in_)

    if stages != 15:
        P.op("dve", "memset", [], ["mixtok_a", "mixtok_b"], ap=mixtok[:], constant=0.0)
        P.op("dve", "memset", [], ["mixT_p", "mixT_t"], ap=mixT[:], constant=0.0)
        for it_ in range(NTT):
            P.op("dve", "memset", [], ["Rbs%d" % it_], ap=Rbs[:, it_, :, :], constant=0.0)
    ld(identf[:], ident_d, "identf")
    P.op("dve", "tensor_copy", ["identf"], ["identb"], out=identb[:], in_=identf[:])
    ld(kTc_s[:], kTc, "kTc_s"); ld(vaugc_s[:], vaugc, "vaugc_s")
    ld(decr[:], dec_row, "decr"); ld(decc[:], dec_col, "decc"); ld(cs[:], cst, "cs"); ld(pms[:], pm, "pms")
    ld(npw[:], npow, "npw"); ld(sto[:], st_own, "sto"); ld(sels[:], sel, "sels")
    ld(stt[:, 0], gf[0:128, 32:224].rearrange("p (c e) -> p c e", c=3), "stt0")
    ld(stt[:, 1], gf[128:256, 224:416].rearrange("p (c e) -> p c e", c=3), "stt1")
    ld(g1x[:], mod[0:1, 2 * D:3 * D].partition_broadcast(128), "g1x")
    ld(g1c[:], mod[1:2, 2 * D:3 * D].partition_broadcast(128), "g1c")
    ld(gnwb[:], gnw.partition_broadcast(128), "gnwb")
    ld(invcs[:], invc, "invcs"); ld(wpf[:], wpool, "wpf"); ld(psc[:], pscale, "psc")
    P.op("dve", "tensor_copy", ["wpf"], ["wpb"], out=wpb[:], in_=wpf[:])
    for c in range(8):
        for hf in range(2):
            ld(wostg[hf][:], w_out[c * 128:(c + 1) * 128, hf * 512:(hf + 1) * 512], "wostg%d" % hf)
            P.op("dve" if hf == 0 else "act", "tensor_copy" if hf == 0 else "copy", ["wostg%d" % hf], ["wo%d" % c], out=wo[:, c, hf * 512:(hf + 1) * 512], in_=wostg[hf][:])
    WO = ["wo%d" % c for c in range(8)]
    for s in range(5):
        ld(m01[:], M01[:, s, :], "m01")
        for h in range(6):
            i = (s * 6 + h) % 2
            ld(Gs[i][:], G[:, s, h, :], "Gs%d" % i)
            P.op("act", "activation", ["Gs%d" % i], ["Gs%d" % i], out=Gs[i][:], in_=Gs[i][:], func=AF.Exp)
            P.op("dve", "tensor_tensor", ["Gs%d" % i, "m01"], ["EB"], out=EB[:, s, h, :], in0=Gs[i][:], in1=m01[:], op=ALU.mult)

    def logsig(dst, src, n, stok, dtok):
        P.op("act", "activation", [stok], ["e12"], out=e12[:, 0:n], in_=src, func=AF.Exp, scale=-1.0)
        P.op("dve", "tensor_scalar", ["e12"], ["t12"], out=t12[:, 0:n], in0=e12[:, 0:n], scalar1=-0.25, scalar2=1.0 / 3, op0=ALU.mult, op1=ALU.add)
        P.op("dve", "tensor_tensor", ["t12", "e12"], ["t12"], out=t12[:, 0:n], in0=t12[:, 0:n], in1=e12[:, 0:n], op=ALU.mult)
        P.op("dve", "tensor_scalar", ["t12"], ["t12"], out=t12[:, 0:n], in0=t12[:, 0:n], scalar1=-1.0, scalar2=0.5, op0=ALU.mult, op1=ALU.add)
        P.op("dve", "tensor_tensor", ["t12", "e12"], ["t12"], out=t12[:, 0:n], in0=t12[:, 0:n], in1=e12[:, 0:n], op=ALU.mult)
        P.op("dve", "tensor_scalar", ["t12"], ["t12"], out=t12[:, 0:n], in0=t12[:, 0:n], scalar1=-1.0, scalar2=1.0, op0=ALU.mult, op1=ALU.add)
        P.op("dve", "scalar_tensor_tensor", ["t12", "e12"], [dtok], out=dst, in0=t12[:, 0:n], scalar=-1.0, in1=e12[:, 0:n], op0=ALU.mult, op1=ALU.mult)
    logsig(lgr[:], decr[:], 12, "decr", "lgr")
    logsig(lgc[:], decc[:], 6, "decc", "lgc")
    sidx = lambda h: (h % 2) * 3 + h // 2
    for h in range(6):
        P.op("act", "activation", ["lgr", "cs"], ["DTf%d" % h], out=DTf[:, sidx(h), :], in_=cs[:, 0, :], func=AF.Exp, scale=lgr[:, h:h + 1])
        P.op("dve", "tensor_tensor", ["DTf%d" % h, "cs"], ["DTf%d" % h], out=DTf[:, sidx(h), :], in0=DTf[:, sidx(h), :], in1=cs[:, 1, :], op=ALU.mult)
        P.op("act", "activation", ["lgr", "cs"], ["DTb%d" % h], out=DTb[:, sidx(h), :], in_=cs[:, 2, :], func=AF.Exp, scale=lgr[:, 6 + h:7 + h])
        P.op("dve", "tensor_tensor", ["DTb%d" % h, "cs"], ["DTb%d" % h], out=DTb[:, sidx(h), :], in0=DTb[:, sidx(h), :], in1=cs[:, 3, :], op=ALU.mult)
    DT = ["DTf%d" % h for h in range(6)] + ["DTb%d" % h for h in range(6)]
    for c in range(3):
        P.op("act", "activation", ["lgc", "cs"], ["XI"], out=XIf[:, c, :], in_=cs[:, 4, :], func=AF.Exp, scale=lgc[:, c:c + 1])
        P.op("act", "activation", ["lgc", "cs"], ["XI"], out=XIb[:, c, :], in_=cs[:, 5, :], func=AF.Exp, scale=lgc[:, 3 + c:4 + c])
    P.op("act", "activation", ["lgr", "pms"], ["ZF"], out=ZF[:], in_=lgr[:, 0:6], func=AF.Exp, scale=pms[:, 0:1], bias=math.log(0.125))
    P.op("act", "activation", ["lgr", "pms"], ["ZB"], out=ZB[:], in_=lgr[:, 6:12], func=AF.Exp, scale=pms[:, 1:2], bias=math.log(0.125))
    P.op("act", "activation", ["lgr"], ["cdrow"], out=cdrow[:], in_=lgr[:], func=AF.Exp, scale=128.0)
    P.op("act", "activation", ["lgc", "npw"], ["scl"], out=scl[:, 0:3], in_=lgc[:, 0:3], func=AF.Exp, scale=npw[:, 0:1])
    P.op("act", "activation", ["lgc", "npw"], ["scl"], out=scl[:, 3:6], in_=lgc[:, 3:6], func=AF.Exp, scale=npw[:, 1:2])
    P.op("dve", "tensor_tensor", ["sto", "scl"], ["Rf"], out=Rf[:], in0=sto[:, 2, :, :], in1=scl[:, 0:3].unsqueeze(2).to_broadcast([128, 3, 64]), op=ALU.mult)
    P.op("dve", "scalar_tensor_tensor", ["Rf", "stt0", "sels"], ["Rf"], out=Rf[:].rearrange("p c e -> p (c e)"), in0=stt[:, 0, :, :].rearrange("p c e -> p (c e)"), scalar=sels[:, 2:3], in1=Rf[:].rearrange("p c e -> p (c e)"), op0=ALU.mult, op1=ALU.add)
    P.op("dve", "tensor_tensor", ["sto", "scl"], ["Rb"], out=Rb[:], in0=sto[:, 3, :, :], in1=scl[:, 3:6].unsqueeze(2).to_broadcast([128, 3, 64]), op=ALU.mult)
    P.op("dve", "scalar_tensor_tensor", ["Rb", "stt1", "sels"], ["Rb"], out=Rb[:].rearrange("p c e -> p (c e)"), in0=stt[:, 1, :, :].rearrange("p c e -> p (c e)"), scalar=sels[:, 3:4], in1=Rb[:].rearrange("p c e -> p (c e)"), op0=ALU.mult, op1=ALU.add)

    cdc = P.sb("cdc", [128, 6])
    P.op("act", "activation", ["lgc"], ["cdc"], out=cdc[:], in_=lgc[:], func=AF.Exp, scale=128.0)

    def state_update(Rm, KZ, V, vtok, dirn, it):
        for h in range(6):
            c, j = h // 2, h % 2
            P.op("pe", "matmul", ["kz", vtok], ["pmi_s"], out=pmi[j * 64:(j + 1) * 64, c * 64:(c + 1) * 64],
                 lhsT=KZ[:, h * 64:(h + 1) * 64], rhs=V[:, h * 64:(h + 1) * 64], start=True, stop=True)
        rtok = "Rf" if dirn == 0 else "Rb"
        P.op("dve", "tensor_tensor", [rtok, "cdc"], ["Rtmp"], out=Rtmp[:], in0=Rm[:],
             in1=cdc[:, dirn * 3:dirn * 3 + 3].unsqueeze(2).to_broadcast([128, 3, 64]), op=ALU.mult)
        P.op("dve", "tensor_tensor", ["Rtmp", "pmi_s"], [rtok], out=Rm[:], in0=Rtmp[:],
             in1=pmi[:, 0:192].rearrange("p (c e) -> p c e", c=3), op=ALU.add)

    order = list(range(NT - 1, -1, -1)) + list(range(NTT - 1, NT - 1, -1))
    for i, it in enumerate(order if stages & 8 else []):
        b = i % 2
        if it == NTT - 1 and NC > 0:
            P.op("dve", "memset", [], ["Rb"], ap=Rb[:], constant=0.0)
        P.op("act", "copy", ["Rb"], ["Rbs%d" % it], out=Rbs[:, it, :, :], in_=Rb[:])
        last = (it == 0) or (it == NT)
        if last:
            continue
        ld(krm2[b][:], kr[:, it, :], "krm2%d" % b); ld(vrm2[b][:], vr[:, it, :], "vrm2%d" % b)
        P.op("dve", "tensor_tensor", ["krm2%d" % b, "ZB"], ["kz"], out=kz[:].rearrange("p (h e) -> p h e", h=6),
             in0=krm2[b][:].rearrange("p (h e) -> p h e", h=6), in1=ZB[:].unsqueeze(2).to_broadcast([128, 6, 64]), op=ALU.mult)
        state_update(Rb, kz, vrm2[b], "vrm2%d" % b, 1, it)

    def loads(it):
        b = it % 2
        is_ctx = it >= NT
        t0 = it * 128
        S = lambda n: "%s%d" % (n, b)
        ld(xt[b][:], x[t0:t0 + 128, :], S("xt"))
        ld(qTt[b][:], qT[:, it], S("qTt"))
        special = (not is_ctx) and (it < 2 or it >= NT - 2)
        nwb = 7 if special else 5
        ext0 = (0 if it < 2 else NT - 3) if special else it
        if not is_ctx:
            ld(kwt[b][:, :, 0:nwb * 128], kx[:, :, ext0 * 128:(ext0 + nwb) * 128], S("kwt")); ld(vwt[b][:, 0:nwb], vx[:, ext0:ext0 + nwb], S("vwt"))
        ld(qrt[b][:], qrT[:, it], S("qrt")); ld(krt[b][:], krT[:, it], S("krt"))
        ld(krm[b][:], kr[:, it, :], S("krm")); ld(vrm[b][:], vr[:, it, :], S("vrm")); ld(grm[b][:], gr[:, it, :], S("grm"))
        if is_ctx:
            ld(ppd[b][:], pxc[:, :, (it - NT) * 128:(it - NT) * 128 + 144], S("ppd"))
        else:
            ld(ppd[b][:], px[:, :, it * 128:it * 128 + 144], S("ppd"))

    loads(0)
    for it in range(NTT):
        b = it % 2
        is_ctx = it >= NT
        t0 = it * 128
        S = lambda n: "%s%d" % (n, b)
        special = (not is_ctx) and (it < 2 or it >= NT - 2)
        nwb = 7 if special else 5
        ext0 = (0 if it < 2 else NT - 3) if special else it
        if it + 1 < NTT:
            loads(it + 1)
        if stages & 1:
            if is_ctx:
                slot = None
            elif it == 0:
                slot = 0
            elif it == 1:
                slot = 1
            elif it == NT - 2:
                slot = 3
            elif it == NT - 1:
                slot = 4
            else:
                slot = 2
            for h in range(6):
                c, j = h // 2, h % 2
                pr = slice(j * 64, (j + 1) * 64)
                blocks = []
                if not is_ctx:
                    for bl in range(nwb):
                        blocks.append((kwt[b][pr, c, bl * 128:(bl + 1) * 128], vwt[b][:, bl, h, :], [S("kwt")], [S("vwt")]))
                for bl in range(2):
                    blocks.append((kTc_s[pr, c, bl * 128:(bl + 1) * 128], vaugc_s[:, bl, h, :], ["kTc_s"], ["vaugc_s"]))
                nb = len(blocks)
                for g0 in range(0, nb, 8):
                    g1 = min(nb, g0 + 8)
                    for bi in range(g0, g1):
                        kap, vap, kt_, vt_ = blocks[bi]
                        P.op("pe", "matmul", kt_ + [S("qTt")], ["pS"], out=pS[:, bi - g0, :], lhsT=kap, rhs=qTt[b][pr, c, :], start=True, stop=True)
                    P.op("act", "activation", ["pS"], ["pexp"], out=pexp[:, g0:g1, :], in_=pS[:, 0:g1 - g0, :], func=AF.Exp, scale=0.125)
                if not is_ctx:
                    P.op("dve", "tensor_tensor", ["pexp", "EB"], ["pexp"], out=pexp[:, 0:nwb, :], in0=pexp[:, 0:nwb, :],
                         in1=EB[:, slot, h, 0:nwb * 128].rearrange("p (k q) -> p k q", k=nwb), op=ALU.mult)
                for bi, (kap, vap, kt_, vt_) in enumerate(blocks):
                    P.op("pe", "matmul", vt_ + ["pexp"], ["po"], out=po[:, h, :], lhsT=pexp[:, bi, :], rhs=vap, start=(bi == 0), stop=(bi == nb - 1))
            P.op("dve", "reciprocal", ["po"], ["rcp"], out=rcp[:], in_=po[:, :, 64])
            P.op("dve", "tensor_tensor", ["po", "rcp"], ["mixtok_a"], out=mixtok[:, 0:384].rearrange("p (h e) -> p h e", h=6),
                 in0=po[:, :, 0:64], in1=rcp[:].unsqueeze(2).to_broadcast([128, 6, 64]), op=ALU.mult)
        if stages & 2:
            if it == NT and NC > 0:
                P.op("dve", "memset", [], ["Rf"], ap=Rf[:], constant=0.0)
            P.op("act", "copy", ["Rf"], ["Rfb"], out=Rfb[:], in_=Rf[:])
            for h in range(6):
                c, j = h // 2, h % 2
                pr = slice(j * 64, (j + 1) * 64)
                P.op("pe", "matmul", [S("krt"), S("qrt")], ["pS"], out=pS[:, j * 4 + c, :], lhsT=krt[b][pr, c, :], rhs=qrt[b][pr, c, :], start=True, stop=True)
            for j_ in range(2):
                P.op("dve", "tensor_tensor", ["pS"] + DT, ["SDf"], out=SDf[:, j_ * 3:j_ * 3 + 3, :], in0=pS[:, j_ * 4:j_ * 4 + 3, :], in1=DTf[:, j_ * 3:j_ * 3 + 3, :], op=ALU.mult)
                P.op("dve", "tensor_tensor", ["pS"] + DT, ["SDb"], out=SDb[:, j_ * 3:j_ * 3 + 3, :], in0=pS[:, j_ * 4:j_ * 4 + 3, :], in1=DTb[:, j_ * 3:j_ * 3 + 3, :], op=ALU.mult)
            P.op("dve", "tensor_tensor", [S("qrt"), "XI"], ["qxf"], out=qxf[:], in0=qrt[b][:], in1=XIf[:], op=ALU.mult)
            P.op("dve", "tensor_tensor", [S("qrt"), "XI"], ["qxb"], out=qxb[:], in0=qrt[b][:], in1=XIb[:], op=ALU.mult)
            for h in range(6):
                c, j = h // 2, h % 2
                pr = slice(j * 64, (j + 1) * 64)
                vh = vrm[b][:, h * 64:(h + 1) * 64]
                P.op("pe", "matmul", ["SDf", S("vrm")], ["py"], out=py[:, h, :], lhsT=SDf[:, sidx(h), :], rhs=vh, start=True, stop=False)
                P.op("pe", "matmul", ["SDb", S("vrm")], ["py"], out=py[:, h, :], lhsT=SDb[:, sidx(h), :], rhs=vh, start=False, stop=False)
                P.op("pe", "matmul", ["qxf", "Rfb"], ["py"], out=py[:, h, :], lhsT=qxf[pr, c, :], rhs=Rfb[pr, c, :], start=False, stop=False)
                P.op("pe", "matmul", ["qxb", "Rbs%d" % it], ["py"], out=py[:, h, :], lhsT=qxb[pr, c, :], rhs=Rbs[pr, it, c, :], start=False, stop=True)
            if it != NT - 1 and it != NTT - 1:
                P.op("dve", "tensor_tensor", [S("krm"), "ZF"], ["kz"], out=kz[:].rearrange("p (h e) -> p h e", h=6),
                     in0=krm[b][:].rearrange("p (h e) -> p h e", h=6), in1=ZF[:].unsqueeze(2).to_broadcast([128, 6, 64]), op=ALU.mult)
                state_update(Rf, kz, vrm[b], S("vrm"), 0, it)
            y3 = py[:, :, :]
            P.op("dve", "tensor_reduce", ["py"], ["ysum"], out=ysum[:], in_=y3, axis=AX.X, op=ALU.add)
            P.op("dve", "tensor_scalar", ["ysum"], ["ysum"], out=ysum[:], in0=ysum[:], scalar1=1.0 / 64, scalar2=None, op0=ALU.mult)
            P.op("dve", "tensor_tensor", ["py", "ysum"], ["yd"], out=yd[:].rearrange("p (h e) -> p h e", h=6), in0=y3,
                 in1=ysum[:].unsqueeze(2).to_broadcast([128, 6, 64]), op=ALU.subtract)
            P.op("dve", "tensor_tensor", ["yd"], ["yq"], out=yq[:], in0=yd[:], in1=yd[:], op=ALU.mult)
            P.op("dve", "tensor_reduce", ["yq"], ["yv"], out=yv[:], in_=yq[:].rearrange("p (h e) -> p h e", h=6), axis=AX.X, op=ALU.add)
            P.op("act", "activation", ["yv"], ["yv"], out=yv[:], in_=yv[:], func=AF.Sqrt, scale=1.0 / 64, bias=1e-6)
            P.op("dve", "reciprocal", ["yv"], ["yv"], out=yv[:], in_=yv[:])
            P.op("dve", "tensor_tensor", ["yd", "yv"], ["yd"], out=yd[:].rearrange("p (h e) -> p h e", h=6),
                 in0=yd[:].rearrange("p (h e) -> p h e", h=6), in1=yv[:].unsqueeze(2).to_broadcast([128, 6, 64]), op=ALU.mult)
            P.op("dve", "tensor_tensor", ["yd", "gnwb"], ["yd"], out=yd[:], in0=yd[:], in1=gnwb[:], op=ALU.mult)
            P.op("act", "activation", [S("grm")], ["sg"], out=sg[:], in_=grm[b][:], func=AF.Silu)
            P.op("dve", "tensor_tensor", ["yd", "sg"], ["mixtok_b"], out=mixtok[:, 384:768], in0=yd[:], in1=sg[:], op=ALU.mult)
        if stages & 4:
            pp = ppd[b]
            P.op("dve", "tensor_tensor", [S("ppd")], ["a2"], out=a2[:], in0=pp[:, :, 0:143], in1=pp[:, :, 1:144], op=ALU.add)
            P.op("dve", "tensor_tensor", ["a2"], ["a4"], out=a4[:], in0=a2[:, :, 0:141], in1=a2[:, :, 2:143], op=ALU.add)
            P.op("dve", "tensor_tensor", ["a4"], ["a8"], out=a8[:], in0=a4[:, :, 0:137], in1=a4[:, :, 4:141], op=ALU.add)
            P.op("dve", "tensor_tensor", ["a8"], ["a16"], out=a16[:], in0=a8[:, :, 0:128], in1=a8[:, :, 8:136], op=ALU.add)
            if is_ctx:
                isl = 3 + (it - NT)
            elif it == 0:
                isl = 0
            elif it == NT - 1:
                isl = 2
            else:
                isl = 1
            srcs = [(slice(0, 64), 0, a2[0:64, 0, 7:135]), (slice(64, 128), 0, a4[64:128, 0, 6:134]),
                    (slice(0, 64), 1, a8[0:64, 1, 4:132]), (slice(64, 128), 1, a16[64:128, 1, :])]
            for pr, c, wap in srcs:
                P.op("dve", "tensor_tensor", ["a2", "a4", "a8", "a16", "invcs"], ["pdm"], out=pdm[pr, c, :], in0=wap,
                     in1=invcs[pr, isl, c, :], op=ALU.mult)
                P.op("dve", "tensor_tensor", ["pdm", S("ppd")], ["pdf"], out=pdf[pr, c, :], in0=pdm[pr, c, :],
                     in1=pp[pr, c, 8:136], op=ALU.subtract)
            for c in range(2):
                P.op("pe", "matmul", ["pdf", "wpb"], ["pmi_p"], out=pmi[:, 256 + c * 128:256 + (c + 1) * 128], lhsT=wpb[:, c, :], rhs=pdf[:, c, :], start=True, stop=True)
                P.op("act", "activation", ["pmi_p", "psc"], ["mixT_p"], out=mixT[:, 6 + c, :], in_=pmi[:, 256 + c * 128:256 + (c + 1) * 128],
                     func=AF.Identity, scale=psc[:, c:c + 1])
        for c in range(6):
            P.op("pe", "transpose", ["mixtok_a", "mixtok_b", "identb"], ["ptr"], out=ptr[:, c, :], in_=mixtok[:, c * 128:(c + 1) * 128], identity=identb[:])
        P.op("act", "copy", ["ptr"], ["mixT_t"], out=mixT[:, 0:6, :], in_=ptr[:, 0:6, :])
        for hf in range(2):
            for c in range(8):
                P.op("pe", "matmul", ["mixT_t", "mixT_p"] + WO, ["pout"], out=pout[:, hf, :], lhsT=mixT[:, c, :], rhs=wo[:, c, hf * 512:(hf + 1) * 512],
                     start=(c == 0), stop=(c == 7))
        gg = g1c if is_ctx else g1x
        P.op("dve", "tensor_tensor", ["pout", "g1x", "g1c"], ["otmp"], out=otmp[:].rearrange("p (a n) -> p a n", a=2), in0=pout[:],
             in1=gg[:].rearrange("p (a n) -> p a n", a=2), op=ALU.mult)
        P.op("dve", "tensor_tensor", ["otmp", S("xt")], ["otmp"], out=otmp[:], in0=otmp[:], in1=xt[b][:], op=ALU.add)
        P.dop("sp", reads=["otmp"], out=x1[t0:t0 + 128, :], in_=otmp[:])
    P.end_phase()


def phase_B2(P, IO, NT, NC, final):
    NTT = NT + NC
    P.begin_phase()
    x1 = IO["x1_src"]; mod = IO["d_mod"]; n2w = IO["n2w"]; fnw = IO["fnw"]; wq = IO["wq"]; keysT = IO["keysT"]
    pu = IO["pu"]; pv = IO["pv"]; iota16 = IO["iota16"]; ident_d = IO["ident"]; x2 = IO["x2_dst"]

    identf = P.sb("identf", [128, 128]); identb = P.sb("identb", [128, 128], BF16)
    wqs = [P.sb("wqs%d" % i, [128, 1024]) for i in range(2)]
    wqb = P.sb("wqb", [128, 8, 2048], BF16)
    kTf = P.sb("kTf", [128, 16, 128]); kTb = P.sb("kTb", [128, 16, 128], BF16)
    io16 = P.sb("io16", [128, 16])
    w2x = P.sb("w2x", [128, D]); sh2x = P.sb("sh2x", [128, D]); g2x = P.sb("g2x", [128, D])
    if NC > 0:
        w2c = P.sb("w2c", [128, D]); sh2c = P.sb("sh2c", [128, D]); g2c = P.sb("g2c", [128, D])
    n2b = P.sb("n2b", [128, D])
    if final:
        fnb = P.sb("fnb", [128, D])

    xt = [P.sb("xt%d" % i, [128, D]) for i in range(2)]
    junk = P.sb("junk", [128, D]); ssq = P.sb("ssq", [128, 1]); rstd = P.sb("rstd", [128, 1])
    h2 = P.sb("h2", [128, D]); h2b2 = [P.sb("h2b%d" % i, [128, D], BF16) for i in range(2)]
    h2T = P.sb("h2T", [128, 8, 128], BF16)
    qTs = P.sb("qTs", [128, 16, 128], BF16)
    ssb = P.sb("ssb", [128, 16, 128]); wk = P.sb("wk", [128, 16, 128])
    va = P.sb("va", [128, 16, 16]); ia = P.sb("ia", [128, 16, 16], U32); iaf = P.sb("iaf", [128, 16, 16])
    cand = P.sb("cand", [128, 8, 256])
    sc = P.sb("sc", [128, 8, 16]); ci = P.sb("ci", [128, 8, 16], U32)
    rk = P.sb("rk", [128, 8, 16], U32); ck = P.sb("ck", [128, 8, 16], U32)
    rkf = P.sb("rkf", [128, 8, 16]); ckf = P.sb("ckf", [128, 8, 16])
    oh = P.sb("oh", [128, 8, 16, 16])
    iak = P.sb("iak", [128, 8, 16]); ibk = P.sb("ibk", [128, 8, 16])
    idxf = P.sb("idxf", [128, 128]); idx2 = [P.sb("idx%d" % i, [128, 128], I32) for i in range(2)]
    ex = P.sb("ex", [128, 8, 16]); zs = P.sb("zs", [128, 8]); gate2 = [P.sb("gate%d" % i, [128, 128]) for i in range(2)]
    aa = P.sb("aa", [128, 128]); coef = P.sb("coef", [128, 128])
    ring = [P.sb("ring%d" % i, [128, D], BF16) for i in range(NRING)]
    otmp = P.sb("otmp", [128, D]); dg = [P.sb("dg%d" % i, [128, 128], BF16) for i in range(4)]
    xo = [P.sb("xo%d" % i, [128, D]) for i in range(2)]

    pT = P.ps("pT", [128, 8, 128], BF16)
    pq = P.ps("pq", [128, 16, 128])
    pacc = P.ps("pacc", [128, 2, 512])

    ld = lambda out, in_, w, r=(): P.dop("sp", reads=list(r), writes=[w], out=out, in_=in_)
    ld(identf[:], ident_d, "identf")
    P.op("dve", "tensor_copy", ["identf"], ["identb"], out=identb[:], in_=identf[:])
    ld(kTf[:], keysT, "kTf"); P.op("dve", "tensor_copy", ["kTf"], ["kTb"], out=kTb[:], in_=kTf[:])
    ld(io16[:], iota16, "io16")
    ld(n2b[:], n2w.partition_broadcast(128), "n2b")
    rows = [(0, w2x, sh2x, g2x, "x")]
    if NC > 0:
        rows.append((1, w2c, sh2c, g2c, "c"))
    for r, w2_, sh2_, g2_, nm in rows:
        ld(sh2_[:], mod[r:r + 1, 3 * D:4 * D].partition_broadcast(128), "sh2" + nm)
        ld(w2_[:], mod[r:r + 1, 4 * D:5 * D].partition_broadcast(128), "w2" + nm)
        ld(g2_[:], mod[r:r + 1, 5 * D:6 * D].partition_broadcast(128), "g2" + nm)
        P.op("dve", "scalar_tensor_tensor", ["w2" + nm, "n2b"], ["w2" + nm], out=w2_[:], in0=w2_[:], scalar=1.0, in1=n2b[:], op0=ALU.add, op1=ALU.mult)
    if final:
        ld(fnb[:], fnw.partition_broadcast(128), "fnb")
    for c in range(8):
        for hf in range(2):
            ld(wqs[hf][:], wq[c * 128:(c + 1) * 128, hf * 1024:(hf + 1) * 1024], "wqs%d" % hf)
            P.op("dve", "tensor_copy", ["wqs%d" % hf], ["wqb%d" % c], out=wqb[:, c, hf * 1024:(hf + 1) * 1024], in_=wqs[hf][:])
    WQ = ["wqb%d" % c for c in range(8)]

    def top16(vals_tok, vals, work, outv, outi, wtok):
        P.op("dve", "max", [vals_tok], [wtok + "v"], out=outv[:, 0:8], in_=vals)
        P.op("dve", "max_index", [vals_tok, wtok + "v"], [wtok + "i"], out=outi[:, 0:8], in_max=outv[:, 0:8], in_values=vals)
        P.op("dve", "match_replace", [vals_tok, wtok + "v"], [wtok + "w"], out=work, in_to_replace=outv[:, 0:8], in_values=vals, imm_value=-1e30)
        P.op("dve", "max", [wtok + "w"], [wtok + "v"], out=outv[:, 8:16], in_=work)
        P.op("dve", "max_index", [wtok + "w", wtok + "v"], [wtok + "i"], out=outi[:, 8:16], in_max=outv[:, 8:16], in_values=work)

    ring_i = 0
    def prologue1(it):
            b = it % 2
            is_ctx = it >= NT
            t0 = it * 128
            S = lambda n: "%s%d" % (n, b)
            w2_, sh2_, g2_, nm = (w2c, sh2c, g2c, "c") if is_ctx else (w2x, sh2x, g2x, "x")
            ld(xt[b][:], x1[t0:t0 + 128, :], S("xt"))
            P.op("act", "activation", [S("xt")], ["junk", "ssq"], out=junk[:], in_=xt[b][:], func=AF.Square, accum_out=ssq[:])
            P.op("act", "activation", ["ssq"], ["rstd"], out=rstd[:], in_=ssq[:], func=AF.Sqrt, scale=1.0 / D, bias=1e-6)
            P.op("dve", "reciprocal", ["rstd"], ["rstd"], out=rstd[:], in_=rstd[:])
            P.op("dve", "scalar_tensor_tensor", [S("xt"), "rstd", "w2" + nm], ["h2"], out=h2[:], in0=xt[b][:], scalar=rstd[:, 0:1], in1=w2_[:], op0=ALU.mult, op1=ALU.mult)
            P.op("dve", "tensor_tensor", ["h2", "sh2" + nm], ["h2"], out=h2[:], in0=h2[:], in1=sh2_[:], op=ALU.add)
            P.op("act", "copy", ["h2"], [S("h2b")], out=h2b2[b][:], in_=h2[:])
            for c in range(8):
                P.op("pe", "transpose", [S("h2b"), "identb"], ["pT"], out=pT[:, c, :], in_=h2b2[b][:, c * 128:(c + 1) * 128], identity=identb[:])
            P.op("act", "copy", ["pT"], ["h2T"], out=h2T[:], in_=pT[:])
            for n in range(16):
                for c in range(8):
                    P.op("pe", "matmul", ["h2T"] + WQ, ["pq"], out=pq[:, n, :], lhsT=wqb[:, c, n * 128:(n + 1) * 128], rhs=h2T[:, c, :], start=(c == 0), stop=(c == 7))
            P.op("act", "copy", ["pq"], ["qTs"], out=qTs[:], in_=pq[:])
            for n in range(16):
                P.op("pe", "matmul", ["qTs", "kTb"], ["pq"], out=pq[:, n, :], lhsT=qTs[:, n, :], rhs=kTb[:, n, :], start=True, stop=True)
            P.op("act", "copy", ["pq"], ["ssb"], out=ssb[:], in_=pq[:])

    def prologue2(it):
            b = it % 2
            is_ctx = it >= NT
            t0 = it * 128
            S = lambda n: "%s%d" % (n, b)
            w2_, sh2_, g2_, nm = (w2c, sh2c, g2c, "c") if is_ctx else (w2x, sh2x, g2x, "x")
            for g in range(16):
                top16("ssb", ssb[:, g, :], wk[:, g, :], va[:, g, :], ia[:, g, :], "t%d" % g)
            TV = ["t%dv" % g for g in range(16)]
            TI = ["t%di" % g for g in range(16)]
            va4 = va[:].rearrange("p (h s) r -> p h s r", s=2)
            P.op("dve", "tensor_tensor", TV + TI, ["cand"], out=cand[:].rearrange("p h (r c) -> p h r c", r=16),
                 in0=va4[:, :, 0, :].unsqueeze(3).to_broadcast([128, 8, 16, 16]),
                 in1=va4[:, :, 1, :].unsqueeze(2).to_broadcast([128, 8, 16, 16]), op=ALU.add)
            for h in range(8):
                top16("cand", cand[:, h, :], wk[:, 2 * h:2 * h + 2, :].rearrange("p a n -> p (a n)"), sc[:, h, :], ci[:, h, :], "c%d" % h)
            CV = ["c%dv" % h for h in range(8)]
            CI = ["c%di" % h for h in range(8)]
            P.op("dve", "tensor_single_scalar", CI, ["rk"], out=rk[:], in_=ci[:], scalar=4, op=ALU.logical_shift_right)
            P.op("dve", "tensor_single_scalar", CI, ["ck"], out=ck[:], in_=ci[:], scalar=15, op=ALU.bitwise_and)
            P.op("dve", "tensor_copy", ["rk"], ["rkf"], out=rkf[:], in_=rk[:])
            P.op("dve", "tensor_copy", ["ck"], ["ckf"], out=ckf[:], in_=ck[:])
            P.op("dve", "tensor_copy", TI, ["iaf"], out=iaf[:], in_=ia[:])
            iaf4 = iaf[:].rearrange("p (h s) r -> p h s r", s=2)
            io_b = io16[:].unsqueeze(1).unsqueeze(1).to_broadcast([128, 8, 16, 16])
            for side, (kf, ktok, dst, dtok) in enumerate([(rkf, "rkf", iak, "iak"), (ckf, "ckf", ibk, "ibk")]):
                P.op("dve", "tensor_tensor", [ktok, "io16"], ["oh"], out=oh[:], in0=io_b,
                     in1=kf[:].unsqueeze(3).to_broadcast([128, 8, 16, 16]), op=ALU.is_equal)
                P.op("dve", "tensor_tensor", ["oh", "iaf"], ["oh"], out=oh[:], in0=oh[:],
                     in1=iaf4[:, :, side, :].unsqueeze(2).to_broadcast([128, 8, 16, 16]), op=ALU.mult)
                P.op("dve", "tensor_reduce", ["oh"], [dtok], out=dst[:], in_=oh[:], axis=AX.X, op=ALU.add)
            P.op("dve", "scalar_tensor_tensor", ["iak", "ibk"], ["idxf"], out=idxf[:], in0=iak[:].rearrange("p h k -> p (h k)"), scalar=128.0,
                 in1=ibk[:].rearrange("p h k -> p (h k)"), op0=ALU.mult, op1=ALU.add)
            P.op("dve", "tensor_copy", ["idxf"], [S("idx")], out=idx2[b][:], in_=idxf[:])
            P.op("dve", "tensor_tensor", CV, ["ex"], out=ex[:], in0=sc[:], in1=sc[:, :, 0:1].to_broadcast([128, 8, 16]), op=ALU.subtract)
            P.op("act", "activation", ["ex"], ["ex"], out=ex[:], in_=ex[:], func=AF.Exp)
            P.op("dve", "tensor_reduce", ["ex"], ["zs"], out=zs[:], in_=ex[:], axis=AX.X, op=ALU.add)
            P.op("dve", "reciprocal", ["zs"], ["zs"], out=zs[:], in_=zs[:])
            P.op("dve", "tensor_tensor", ["ex", "zs"], [S("gate")], out=gate2[b][:].rearrange("p (h k) -> p h k", h=8), in0=ex[:],
                 in1=zs[:].unsqueeze(2).to_broadcast([128, 8, 16]), op=ALU.mult)


    def uphase(it):
            nonlocal ring_i
            b = it % 2
            is_ctx = it >= NT
            t0 = it * 128
            S = lambda n: "%s%d" % (n, b)
            w2_, sh2_, g2_, nm = (w2c, sh2c, g2c, "c") if is_ctx else (w2x, sh2x, g2x, "x")
            for kk in range(128):
                rg = ring[ring_i % NRING]; rtok = "ring%d" % (ring_i % NRING); ring_i += 1
                P.dop("pool", reads=[S("idx")], writes=[rtok], method="indirect_dma_start", out=rg[:], out_offset=None, in_=pu,
                      in_offset=bass.IndirectOffsetOnAxis(ap=idx2[b][:, kk:kk + 1], axis=0))
                P.op("dve", "tensor_tensor", [rtok, S("h2b")], [rtok], out=rg[:], in0=rg[:], in1=h2b2[b][:], op=ALU.mult)
                P.op("act", "activation", [rtok], [rtok, "aa%d" % kk], out=rg[:], in_=rg[:], func=AF.Identity, accum_out=aa[:, kk:kk + 1])
            P.op("act", "activation", ["aa%d" % kk for kk in range(128)], ["coef"], out=coef[:], in_=aa[:], func=AF.Gelu)
            P.op("dve", "tensor_tensor", ["coef", S("gate")], ["coef"], out=coef[:], in0=coef[:], in1=gate2[b][:], op=ALU.mult)

    def vphase(it):
            nonlocal ring_i
            b = it % 2
            is_ctx = it >= NT
            t0 = it * 128
            S = lambda n: "%s%d" % (n, b)
            w2_, sh2_, g2_, nm = (w2c, sh2c, g2c, "c") if is_ctx else (w2x, sh2x, g2x, "x")
            for kk in range(128):
                rg = ring[ring_i % NRING]; rtok = "ring%d" % (ring_i % NRING); ring_i += 1
                P.dop("pool", reads=[S("idx")], writes=[rtok], method="indirect_dma_start", out=rg[:], out_offset=None, in_=pv,
                      in_offset=bass.IndirectOffsetOnAxis(ap=idx2[b][:, kk:kk + 1], axis=0))
                dgi = kk % 4
                P.op("act", "activation", ["coef", "identb"], ["dg%d" % dgi], out=dg[dgi][:], in_=identb[:], func=AF.Identity, scale=coef[:, kk:kk + 1])
                for hf in range(2):
                    P.op("pe", "matmul", ["dg%d" % dgi, rtok], ["pacc"], out=pacc[:, hf, :], lhsT=dg[dgi][:], rhs=rg[:, hf * 512:(hf + 1) * 512],
                         start=(kk == 0), stop=(kk == 127))

    def epilogue(it):
            b = it % 2
            is_ctx = it >= NT
            t0 = it * 128
            S = lambda n: "%s%d" % (n, b)
            w2_, sh2_, g2_, nm = (w2c, sh2c, g2c, "c") if is_ctx else (w2x, sh2x, g2x, "x")
            P.op("dve", "tensor_tensor", ["pacc", "g2" + nm], ["otmp"], out=otmp[:].rearrange("p (a n) -> p a n", a=2), in0=pacc[:],
                 in1=g2_[:].rearrange("p (a n) -> p a n", a=2), op=ALU.mult)
            P.op("dve", "tensor_tensor", ["otmp", S("xt")], [S("xo")], out=xo[b][:], in0=otmp[:], in1=xt[b][:], op=ALU.add)
            if final and not is_ctx:
                P.op("act", "activation", [S("xo")], ["junk", "ssq"], out=junk[:], in_=xo[b][:], func=AF.Square, accum_out=ssq[:])
                P.op("act", "activation", ["ssq"], ["rstd"], out=rstd[:], in_=ssq[:], func=AF.Sqrt, scale=1.0 / D, bias=1e-6)
                P.op("dve", "reciprocal", ["rstd"], ["rstd"], out=rstd[:], in_=rstd[:])
                P.op("dve", "scalar_tensor_tensor", [S("xo"), "rstd", "fnb"], [S("xo")], out=xo[b][:], in0=xo[b][:], scalar=rstd[:, 0:1], in1=fnb[:],
                     op0=ALU.mult, op1=ALU.mult)
            P.dop("sp", reads=[S("xo")], out=x2[t0:t0 + 128, :], in_=xo[b][:])


    prologue1(0)
    prologue2(0)
    for it in range(NTT):
        uphase(it)
        if it + 1 < NTT:
            prologue1(it + 1)
        vphase(it)
        if it + 1 < NTT:
            prologue2(it + 1)
        epilogue(it)
    P.end_phase()


import numpy as np
import ml_dtypes

BF = ml_dtypes.bfloat16
D = 1024
GRID_W = 64
POOL_W = (2, 4, 8, 16)


def rope_tables(S):
    t = np.arange(S)
    row = (t // GRID_W).astype(np.float32)
    col = (t % GRID_W).astype(np.float32)
    inv = (10000.0 ** (-np.arange(16, dtype=np.float32) / 16)).astype(np.float32)
    ang = np.concatenate([row[:, None] * inv, col[:, None] * inv], axis=-1)
    return np.cos(ang).astype(np.float32), np.sin(ang).astype(np.float32)


def na_slot_tables(rpb, half, NT):
    NB = 2 * NT
    rows = 2 * NB
    locs = [0, 1, 2, NT - 2, NT - 1]
    G = np.zeros((128, 5, 6, 640), np.float32)
    M = np.zeros((128, 5, 640), np.float32)
    k = np.arange(128)[:, None, None]
    blk = np.arange(5)[None, :, None]
    q = np.arange(128)[None, None, :]
    for s, ml in enumerate(locs):
        m = half * NT + ml
        bs = int(np.clip(m - 2, 0, NB - 5))
        kr = 2 * (bs + blk) + k // 64
        ck = k % 64
        qr = 2 * m + q // 64
        cq = q % 64
        r_start = np.clip(qr - 4, 0, rows - 8)
        valid_r = (kr >= r_start) & (kr < r_start + 8)
        roff = np.clip(kr - qr + 7, 0, 14)
        c_start = np.clip(cq - 8, 0, GRID_W - 16)
        valid_c = (ck >= c_start) & (ck < c_start + 16)
        coff = np.clip(ck - cq + 15, 0, 30)
        valid = np.broadcast_to(valid_r & valid_c, (128, 5, 128))
        roff_b = np.broadcast_to(roff, (128, 5, 128))
        coff_b = np.broadcast_to(coff, (128, 5, 128))
        M[:, s, :] = valid.reshape(128, 640)
        for h in range(6):
            g = rpb[h][roff_b, coff_b]
            G[:, s, h, :] = np.where(valid, g, np.float32(0)).reshape(128, 640)
    return G, M


def window_start(m, NT):
    return int(np.clip(m - 2, 0, 2 * NT - 5))


def ret_consts():
    m = np.arange(128)[:, None]
    n = np.arange(128)[None, :]
    cst = np.zeros((128, 6, 128), np.float32)
    cst[:, 0] = np.maximum(n - m, 0)
    cst[:, 1] = 0.125 * (n >= m)
    cst[:, 2] = np.maximum(m - n, 0)
    cst[:, 3] = 0.125 * (m >= n)
    cst[:, 4] = np.broadcast_to(n + 1, (128, 128))
    cst[:, 5] = np.broadcast_to(128 - n, (128, 128))
    pm = np.stack([127 - np.arange(128), np.arange(128)], 1).astype(np.float32)
    return cst, pm


def inv_counts(half, NT):
    S = 2 * NT * 128
    out = np.zeros((128, 5, 2, 128), np.float32)
    specs = [(half * NT * 128, S), (half * NT * 128 + 128, S), (half * NT * 128 + (NT - 1) * 128, S), (0, 256), (128, 256)]
    for s, (tstart, Tseq) in enumerate(specs):
        t = tstart + np.arange(128)
        for c in range(2):
            for gi in range(2):
                w = POOL_W[2 * c + gi]
                lo = np.clip(t - w // 2, 0, Tseq)
                hi = np.clip(t + w // 2, 0, Tseq)
                out[gi * 64:(gi + 1) * 64, s, c, :] = (1.0 / (hi - lo).astype(np.float32))[None, :]
    return out


def pair_pack(st):
    a = st.reshape(64, 4, 3, 2, 64)
    return np.ascontiguousarray(a.transpose(3, 0, 1, 2, 4).reshape(128, 4, 3, 64))


def phase_C(P, tables):
    P.begin_phase()
    stg = [P.sb("cstg%d" % i, [128, 4096]) for i in range(2)]
    stb = [P.sb("cstb%d" % i, [128, 4096], BF16) for i in range(2)]
    n = 0
    for src, dst in tables:
        sv = src.rearrange("(p j) d -> p (j d)", p=128)
        dv = dst.rearrange("(p j) d -> p (j d)", p=128)
        for ch in range(32):
            i = n % 2
            n += 1
            P.dop("sp", writes=["cstg%d" % i], out=stg[i][:], in_=sv[:, ch * 4096:(ch + 1) * 4096])
            if i == 0:
                P.op("dve", "tensor_copy", ["cstg%d" % i], ["cstb%d" % i], out=stb[i][:], in_=stg[i][:])
            else:
                P.op("act", "copy", ["cstg%d" % i], ["cstb%d" % i], out=stb[i][:], in_=stg[i][:])
            P.dop("sp", reads=["cstb%d" % i], out=dv[:, ch * 4096:(ch + 1) * 4096], in_=stb[i][:])
    P.end_phase()


def build_fused(NT, ncores=8):
    NC = 2
    NTT = NT + NC
    P = Prog()
    di = lambda n, s, dt=F32: P.dram(n, s, dt, "ExternalInput")
    xcat = di("xcat", [NTT * 128, D]); cvT = di("cvT", [128, 8, 2])
    w_ada = di("w_ada", [2, D, 6 * D]); b_ada = di("b_ada", [2, 1, 6 * D]); n1w = di("n1w", [2, 8, 128]); w_in = di("w_in", [2, D, DPROJ])
    ropec = di("ropec", [NTT * 128, 32]); ropes = di("ropes", [NTT * 128, 32]); dec = di("dec", [2, 128, 12])
    posf = di("posf", [128, NTT]); posb = di("posb", [128, NTT]); ident = di("ident", [128, 128])
    G = di("G", [2, 128, 5, 6, 896]); M01 = di("M01", [128, 5, 896]); npow = di("npow", [128, 2]); sel = di("sel", [128, 4])
    dec_col = di("dec_col", [2, 128, 6]); cst = di("cst", [128, 6, 128]); pm = di("pm", [128, 2]); gnw = di("gnw", [2, 1, 384])
    invc = di("invc", [128, 5, 2, 128]); wpool = di("wpool", [2, 128, 2, 128]); pscale = di("pscale", [2, 128, 2]); w_out = di("w_out", [2, D, D])
    n2w = di("n2w", [2, 1, D]); fnw = di("fnw", [1, D]); wq = di("wq", [2, D, 2048]); keysT = di("keysT", [2, 128, 16, 128])
    pu = [di("pu%d" % l_, [16384, D]) for l_ in range(2)]; pv = [di("pv%d" % l_, [16384, D]) for l_ in range(2)]; iota16 = di("iota16", [128, 16])
    out = P.dram("out", [NT * 128, D], F32, "ExternalOutput")

    sc = {}
    def mk(name, shape, dt=F32):
        sc["t_" + name] = P.scratch("s_" + name, shape, dt)
        sc["d_" + name] = sc["t_" + name].ap()
    mk("mod", [2, 6 * D]); mk("qT", [128, NTT, 3, 128], BF16); mk("qrT", [128, NTT, 3, 128], BF16); mk("krT", [128, NTT, 3, 128], BF16)
    mk("kx", [128, 3, (NT + 4) * 128], BF16); mk("kTc", [128, 3, 256], BF16); mk("vx", [128, NT + 4, 6, 65], BF16); mk("vaugc", [128, 2, 6, 65], BF16)
    mk("kr", [128, NTT, 384], BF16); mk("vr", [128, NTT, 384], BF16); mk("gr", [128, NTT, 384]); mk("px", [128, 2, NT * 128 + 16]); mk("pxc", [128, 2, 272])
    mk("stpp", [128, 4, 3, 64]); mk("xb", [128, 3096], BF16); mk("xf", [128, 416]); mk("gb", [256, 3096], BF16); mk("gf", [256, 416])
    mk("x1", [NTT * 128, D]); mk("x2", [NTT * 128, D])
    for l_ in range(2):
        mk("pub%d" % l_, [16384, D], BF16); mk("pvb%d" % l_, [16384, D], BF16)
    phase_C(P, [(pu[0], sc["d_pub0"]), (pv[0], sc["d_pvb0"]), (pu[1], sc["d_pub1"]), (pv[1], sc["d_pvb1"])])

    for l in range(2):
        last = l == 1
        NCB = 0 if last else NC
        src = xcat if l == 0 else sc["d_x2"]
        IO = dict(sc)
        IO.update(x_src=src[0:NT * 128, :], ctx_src=src[NT * 128:NTT * 128, :], cvT=cvT, w_ada=w_ada[l], b_ada=b_ada[l], n1w=n1w[l], w_in=w_in[l],
                  ropec=ropec, ropes=ropes, dec=dec[l], posf=posf, posb=posb, ident=ident)
        phase_A(P, IO, NT, NC)
        IO = dict(sc); IO.update(sel=sel)
        phase_X(P, IO, NT, ncores)
        IO = dict(sc)
        IO.update(x_src=src, G=G[l], M01=M01, npow=npow, sel=sel, dec_row=dec[l], dec_col=dec_col[l], cst=cst, pm=pm, ident=ident, gnw=gnw[l],
                  invc=invc, wpool=wpool[l], pscale=pscale[l], w_out=w_out[l], x1_dst=sc["d_x1"])
        phase_B1(P, IO, NT, NCB)
        IO = dict(sc)
        IO.update(x1_src=sc["d_x1"], n2w=n2w[l], fnw=fnw, wq=wq[l], keysT=keysT[l], pu=sc["d_pub%d" % l], pv=sc["d_pvb%d" % l], iota16=iota16, ident=ident,
                  x2_dst=out if last else sc["d_x2"])
        phase_B2(P, IO, NT, NCB, last)
    P.es.close()
    return P.nc


def na_slot_tables_fused(rpb, half, NT):
    NB = 2 * NT
    rows = 2 * NB
    specs = [(0, 0, 7), (1, 0, 7), (2, 2, 5), (NT - 2, NT - 3, 7), (NT - 1, NT - 3, 7)]
    G = np.zeros((128, 5, 6, 896), np.float32)
    M = np.zeros((128, 5, 896), np.float32)
    k = np.arange(128)[:, None, None]
    q = np.arange(128)[None, None, :]
    for s, (ml, ext0, nb) in enumerate(specs):
        m = half * NT + ml
        e = np.arange(nb)[None, :, None]
        g = half * NT + ext0 + e - 2
        inseq = (g >= 0) & (g < NB)
        kr = 2 * g + k // 64
        ck = k % 64
        qr = 2 * m + q // 64
        cq = q % 64
        r_start = np.clip(qr - 4, 0, rows - 8)
        valid_r = (kr >= r_start) & (kr < r_start + 8) & inseq
        roff = np.clip(kr - qr + 7, 0, 14)
        c_start = np.clip(cq - 8, 0, GRID_W - 16)
        valid_c = (ck >= c_start) & (ck < c_start + 16)
        coff = np.clip(ck - cq + 15, 0, 30)
        valid = np.broadcast_to(valid_r & valid_c, (128, nb, 128))
        roff_b = np.broadcast_to(roff, (128, nb, 128))
        coff_b = np.broadcast_to(coff, (128, nb, 128))
        M[:, s, :nb * 128] = valid.reshape(128, nb * 128)
        for h in range(6):
            gg = rpb[h][roff_b, coff_b]
            G[:, s, h, :nb * 128] = np.where(valid, gg, np.float32(0)).reshape(128, nb * 128)
    return G, M


def fused_inputs(inp, B, NT):
    S = 2 * NT * 128
    NTT = NT + 2
    own_T = NT * 128
    ident = np.eye(128, dtype=np.float32)
    cos, sin = rope_tables(S)
    cst, pm = ret_consts()
    iota16 = np.broadcast_to(np.arange(16, dtype=np.float32), (128, 16)).copy()
    DEPTH = 2
    dec = np.stack([np.tile(np.concatenate([inp["ret_decay_fwd"][l], inp["ret_decay_bwd"][l]])[None, :], (128, 1)) for l in range(DEPTH)]).astype(np.float32)
    dec_col = np.zeros((DEPTH, 128, 6), np.float32)
    wpool = np.zeros((DEPTH, 128, 2, 128), np.float32)
    for l in range(DEPTH):
        for cc in range(3):
            for j in range(2):
                dec_col[l, j * 64:(j + 1) * 64, cc] = inp["ret_decay_fwd"][l][2 * cc + j]
                dec_col[l, j * 64:(j + 1) * 64, 3 + cc] = inp["ret_decay_bwd"][l][2 * cc + j]
        for cc in range(2):
            for gi in range(2):
                wpool[l, gi * 64:(gi + 1) * 64, cc, gi * 64:(gi + 1) * 64] = inp["pool_w"][l][2 * cc + gi]
    pscale = np.ascontiguousarray(inp["pool_scale"].reshape(DEPTH, 2, 128).transpose(0, 2, 1))
    keysT = np.ascontiguousarray(inp["peer_keys"].transpose(0, 4, 2, 1, 3).reshape(DEPTH, 128, 16, 128))
    shared = {
        "w_ada": inp["w_ada"], "b_ada": inp["b_ada"][:, None, :], "n1w": inp["norm1_w"].reshape(DEPTH, 8, 128), "w_in": inp["w_in"],
        "dec": dec, "ident": ident, "dec_col": dec_col, "cst": cst, "pm": pm, "gnw": inp["ret_gn_w"][:, None, :], "wpool": wpool,
        "pscale": pscale, "w_out": inp["w_out"], "n2w": inp["norm2_w"][:, None, :], "fnw": inp["final_norm_w"][None, :], "wq": inp["peer_wq"],
        "keysT": keysT, "pu0": inp["peer_u"][0], "pu1": inp["peer_u"][1], "pv0": inp["peer_v"][0], "pv1": inp["peer_v"][1], "iota16": iota16}
    t = np.arange(NTT * 128)
    own = t < own_T
    posf = np.where(own, own_T - 1 - t, 255 - (t - own_T)).astype(np.float32)
    posb = np.where(own, t, t - own_T).astype(np.float32)
    posf = np.ascontiguousarray(posf.reshape(NTT, 128).T); posb = np.ascontiguousarray(posb.reshape(NTT, 128).T)
    tabs = {}
    for half in range(2):
        GM = [na_slot_tables_fused(inp["na_rpb"][l], half, NT) for l in range(DEPTH)]
        tabs[half] = (np.stack([g for g, _ in GM]), GM[0][1], inv_counts(half, NT))
    maps = []
    for c in range(2 * B):
        b, half = c // 2, c % 2
        cv = np.stack([inp["c"][b], inp["c_ctx"]], 0)
        m = dict(shared)
        m["xcat"] = np.concatenate([inp["x"][b, half * own_T:(half + 1) * own_T], inp["ctx"][b]], 0)
        m["cvT"] = np.ascontiguousarray(cv.reshape(2, 8, 128).transpose(2, 1, 0))
        m["ropec"] = np.concatenate([cos[half * own_T:(half + 1) * own_T], np.ones((256, 32), np.float32)], 0)
        m["ropes"] = np.concatenate([sin[half * own_T:(half + 1) * own_T], np.zeros((256, 32), np.float32)], 0)
        m["posf"] = posf; m["posb"] = posb
        m["G"], m["M01"], m["invc"] = tabs[half]
        npow = np.zeros((128, 2), np.float32); npow[:, 0] = own_T if half == 1 else 0; npow[:, 1] = own_T if half == 0 else 0
        m["npow"] = npow
        selv = np.zeros((128, 4), np.float32)
        selv[:, 0] = half; selv[:, 1] = 1 - half; selv[:, 2] = half; selv[:, 3] = 1 - half
        m["sel"] = selv
        maps.append(m)
    return maps


from concourse.bass_utils import run_bass_kernel_spmd


def kernel(**inputs):
    inp = {k: np.asarray(v) for k, v in inputs.items()}
    B, S, _ = inp["x"].shape
    NT = S // 256
    maps = fused_inputs(inp, B, NT)
    nc = build_fused(NT, len(maps))
    res = run_bass_kernel_spmd(nc, maps, core_ids=list(range(len(maps)))).results
    out = np.zeros((B, S, D), np.float32)
    for c in range(len(maps)):
        out[c // 2, (c % 2) * NT * 128:(c % 2 + 1) * NT * 128] = np.asarray(res[c]["out"])
    return out
```

```python
D = 1024
DPROJ = 2944
NTOK_TM = 1920
NRING = 16
import math
import numpy as np
import ml_dtypes
from contextlib import ExitStack

import concourse.bass as bass
import concourse.mybir as mybir

F32 = mybir.dt.float32
BF16 = mybir.dt.bfloat16
U32 = mybir.dt.uint32
I32 = mybir.dt.int32
AF = mybir.ActivationFunctionType
ALU = mybir.AluOpType
AX = mybir.AxisListType

ENGS = ("pe", "dve", "act", "pool", "sp")
NDSEM = {"sp": 16, "pool": 20, "act": 8}


class Prog:
    def __init__(self, name="k"):
        self.nc = bass.Bass("TRN2", target_bir_lowering=False)
        self.es = ExitStack()
        self.stream = {e: [] for e in ENGS}
        self.cnt = {e: 0 for e in ENGS}
        self.sem = {}
        for e in ENGS:
            self.sem["E" + e] = self.es.enter_context(self.nc.semaphore("s_" + e))
        self.dsem_use = {}
        self.dsem_rr = {}
        for q, n in NDSEM.items():
            for j in range(n):
                key = "D%s%d" % (q, j)
                self.sem[key] = self.es.enter_context(self.nc.semaphore("d_%s%d" % (q, j)))
                self.dsem_use[key] = 0
            self.dsem_rr[q] = 0
        self.known = {e: {} for e in ENGS}
        self.targets = {"E" + e: set() for e in ENGS}
        self.pes = None
        self.nalloc = 0
        self.ncc = 0
        self.ccev = []
        self.rank = {}
        self.sigcount = {"E" + e: 0 for e in ENGS}
        self.emitted = {"E" + e: 0 for e in ENGS}
        self.last_w = {}
        self.readers = {}
        self.uid = 0

    def dram(self, name, shape, dtype, kind):
        return self.nc.dram_tensor(name, list(shape), dtype, kind=kind).ap()

    def sb(self, name, shape, dtype=F32):
        es = self.pes if self.pes is not None else self.es
        self.nalloc += 1
        return es.enter_context(self.nc.sbuf_tensor("%s_%d" % (name, self.nalloc), list(shape), dtype))

    def ps(self, name, shape, dtype=F32):
        es = self.pes if self.pes is not None else self.es
        self.nalloc += 1
        return es.enter_context(self.nc.psum_tensor("%s_%d" % (name, self.nalloc), list(shape), dtype))

    def scratch(self, name, shape, dtype=F32):
        return self.nc.dram_tensor(name, list(shape), dtype)

    def begin_phase(self):
        self.pes = ExitStack()
        self.last_w = {}
        self.readers = {}

    def cc(self, kind, groups, in_t, out_t, reads=(), writes=()):
        key = "CC%d" % self.ncc
        self.ncc += 1
        self.sem[key] = self.es.enter_context(self.nc.semaphore(key.lower()))
        deps = self._deps(reads, writes)
        waits = self._waits("pool", deps)
        fn = lambda e: e.collective_compute(kind, ALU.bypass, replica_groups=groups, ins=[in_t.ap().opt()], outs=[out_t.ap().opt()])
        self.stream["pool"].append((waits, fn, key, "cc"))
        ev = (key, 1)
        self.ccev.append(ev)
        self._commit(ev, reads, writes)
        return ev

    def end_phase(self):
        for e in ENGS:
            deps = {}
            for o in ENGS:
                if self.cnt[o] > 0 and o != "sp":
                    deps["E" + o] = self.cnt[o]
            for key, n in self.dsem_use.items():
                if n > 0:
                    deps[key] = 16 * n
            for key, v in self.ccev:
                deps[key] = v
            waits = self._waits(e, deps)
            if e == "pe" and self.cnt["pe"] > 0:
                self.known["pe"]["Epe"] = self.cnt["pe"]
            self.stream[e].append((waits, None, None, None))
        self._emit()
        if self.pes is not None:
            self.pes.close()
            self.pes = None

    def _deps(self, reads, writes):
        deps = {}

        def add(ev):
            if ev is None:
                return
            s, v = ev
            if deps.get(s, 0) < v:
                deps[s] = v

        for r in reads:
            add(self.last_w.get(r))
        for w in writes:
            add(self.last_w.get(w))
            for ev in self.readers.get(w, ()):
                add(ev)
        return deps

    def _commit(self, ev, reads, writes):
        for r in reads:
            self.readers.setdefault(r, []).append(ev)
        for w in writes:
            self.last_w[w] = ev
            self.readers[w] = []

    def _waits(self, eng, deps):
        out = []
        kn = self.known[eng]
        for s, v in deps.items():
            if eng == "pe" and s == "Epe":
                continue
            if kn.get(s, 0) >= v:
                continue
            kn[s] = v
            out.append((s, v))
            if s[0] == "E":
                self.targets[s].add(v)
        return out

    def ins(self, eng, fn, reads=(), writes=()):
        deps = self._deps(reads, writes)
        waits = self._waits(eng, deps)
        self.cnt[eng] += 1
        ev = ("E" + eng, self.cnt[eng])
        self.stream[eng].append((waits, fn, ev[0], self.cnt[eng]))
        self._commit(ev, reads, writes)
        return ev

    def op(self, eng, method, reads=(), writes=(), **kw):
        return self.ins(eng, lambda e: getattr(e, method)(**kw), reads, writes)

    def dop(self, q, reads=(), writes=(), method="dma_start", **kw):
        return self.dma(q, lambda e: getattr(e, method)(**kw), reads, writes)

    def dma(self, q, fn, reads=(), writes=()):
        deps = self._deps(reads, writes)
        j = self.dsem_rr[q]
        self.dsem_rr[q] = (j + 1) % NDSEM[q]
        key = "D%s%d" % (q, j)
        prev = self.dsem_use[key]
        if prev > 0:
            if deps.get(key, 0) < 16 * prev:
                deps[key] = 16 * prev
        waits = self._waits(q, deps)
        self.dsem_use[key] = prev + 1
        ev = (key, 16 * (prev + 1))
        self.stream[q].append((waits, fn, key, None))
        self._commit(ev, reads, writes)
        return ev

    def _emit(self):
        rank = self.rank
        for skey, tg in self.targets.items():
            new = sorted(i for i in tg if i > self.emitted[skey])
            assert all((skey, i) in rank for i in tg if i <= self.emitted[skey]), "wait on an already-emitted, unsignalled instruction"
            for i in new:
                self.sigcount[skey] += 1
                rank[(skey, i)] = self.sigcount[skey]
        nc = self.nc
        sem = self.sem
        stream = self.stream

        def wval(s, v):
            return rank[(s, v)] if s[0] == "E" else v

        with nc.Block() as block:
            def emit(e, handle):
                for waits, fn, skey, idx in stream[e]:
                    for s, v in waits:
                        handle.wait_ge(sem[s], wval(s, v))
                    if fn is None:
                        continue
                    ins = fn(handle)
                    if idx is None:
                        ins.then_inc(sem[skey], 16)
                    elif idx == "cc":
                        ins.then_inc(sem[skey])
                    elif (skey, idx) in rank:
                        ins.then_inc(sem[skey], 1)

            @block.tensor
            def _(h):
                emit("pe", h)

            @block.vector
            def _(h):
                emit("dve", h)

            @block.scalar
            def _(h):
                emit("act", h)

            @block.gpsimd
            def _(h):
                emit("pool", h)

            @block.sync
            def _(h):
                emit("sp", h)
        for e in ENGS:
            self.emitted["E" + e] = self.cnt[e]
            self.stream[e] = []

    def finish(self):
        self.end_phase()
        self.es.close()
        return self.nc


def phase_A(P, IO, NT, NC):
    NTT = NT + NC
    T = NTT * 128
    P.begin_phase()
    x = IO["x_src"]; ctx = IO["ctx_src"]; cvT = IO["cvT"]; w_ada = IO["w_ada"]; b_ada = IO["b_ada"]; n1w = IO["n1w"]
    w_in = IO["w_in"]; ropec = IO["ropec"]; ropes = IO["ropes"]; dec = IO["dec"]; posf = IO["posf"]; posb = IO["posb"]
    ident_d = IO["ident"]
    mod = IO["d_mod"]; o_qT = IO["d_qT"]; o_kx = IO["d_kx"]; o_kTc = IO["d_kTc"]; o_vx = IO["d_vx"]; o_vaugc = IO["d_vaugc"]
    o_qrT = IO["d_qrT"]; o_krT = IO["d_krT"]; o_kr = IO["d_kr"]; o_vr = IO["d_vr"]; o_gr = IO["d_gr"]
    o_px = IO["d_px"]; o_pxc = IO["d_pxc"]; o_stpp = IO["d_stpp"]; xb = IO["d_xb"]; xf = IO["d_xf"]
    dbg = 0

    identf = P.sb("identf", [128, 128], F32)
    identb = P.sb("identb", [128, 128], BF16)
    cvt = P.sb("cvt", [128, 8, 2], F32)
    scv = P.sb("scv", [128, 8, 2], F32)
    wada = [P.sb("wada%d" % i, [128, 8, 512], F32) for i in range(2)]
    bada = P.sb("bada", [2, 6 * D], F32)
    modrow = P.sb("modrow", [2, 6 * D], F32)
    colsrc = P.sb("colsrc", [40, 128], F32)
    cols = P.sb("cols", [128, 40], F32)
    w1x = P.sb("w1x", [128, 8], F32)
    w1c = P.sb("w1c", [128, 8], F32)
    wstg = [P.sb("wstg%d" % i, [128, DPROJ], F32) for i in range(2)]
    wb = P.sb("wb", [128, 8, DPROJ], BF16)
    dect = P.sb("dect", [128, 12], F32)
    lg = P.sb("lg", [128, 12], F32)
    tmp12 = P.sb("tmp12", [128, 12], F32)
    eps12 = P.sb("eps12", [128, 12], F32)
    posft = P.sb("posft", [128, 64], F32)
    posbt = P.sb("posbt", [128, 64], F32)

    xt = [P.sb("xt%d" % i, [128, D], F32) for i in range(2)]
    rc = [P.sb("rc%d" % i, [128, 32], F32) for i in range(2)]
    rs = [P.sb("rs%d" % i, [128, 32], F32) for i in range(2)]
    junk = P.sb("junk", [128, D], F32)
    ssq = P.sb("ssq", [128, 1], F32)
    rstd = P.sb("rstd", [128, 1], F32)
    xn = P.sb("xn", [128, D], BF16)
    hxT = P.sb("hxT", [128, 8, 128], BF16)
    tm = P.sb("tm", [128, NTOK_TM], F32)
    ra = P.sb("ra", [128, 384], F32)
    rb_ = P.sb("rb", [128, 384], F32)
    qk = P.sb("qk", [128, 768], F32)
    qkb = P.sb("qkb", [128, 768], BF16)
    wF = P.sb("wF", [128, 12], F32)
    kw = P.sb("kw", [128, 768], BF16)
    acc = P.sb("acc", [64, 4, 384], F32)

    s_qT = [P.sb("s_qT%d" % i, [128, 3, 128], BF16) for i in range(2)]
    s_kT = [P.sb("s_kT%d" % i, [128, 3, 128], BF16) for i in range(2)]
    s_v = [P.sb("s_v%d" % i, [128, 6, 65], BF16) for i in range(2)]
    s_qrT = [P.sb("s_qrT%d" % i, [128, 3, 128], BF16) for i in range(2)]
    s_krT = [P.sb("s_krT%d" % i, [128, 3, 128], BF16) for i in range(2)]
    s_vr = [P.sb("s_vr%d" % i, [128, 384], BF16) for i in range(2)]
    s_gr = [P.sb("s_gr%d" % i, [128, 384], F32) for i in range(2)]
    s_pT = [P.sb("s_pT%d" % i, [128, 2, 128], F32) for i in range(2)]

    pT = P.ps("pT", [128, 8, 128], BF16)
    pfm = P.ps("pfm", [128, 8, 128], F32)
    ptm = P.ps("ptm", [128, 2, 512], F32)
    pst = P.ps("pst", [64, 2, 512], F32)
    pmisc = P.ps("pmisc", [128, 512], F32)

    P.dma("sp", lambda e: e.dma_start(out=identf[:], in_=ident_d), writes=["identf"])
    P.dma("sp", lambda e: e.dma_start(out=cvt[:], in_=cvT), writes=["cvt"])
    P.dma("sp", lambda e: e.dma_start(out=bada[0:1, :], in_=b_ada), writes=["bada0"])
    P.dma("sp", lambda e: e.dma_start(out=bada[1:2, :], in_=b_ada), writes=["bada1"])
    P.dma("sp", lambda e: e.dma_start(out=dect[:], in_=dec), writes=["dect"])
    P.dma("sp", lambda e: e.dma_start(out=posft[:, 0:NTT], in_=posf), writes=["posft"])
    P.dma("sp", lambda e: e.dma_start(out=posbt[:, 0:NTT], in_=posb), writes=["posbt"])
    P.dma("sp", lambda e: e.dma_start(out=colsrc[32:40, :], in_=n1w), writes=["colsrc_n"])
    P.ins("dve", lambda e: e.tensor_copy(out=identb[:], in_=identf[:]), reads=["identf"], writes=["identb"])
    P.ins("act", lambda e: e.activation(out=scv[:], in_=cvt[:], func=AF.Silu), reads=["cvt"], writes=["scv"])
    P.ins("dve", lambda e: e.memset(acc[:], 0.0), writes=["acc"])
    for i_ in range(2):
        P.op("dve", "memset", [], ["s_v%d" % i_], ap=s_v[i_][:], constant=1.0)

    for nb in range(12):
        wa = wada[nb % 2]
        wtok = "wada%d" % (nb % 2)
        P.dma("sp", lambda e, wa=wa, nb=nb: e.dma_start(
            out=wa[:], in_=w_ada[:, nb * 512:(nb + 1) * 512].rearrange("(c p) n -> p c n", p=128)),
            writes=[wtok])
        for c in range(8):
            P.ins("pe", lambda e, wa=wa, c=c: e.matmul(out=pmisc[0:2, :], lhsT=scv[:, c, :], rhs=wa[:, c, :],
                                                       start=(c == 0), stop=(c == 7)),
                  reads=[wtok, "scv"], writes=["pmisc"])
        P.ins("dve", lambda e, nb=nb: e.tensor_tensor(out=modrow[:, nb * 512:(nb + 1) * 512], in0=pmisc[0:2, :],
                                                      in1=bada[:, nb * 512:(nb + 1) * 512], op=ALU.add),
              reads=["pmisc", "bada0", "bada1"], writes=["modrow"])
    P.dma("sp", lambda e: e.dma_start(out=mod, in_=modrow[:]), reads=["modrow"], writes=["mod_dram"])
    for v, (r, off) in enumerate([(0, 0), (0, D), (1, 0), (1, D)]):
        P.dma("sp", lambda e, v=v, r=r, off=off: e.dma_start(
            out=colsrc[v * 8:(v + 1) * 8, :],
            in_=mod[r:r + 1, off:off + D].rearrange("o (c p) -> (o c) p", p=128)),
            reads=["mod_dram"], writes=["colsrc%d" % v])
    P.ins("pe", lambda e: e.transpose(out=pmisc[:, 0:40], in_=colsrc[:, :], identity=identf[0:40, 0:40]),
          reads=["colsrc_n", "colsrc0", "colsrc1", "colsrc2", "colsrc3", "identf", "modrow"], writes=["pmisc"])
    P.ins("dve", lambda e: e.tensor_copy(out=cols[:], in_=pmisc[:, 0:40]), reads=["pmisc"], writes=["cols"])
    P.ins("dve", lambda e: e.scalar_tensor_tensor(out=w1x[:], in0=cols[:, 8:16], scalar=1.0, in1=cols[:, 32:40],
                                                  op0=ALU.add, op1=ALU.mult), reads=["cols"], writes=["w1x"])
    P.ins("dve", lambda e: e.scalar_tensor_tensor(out=w1c[:], in0=cols[:, 24:32], scalar=1.0, in1=cols[:, 32:40],
                                                  op0=ALU.add, op1=ALU.mult), reads=["cols"], writes=["w1c"])
    P.ins("act", lambda e: e.activation(out=eps12[:], in_=dect[:], func=AF.Exp, scale=-1.0), reads=["dect"], writes=["eps12"])
    P.ins("dve", lambda e: e.tensor_scalar(out=tmp12[:], in0=eps12[:], scalar1=-0.25, scalar2=1.0 / 3, op0=ALU.mult, op1=ALU.add),
          reads=["eps12"], writes=["tmp12"])
    P.ins("dve", lambda e: e.tensor_tensor(out=tmp12[:], in0=tmp12[:], in1=eps12[:], op=ALU.mult), reads=["tmp12", "eps12"], writes=["tmp12"])
    P.ins("dve", lambda e: e.tensor_scalar(out=tmp12[:], in0=tmp12[:], scalar1=-1.0, scalar2=0.5, op0=ALU.mult, op1=ALU.add),
          reads=["tmp12"], writes=["tmp12"])
    P.ins("dve", lambda e: e.tensor_tensor(out=tmp12[:], in0=tmp12[:], in1=eps12[:], op=ALU.mult), reads=["tmp12", "eps12"], writes=["tmp12"])
    P.ins("dve", lambda e: e.tensor_scalar(out=tmp12[:], in0=tmp12[:], scalar1=-1.0, scalar2=1.0, op0=ALU.mult, op1=ALU.add),
          reads=["tmp12"], writes=["tmp12"])
    P.ins("dve", lambda e: e.scalar_tensor_tensor(out=lg[:], in0=tmp12[:], scalar=-1.0, in1=eps12[:], op0=ALU.mult, op1=ALU.mult),
          reads=["tmp12", "eps12"], writes=["lg"])

    for c in range(8):
        ws = wstg[c % 2]
        wtok = "wstg%d" % (c % 2)
        P.dma("sp", lambda e, ws=ws, c=c: e.dma_start(out=ws[:], in_=w_in[c * 128:(c + 1) * 128, :]), writes=[wtok])
        eng = "dve"
        P.ins(eng, lambda e, ws=ws, c=c: e.tensor_copy(out=wb[:, c, :], in_=ws[:]), reads=[wtok], writes=["wb%d" % c])
    WB = ["wb%d" % c for c in range(8)]

    fm_cols = [0, 128, 256, 384, 512, 640, 2688, 2816]
    def loadsA(it):
        b = it % 2
        src = ctx[(it - NT) * 128:(it - NT + 1) * 128, :] if it >= NT else x[it * 128:(it + 1) * 128, :]
        t0 = it * 128
        P.dop("sp", writes=["xt%d" % b], out=xt[b][:], in_=src)
        P.dop("sp", writes=["rc%d" % b], out=rc[b][:], in_=ropec[t0:t0 + 128, :])
        P.dop("sp", writes=["rs%d" % b], out=rs[b][:], in_=ropes[t0:t0 + 128, :])

    loadsA(0)
    for it in range(NTT):
        b = it % 2
        is_ctx = it >= NT
        src = ctx[(it - NT) * 128:(it - NT + 1) * 128, :] if is_ctx else x[it * 128:(it + 1) * 128, :]
        w1 = w1c if is_ctx else w1x
        shc = 16 if is_ctx else 0
        t0 = it * 128
        X, RC, RS = xt[b], rc[b], rs[b]
        xtok, rctok, rstok = "xt%d" % b, "rc%d" % b, "rs%d" % b
        if it + 1 < NTT:
            loadsA(it + 1)
        P.ins("act", lambda e, X=X: e.activation(out=junk[:], in_=X[:], func=AF.Square, accum_out=ssq[:]),
              reads=[xtok], writes=["junk", "ssq"])
        P.ins("act", lambda e: e.activation(out=rstd[:], in_=ssq[:], func=AF.Sqrt, scale=1.0 / D, bias=1e-6),
              reads=["ssq"], writes=["rstd"])
        P.ins("dve", lambda e: e.reciprocal(out=rstd[:], in_=rstd[:]), reads=["rstd"], writes=["rstd"])
        P.ins("dve", lambda e, X=X: e.tensor_scalar(out=xn[:], in0=X[:], scalar1=rstd[:, 0:1], scalar2=None, op0=ALU.mult),
              reads=[xtok, "rstd"], writes=["xn"])
        for c in range(8):
            P.ins("pe", lambda e, c=c: e.transpose(out=pT[:, c, :], in_=xn[:, c * 128:(c + 1) * 128], identity=identb[:]),
                  reads=["xn", "identb"], writes=["pT"])
        for c in range(8):
            P.ins("act", lambda e, c=c, w1=w1, shc=shc: e.activation(
                out=hxT[:, c, :], in_=pT[:, c, :], func=AF.Identity,
                scale=w1[:, c:c + 1], bias=cols[:, shc + c:shc + c + 1]),
                reads=["pT", "w1x", "w1c", "cols"], writes=["hxT"])
        for g, col in enumerate(fm_cols):
            for c in range(8):
                P.ins("pe", lambda e, g=g, col=col, c=c: e.matmul(
                    out=pfm[:, g, :], lhsT=wb[:, c, col:col + 128], rhs=hxT[:, c, :], start=(c == 0), stop=(c == 7)),
                    reads=["hxT"] + WB, writes=["pfm"])
        P.ins("act", lambda e, b=b: e.copy(out=s_qT[b][:], in_=pfm[:, 0:3, :]), reads=["pfm"], writes=["s_qT%d" % b])
        P.ins("act", lambda e, b=b: e.copy(out=s_kT[b][:], in_=pfm[:, 3:6, :]), reads=["pfm"], writes=["s_kT%d" % b])
        P.ins("act", lambda e, b=b: e.copy(out=s_pT[b][:], in_=pfm[:, 6:8, :]), reads=["pfm"], writes=["s_pT%d" % b])
        P.dop("sp", reads=["s_qT%d" % b], out=o_qT[:, it], in_=s_qT[b][:])
        if is_ctx:
            P.dop("sp", reads=["s_kT%d" % b], out=o_kTc[:, :, (it - NT) * 128:(it - NT + 1) * 128], in_=s_kT[b][:])
        else:
            P.dop("sp", reads=["s_kT%d" % b], out=o_kx[:, :, (2 + it) * 128:(3 + it) * 128], in_=s_kT[b][:])
            if it < 2:
                P.dop("sp", reads=["s_kT%d" % b], out=xb[:, 0:768].rearrange("p (c t) -> p c t", c=3)[:, :, it * 128:(it + 1) * 128], in_=s_kT[b][:])
            if it >= NT - 2:
                j_ = it - (NT - 2)
                P.dop("sp", reads=["s_kT%d" % b], out=xb[:, 768:1536].rearrange("p (c t) -> p c t", c=3)[:, :, j_ * 128:(j_ + 1) * 128], in_=s_kT[b][:])
        if is_ctx:
            P.dop("sp", reads=["s_pT%d" % b], out=o_pxc[:, :, 8 + (it - NT) * 128:8 + (it - NT + 1) * 128], in_=s_pT[b][:])
        else:
            P.dop("sp", reads=["s_pT%d" % b], out=o_px[:, :, 8 + it * 128:8 + (it + 1) * 128], in_=s_pT[b][:])
            if it == 0:
                P.dop("sp", reads=["s_pT%d" % b], out=xf[:, 0:16].rearrange("p (c t) -> p c t", c=2), in_=s_pT[b][:, :, 0:8])
            if it == NT - 1:
                P.dop("sp", reads=["s_pT%d" % b], out=xf[:, 16:32].rearrange("p (c t) -> p c t", c=2), in_=s_pT[b][:, :, 120:128])
        for hf in range(2):
            for j in range(2):
                col = 768 + (hf * 2 + j) * 480
                for c in range(8):
                    P.ins("pe", lambda e, j=j, col=col, c=c: e.matmul(
                        out=ptm[:, j, 0:480], lhsT=hxT[:, c, :], rhs=wb[:, c, col:col + 480], start=(c == 0), stop=(c == 7)),
                        reads=["hxT"] + WB, writes=["ptm"])
            P.ins("act", lambda e, hf=hf: e.copy(
                out=tm[:, hf * 960:(hf + 1) * 960].rearrange("p (j n) -> p j n", j=2), in_=ptm[:, :, 0:480]),
                reads=["ptm"], writes=["tm"])
        P.ins("act", lambda e, b=b: e.copy(out=s_v[b][:, :, 0:64], in_=tm[:, 0:384].rearrange("p (h e) -> p h e", h=6)), reads=["tm"], writes=["s_v%d" % b])
        P.ins("act", lambda e, b=b: e.copy(out=s_vr[b][:], in_=tm[:, 1152:1536]), reads=["tm"], writes=["s_vr%d" % b])
        P.ins("act", lambda e, b=b: e.copy(out=s_gr[b][:], in_=tm[:, 1536:1920]), reads=["tm"], writes=["s_gr%d" % b])
        if is_ctx:
            P.dop("sp", reads=["s_v%d" % b], out=o_vaugc[:, it - NT], in_=s_v[b][:])
        else:
            P.dop("sp", reads=["s_v%d" % b], out=o_vx[:, 2 + it], in_=s_v[b][:])
            if it < 2:
                P.dop("sp", reads=["s_v%d" % b], out=xb[:, 1536:2316].rearrange("p (k h e) -> p k h e", k=2, h=6)[:, it], in_=s_v[b][:])
            if it >= NT - 2:
                P.dop("sp", reads=["s_v%d" % b], out=xb[:, 2316:3096].rearrange("p (k h e) -> p k h e", k=2, h=6)[:, it - (NT - 2)], in_=s_v[b][:])
        P.dop("sp", reads=["s_vr%d" % b], out=o_vr[:, it, :], in_=s_vr[b][:])
        P.dop("sp", reads=["s_gr%d" % b], out=o_gr[:, it, :], in_=s_gr[b][:])
        src5 = tm[:, 384:1152].rearrange("p (h f s i) -> p h f s i", h=12, f=2, s=2, i=16)
        dst5 = qk[:].rearrange("p (h f s i) -> p h f s i", h=12, f=2, s=2, i=16)
        ra4 = ra[:].rearrange("p (h f i) -> p h f i", h=12, f=2, i=16)
        rb4 = rb_[:].rearrange("p (h f i) -> p h f i", h=12, f=2, i=16)
        cosb = RC[:].rearrange("p (f i) -> p f i", f=2).unsqueeze(1).to_broadcast([128, 12, 2, 16])
        sinb = RS[:].rearrange("p (f i) -> p f i", f=2).unsqueeze(1).to_broadcast([128, 12, 2, 16])
        A_ = src5[:, :, :, 0, :]
        B_ = src5[:, :, :, 1, :]
        P.ins("dve", lambda e, A_=A_, cosb=cosb: e.tensor_tensor(out=ra4, in0=A_, in1=cosb, op=ALU.mult), reads=["tm", rctok], writes=["ra"])
        P.ins("dve", lambda e, B_=B_, sinb=sinb: e.tensor_tensor(out=rb4, in0=B_, in1=sinb, op=ALU.mult), reads=["tm", rstok], writes=["rb"])
        P.ins("dve", lambda e, dst5=dst5: e.tensor_tensor(out=dst5[:, :, :, 0, :], in0=ra4, in1=rb4, op=ALU.subtract), reads=["ra", "rb"], writes=["qk_a"])
        P.ins("dve", lambda e, A_=A_, sinb=sinb: e.tensor_tensor(out=ra4, in0=A_, in1=sinb, op=ALU.mult), reads=["tm", rstok], writes=["ra"])
        P.ins("dve", lambda e, B_=B_, cosb=cosb: e.tensor_tensor(out=rb4, in0=B_, in1=cosb, op=ALU.mult), reads=["tm", rctok], writes=["rb"])
        P.ins("dve", lambda e, dst5=dst5: e.tensor_tensor(out=dst5[:, :, :, 1, :], in0=ra4, in1=rb4, op=ALU.add), reads=["ra", "rb"], writes=["qk_b"])
        P.ins("act", lambda e: e.copy(out=qkb[:], in_=qk[:]), reads=["qk_a", "qk_b"], writes=["qkb"])
        P.dop("sp", reads=["qkb"], out=o_kr[:, it, :], in_=qkb[:, 384:768])
        for c in range(6):
            P.ins("pe", lambda e, c=c: e.transpose(out=pT[:, c, :], in_=qkb[:, c * 128:(c + 1) * 128], identity=identb[:]),
                  reads=["qkb", "identb"], writes=["pT"])
        P.ins("act", lambda e, b=b: e.copy(out=s_qrT[b][:], in_=pT[:, 0:3, :]), reads=["pT"], writes=["s_qrT%d" % b])
        P.ins("act", lambda e, b=b: e.copy(out=s_krT[b][:], in_=pT[:, 3:6, :]), reads=["pT"], writes=["s_krT%d" % b])
        P.dop("sp", reads=["s_qrT%d" % b], out=o_qrT[:, it], in_=s_qrT[b][:])
        P.dop("sp", reads=["s_krT%d" % b], out=o_krT[:, it], in_=s_krT[b][:])
        P.ins("act", lambda e, it=it: e.activation(out=wF[:, 0:6], in_=lg[:, 0:6], func=AF.Exp, scale=posft[:, it:it + 1],
                                                   bias=math.log(0.125)), reads=["lg", "posft"], writes=["wF0"])
        P.ins("act", lambda e, it=it: e.activation(out=wF[:, 6:12], in_=lg[:, 6:12], func=AF.Exp, scale=posbt[:, it:it + 1],
                                                   bias=math.log(0.125)), reads=["lg", "posbt"], writes=["wF1"])
        for d_ in range(2):
            P.ins("dve", lambda e, d_=d_: e.tensor_tensor(
                out=kw[:, d_ * 384:(d_ + 1) * 384].rearrange("p (h e) -> p h e", h=6),
                in0=qk[:, 384:768].rearrange("p (h e) -> p h e", h=6),
                in1=wF[:, d_ * 6:(d_ + 1) * 6].unsqueeze(2).to_broadcast([128, 6, 64]), op=ALU.mult),
                reads=["qk_a", "qk_b", "wF0", "wF1"], writes=["kw%d" % d_])
        for d_ in range(2):
            for h in range(6):
                P.ins("pe", lambda e, d_=d_, h=h, b=b: e.matmul(
                    out=pst[:, d_, h * 64:(h + 1) * 64], lhsT=kw[:, d_ * 384 + h * 64:d_ * 384 + (h + 1) * 64],
                    rhs=s_vr[b][:, h * 64:(h + 1) * 64], start=True, stop=True),
                    reads=["kw%d" % d_, "s_vr%d" % b], writes=["pst"])
        so = 2 if is_ctx else 0
        for d_ in range(2):
            P.ins("dve", lambda e, d_=d_, so=so: e.tensor_tensor(out=acc[:, so + d_, :], in0=acc[:, so + d_, :],
                                                                  in1=pst[:, d_, 0:384], op=ALU.add),
                  reads=["pst", "acc"], writes=["acc"])

    acc5 = acc[:].rearrange("d s (c j e) -> d s c j e", j=2, e=64)
    for j_ in range(2):
        P.dop("sp", reads=["acc"], out=o_stpp[j_ * 64:(j_ + 1) * 64], in_=acc5[:, :, :, j_, :])
        P.dop("sp", reads=["acc"], out=xf[j_ * 64:(j_ + 1) * 64, 32:416].rearrange("p (s c e) -> p s c e", s=2, c=3), in_=acc5[:, 0:2, :, j_, :])
    P.end_phase()


def phase_X(P, IO, NT, ncores=8):
    P.begin_phase()
    xb_t = IO["t_xb"]; xf_t = IO["t_xf"]; gb_t = IO["t_gb"]; gf_t = IO["t_gf"]
    gb = IO["d_gb"]; gf = IO["d_gf"]; kx = IO["d_kx"]; vx = IO["d_vx"]; px = IO["d_px"]; pxc = IO["d_pxc"]; sel = IO["sel"]
    groups = [[2 * i, 2 * i + 1] for i in range(ncores // 2)]
    P.cc("AllGather", groups, xb_t, gb_t, writes=["gb"])
    P.cc("AllGather", groups, xf_t, gf_t, writes=["gf"])
    kb = P.sb("kb", [128, 2, 768], BF16)
    vb = P.sb("vb", [128, 2, 780], BF16)
    hal = P.sb("hal", [128, 2, 16]); sels = P.sb("sels", [128, 4]); zer = P.sb("zer", [128, 16])
    P.dop("sp", writes=["sels"], out=sels[:], in_=sel)
    P.op("dve", "memset", [], ["zer"], ap=zer[:], constant=0.0)
    P.dop("sp", reads=["gb"], writes=["kb0"], out=kb[:, 0, :], in_=gb[0:128, 768:1536])
    P.dop("sp", reads=["gb"], writes=["kb1"], out=kb[:, 1, :], in_=gb[128:256, 0:768])
    P.dop("sp", reads=["gb"], writes=["vb0"], out=vb[:, 0, :], in_=gb[0:128, 2316:3096])
    P.dop("sp", reads=["gb"], writes=["vb1"], out=vb[:, 1, :], in_=gb[128:256, 1536:2316])
    P.dop("sp", reads=["kb0"], out=kx[:, :, 0:256], in_=kb[:, 0, :].rearrange("p (c t) -> p c t", c=3))
    P.dop("sp", reads=["kb1"], out=kx[:, :, (NT + 2) * 128:(NT + 4) * 128], in_=kb[:, 1, :].rearrange("p (c t) -> p c t", c=3))
    P.dop("sp", reads=["vb0"], out=vx[:, 0:2], in_=vb[:, 0, :].rearrange("p (k h e) -> p k h e", k=2, h=6))
    P.dop("sp", reads=["vb1"], out=vx[:, NT + 2:NT + 4], in_=vb[:, 1, :].rearrange("p (k h e) -> p k h e", k=2, h=6))
    P.dop("sp", reads=["gf"], writes=["hal0"], out=hal[:, 0, :], in_=gf[0:128, 16:32])
    P.dop("sp", reads=["gf"], writes=["hal1"], out=hal[:, 1, :], in_=gf[128:256, 0:16])
    P.op("dve", "tensor_scalar", ["hal0", "sels"], ["hal0"], out=hal[:, 0, :], in0=hal[:, 0, :], scalar1=sels[:, 0:1], scalar2=None, op0=ALU.mult)
    P.op("dve", "tensor_scalar", ["hal1", "sels"], ["hal1"], out=hal[:, 1, :], in0=hal[:, 1, :], scalar1=sels[:, 1:2], scalar2=None, op0=ALU.mult)
    P.dop("sp", reads=["hal0"], out=px[:, :, 0:8], in_=hal[:, 0, :].rearrange("p (c t) -> p c t", c=2))
    P.dop("sp", reads=["hal1"], out=px[:, :, 8 + NT * 128:16 + NT * 128], in_=hal[:, 1, :].rearrange("p (c t) -> p c t", c=2))
    P.dop("sp", reads=["zer"], out=pxc[:, :, 0:8], in_=zer[:].rearrange("p (c t) -> p c t", c=2))
    P.dop("sp", reads=["zer"], out=pxc[:, :, 264:272], in_=zer[:].rearrange("p (c t) -> p c t", c=2))
    P.end_phase()


def phase_B1(P, IO, NT, NC):
    NTT = NT + NC
    T = NTT * 128
    stages = 15
    P.begin_phase()
    x = IO["x_src"]; mod = IO["d_mod"]; qT = IO["d_qT"]; kx = IO["d_kx"]; vx = IO["d_vx"]; kTc = IO["d_kTc"]; vaugc = IO["d_vaugc"]
    G = IO["G"]; M01 = IO["M01"]; qrT = IO["d_qrT"]; krT = IO["d_krT"]; kr = IO["d_kr"]; vr = IO["d_vr"]; gr = IO["d_gr"]
    st_own = IO["d_stpp"]; gf = IO["d_gf"]; npow = IO["npow"]; sel = IO["sel"]; dec_row = IO["dec_row"]; dec_col = IO["dec_col"]
    cst = IO["cst"]; pm = IO["pm"]; ident_d = IO["ident"]; gnw = IO["gnw"]; px = IO["d_px"]; pxc = IO["d_pxc"]; invc = IO["invc"]
    wpool = IO["wpool"]; pscale = IO["pscale"]; w_out = IO["w_out"]; x1 = IO["x1_dst"]

    identf = P.sb("identf", [128, 128]); identb = P.sb("identb", [128, 128], BF16)
    wostg = [P.sb("wostg%d" % i, [128, 512]) for i in range(2)]
    wo = P.sb("wo", [128, 8, D], BF16)
    Gs = [P.sb("Gs%d" % i, [128, 896]) for i in range(2)]
    m01 = P.sb("m01", [128, 896])
    EB = P.sb("EB", [128, 5, 6, 896], BF16)
    kTc_s = P.sb("kTc_s", [128, 3, 256], BF16)
    vaugc_s = P.sb("vaugc_s", [128, 2, 6, 65], BF16)
    decr = P.sb("decr", [128, 12]); decc = P.sb("decc", [128, 6])
    lgr = P.sb("lgr", [128, 12]); lgc = P.sb("lgc", [128, 6])
    t12 = P.sb("t12", [128, 12]); e12 = P.sb("e12", [128, 12])
    cs = P.sb("cs", [128, 6, 128])
    pms = P.sb("pms", [128, 2])
    DTf = P.sb("DTf", [128, 6, 128]); DTb = P.sb("DTb", [128, 6, 128])
    XIf = P.sb("XIf", [128, 3, 128]); XIb = P.sb("XIb", [128, 3, 128])
    ZF = P.sb("ZF", [128, 6]); ZB = P.sb("ZB", [128, 6])
    cdrow = P.sb("cdrow", [128, 12])
    npw = P.sb("npw", [128, 2])
    sto = P.sb("sto", [128, 4, 3, 64]); stt = P.sb("stt", [128, 2, 3, 64]); sels = P.sb("sels", [128, 4]); hal = P.sb("hal", [128, 2, 16])
    scl = P.sb("scl", [128, 6])
    Rf = P.sb("Rf", [128, 3, 64]); Rb = P.sb("Rb", [128, 3, 64]); Rtmp = P.sb("Rtmp", [128, 3, 64])
    Rfb = P.sb("Rfb", [128, 3, 64], BF16)
    Rbs = P.sb("Rbs", [128, NTT, 3, 64], BF16)
    g1x = P.sb("g1x", [128, D]); g1c = P.sb("g1c", [128, D])
    gnwb = P.sb("gnwb", [128, 384])
    invcs = P.sb("invcs", [128, 5, 2, 128])
    wpf = P.sb("wpf", [128, 2, 128]); wpb = P.sb("wpb", [128, 2, 128], BF16)
    psc = P.sb("psc", [128, 2])

    xt = [P.sb("xt%d" % i, [128, D]) for i in range(2)]
    qTt = [P.sb("qTt%d" % i, [128, 3, 128], BF16) for i in range(2)]
    kwt = [P.sb("kwt%d" % i, [128, 3, 896], BF16) for i in range(2)]
    vwt = [P.sb("vwt%d" % i, [128, 7, 6, 65], BF16) for i in range(2)]
    qrt = [P.sb("qrt%d" % i, [128, 3, 128], BF16) for i in range(2)]
    krt = [P.sb("krt%d" % i, [128, 3, 128], BF16) for i in range(2)]
    krm = [P.sb("krm%d" % i, [128, 384], BF16) for i in range(2)]
    vrm = [P.sb("vrm%d" % i, [128, 384], BF16) for i in range(2)]
    grm = [P.sb("grm%d" % i, [128, 384]) for i in range(2)]
    ppd = [P.sb("ppd%d" % i, [128, 2, 144]) for i in range(2)]
    krm2 = [P.sb("krm2%d" % i, [128, 384], BF16) for i in range(2)]
    vrm2 = [P.sb("vrm2%d" % i, [128, 384], BF16) for i in range(2)]
    kz = P.sb("kz", [128, 384], BF16)
    pexp = P.sb("pexp", [128, 9, 128], BF16)
    rcp = P.sb("rcp", [128, 6])
    mixtok = P.sb("mixtok", [128, 768], BF16)
    mixT = P.sb("mixT", [128, 8, 128], BF16)
    SDf = P.sb("SDf", [128, 6, 128], BF16); SDb = P.sb("SDb", [128, 6, 128], BF16)
    qxf = P.sb("qxf", [128, 3, 128], BF16); qxb = P.sb("qxb", [128, 3, 128], BF16)
    ysum = P.sb("ysum", [128, 6]); yd = P.sb("yd", [128, 384]); yq = P.sb("yq", [128, 384])
    yv = P.sb("yv", [128, 6]); sg = P.sb("sg", [128, 384])
    a2 = P.sb("a2", [128, 2, 143]); a4 = P.sb("a4", [128, 2, 141]); a8 = P.sb("a8", [128, 2, 137]); a16 = P.sb("a16", [128, 2, 128])
    pdm = P.sb("pdm", [128, 2, 128])
    pdf = P.sb("pdf", [128, 2, 128], BF16)
    otmp = P.sb("otmp", [128, D])

    pS = P.ps("pS", [128, 8, 128])
    po = P.ps("po", [128, 6, 65])
    py = P.ps("py", [128, 6, 64])
    pmi = P.ps("pmi", [128, 512])
    ptr = P.ps("ptr", [128, 8, 128], BF16)
    pout = P.ps("pout", [128, 2, 512])

    ld = lambda out, in_, w, r=(): P.dop("sp", reads=list(r), writes=[w], out=out, in_=in_)

    if stages != 15:
        P.op("dve", "memset", [], ["mixtok_a", "mixtok_b"], ap=mixtok[:], constant=0.0)
        P.op("dve", "memset", [], ["mixT_p", "mixT_t"], ap=mixT[:], constant=0.0)
        for it_ in range(NTT):
            P.op("dve", "memset", [], ["Rbs%d" % it_], ap=Rbs[:, it_, :, :], constant=0.0)
    ld(identf[:], ident_d, "identf")
    P.op("dve", "tensor_copy", ["identf"], ["identb"], out=identb[:], in_=identf[:])
    ld(kTc_s[:], kTc, "kTc_s"); ld(vaugc_s[:], vaugc, "vaugc_s")
    ld(decr[:], dec_row, "decr"); ld(decc[:], dec_col, "decc"); ld(cs[:], cst, "cs"); ld(pms[:], pm, "pms")
    ld(npw[:], npow, "npw"); ld(sto[:], st_own, "sto"); ld(sels[:], sel, "sels")
    ld(stt[:, 0], gf[0:128, 32:224].rearrange("p (c e) -> p c e", c=3), "stt0")
    ld(stt[:, 1], gf[128:256, 224:416].rearrange("p (c e) -> p c e", c=3), "stt1")
    ld(g1x[:], mod[0:1, 2 * D:3 * D].partition_broadcast(128), "g1x")
    ld(g1c[:], mod[1:2, 2 * D:3 * D].partition_broadcast(128), "g1c")
    ld(gnwb[:], gnw.partition_broadcast(128), "gnwb")
    ld(invcs[:], invc, "invcs"); ld(wpf[:], wpool, "wpf"); ld(psc[:], pscale, "psc")
    P.op("dve", "tensor_copy", ["wpf"], ["wpb"], out=wpb[:], in_=wpf[:])
    for c in range(8):
        for hf in range(2):
            ld(wostg[hf][:], w_out[c * 128:(c + 1) * 128, hf * 512:(hf + 1) * 512], "wostg%d" % hf)
            P.op("dve" if hf == 0 else "act", "tensor_copy" if hf == 0 else "copy", ["wostg%d" % hf], ["wo%d" % c], out=wo[:, c, hf * 512:(hf + 1) * 512], in_=wostg[hf][:])
    WO = ["wo%d" % c for c in range(8)]
    for s in range(5):
        ld(m01[:], M01[:, s, :], "m01")
        for h in range(6):
            i = (s * 6 + h) % 2
            ld(Gs[i][:], G[:, s, h, :], "Gs%d" % i)
            P.op("act", "activation", ["Gs%d" % i], ["Gs%d" % i], out=Gs[i][:], in_=Gs[i][:], func=AF.Exp)
            P.op("dve", "tensor_tensor", ["Gs%d" % i, "m01"], ["EB"], out=EB[:, s, h, :], in0=Gs[i][:], in1=m01[:], op=ALU.mult)

    def logsig(dst, src, n, stok, dtok):
        P.op("act", "activation", [stok], ["e12"], out=e12[:, 0:n], in_=src, func=AF.Exp, scale=-1.0)
        P.op("dve", "tensor_scalar", ["e12"], ["t12"], out=t12[:, 0:n], in0=e12[:, 0:n], scalar1=-0.25, scalar2=1.0 / 3, op0=ALU.mult, op1=ALU.add)
        P.op("dve", "tensor_tensor", ["t12", "e12"], ["t12"], out=t12[:, 0:n], in0=t12[:, 0:n], in1=e12[:, 0:n], op=ALU.mult)
        P.op("dve", "tensor_scalar", ["t12"], ["t12"], out=t12[:, 0:n], in0=t12[:, 0:n], scalar1=-1.0, scalar2=0.5, op0=ALU.mult, op1=ALU.add)
        P.op("dve", "tensor_tensor", ["t12", "e12"], ["t12"], out=t12[:, 0:n], in0=t12[:, 0:n], in1=e12[:, 0:n], op=ALU.mult)
        P.op("dve", "tensor_scalar", ["t12"], ["t12"], out=t12[:, 0:n], in0=t12[:, 0:n], scalar1=-1.0, scalar2=1.0, op0=ALU.mult, op1=ALU.add)
        P.op("dve", "scalar_tensor_tensor", ["t12", "e12"], [dtok], out=dst, in0=t12[:, 0:n], scalar=-1.0, in1=e12[:, 0:n], op0=ALU.mult, op1=ALU.mult)
    logsig(lgr[:], decr[:], 12, "decr", "lgr")
    logsig(lgc[:], decc[:], 6, "decc", "lgc")
    sidx = lambda h: (h % 2) * 3 + h // 2
    for h in range(6):
        P.op("act", "activation", ["lgr", "cs"], ["DTf%d" % h], out=DTf[:, sidx(h), :], in_=cs[:, 0, :], func=AF.Exp, scale=lgr[:, h:h + 1])
        P.op("dve", "tensor_tensor", ["DTf%d" % h, "cs"], ["DTf%d" % h], out=DTf[:, sidx(h), :], in0=DTf[:, sidx(h), :], in1=cs[:, 1, :], op=ALU.mult)
        P.op("act", "activation", ["lgr", "cs"], ["DTb%d" % h], out=DTb[:, sidx(h), :], in_=cs[:, 2, :], func=AF.Exp, scale=lgr[:, 6 + h:7 + h])
        P.op("dve", "tensor_tensor", ["DTb%d" % h, "cs"], ["DTb%d" % h], out=DTb[:, sidx(h), :], in0=DTb[:, sidx(h), :], in1=cs[:, 3, :], op=ALU.mult)
    DT = ["DTf%d" % h for h in range(6)] + ["DTb%d" % h for h in range(6)]
    for c in range(3):
        P.op("act", "activation", ["lgc", "cs"], ["XI"], out=XIf[:, c, :], in_=cs[:, 4, :], func=AF.Exp, scale=lgc[:, c:c + 1])
        P.op("act", "activation", ["lgc", "cs"], ["XI"], out=XIb[:, c, :], in_=cs[:, 5, :], func=AF.Exp, scale=lgc[:, 3 + c:4 + c])
    P.op("act", "activation", ["lgr", "pms"], ["ZF"], out=ZF[:], in_=lgr[:, 0:6], func=AF.Exp, scale=pms[:, 0:1], bias=math.log(0.125))
    P.op("act", "activation", ["lgr", "pms"], ["ZB"], out=ZB[:], in_=lgr[:, 6:12], func=AF.Exp, scale=pms[:, 1:2], bias=math.log(0.125))
    P.op("act", "activation", ["lgr"], ["cdrow"], out=cdrow[:], in_=lgr[:], func=AF.Exp, scale=128.0)
    P.op("act", "activation", ["lgc", "npw"], ["scl"], out=scl[:, 0:3], in_=lgc[:, 0:3], func=AF.Exp, scale=npw[:, 0:1])
    P.op("act", "activation", ["lgc", "npw"], ["scl"], out=scl[:, 3:6], in_=lgc[:, 3:6], func=AF.Exp, scale=npw[:, 1:2])
    P.op("dve", "tensor_tensor", ["sto", "scl"], ["Rf"], out=Rf[:], in0=sto[:, 2, :, :], in1=scl[:, 0:3].unsqueeze(2).to_broadcast([128, 3, 64]), op=ALU.mult)
    P.op("dve", "scalar_tensor_tensor", ["Rf", "stt0", "sels"], ["Rf"], out=Rf[:].rearrange("p c e -> p (c e)"), in0=stt[:, 0, :, :].rearrange("p c e -> p (c e)"), scalar=sels[:, 2:3], in1=Rf[:].rearrange("p c e -> p (c e)"), op0=ALU.mult, op1=ALU.add)
    P.op("dve", "tensor_tensor", ["sto", "scl"], ["Rb"], out=Rb[:], in0=sto[:, 3, :, :], in1=scl[:, 3:6].unsqueeze(2).to_broadcast([128, 3, 64]), op=ALU.mult)
    P.op("dve", "scalar_tensor_tensor", ["Rb", "stt1", "sels"], ["Rb"], out=Rb[:].rearrange("p c e -> p (c e)"), in0=stt[:, 1, :, :].rearrange("p c e -> p (c e)"), scalar=sels[:, 3:4], in1=Rb[:].rearrange("p c e -> p (c e)"), op0=ALU.mult, op1=ALU.add)

    cdc = P.sb("cdc", [128, 6])
    P.op("act", "activation", ["lgc"], ["cdc"], out=cdc[:], in_=lgc[:], func=AF.Exp, scale=128.0)

    def state_update(Rm, KZ, V, vtok, dirn, it):
        for h in range(6):
            c, j = h // 2, h % 2
            P.op("pe", "matmul", ["kz", vtok], ["pmi_s"], out=pmi[j * 64:(j + 1) * 64, c * 64:(c + 1) * 64],
                 lhsT=KZ[:, h * 64:(h + 1) * 64], rhs=V[:, h * 64:(h + 1) * 64], start=True, stop=True)
        rtok = "Rf" if dirn == 0 else "Rb"
        P.op("dve", "tensor_tensor", [rtok, "cdc"], ["Rtmp"], out=Rtmp[:], in0=Rm[:],
             in1=cdc[:, dirn * 3:dirn * 3 + 3].unsqueeze(2).to_broadcast([128, 3, 64]), op=ALU.mult)
        P.op("dve", "tensor_tensor", ["Rtmp", "pmi_s"], [rtok], out=Rm[:], in0=Rtmp[:],
             in1=pmi[:, 0:192].rearrange("p (c e) -> p c e", c=3), op=ALU.add)

    order = list(range(NT - 1, -1, -1)) + list(range(NTT - 1, NT - 1, -1))
    for i, it in enumerate(order if stages & 8 else []):
        b = i % 2
        if it == NTT - 1 and NC > 0:
            P.op("dve", "memset", [], ["Rb"], ap=Rb[:], constant=0.0)
        P.op("act", "copy", ["Rb"], ["Rbs%d" % it], out=Rbs[:, it, :, :], in_=Rb[:])
        last = (it == 0) or (it == NT)
        if last:
            continue
        ld(krm2[b][:], kr[:, it, :], "krm2%d" % b); ld(vrm2[b][:], vr[:, it, :], "vrm2%d" % b)
        P.op("dve", "tensor_tensor", ["krm2%d" % b, "ZB"], ["kz"], out=kz[:].rearrange("p (h e) -> p h e", h=6),
             in0=krm2[b][:].rearrange("p (h e) -> p h e", h=6), in1=ZB[:].unsqueeze(2).to_broadcast([128, 6, 64]), op=ALU.mult)
        state_update(Rb, kz, vrm2[b], "vrm2%d" % b, 1, it)

    def loads(it):
        b = it % 2
        is_ctx = it >= NT
        t0 = it * 128
        S = lambda n: "%s%d" % (n, b)
        ld(xt[b][:], x[t0:t0 + 128, :], S("xt"))
        ld(qTt[b][:], qT[:, it], S("qTt"))
        special = (not is_ctx) and (it < 2 or it >= NT - 2)
        nwb = 7 if special else 5
        ext0 = (0 if it < 2 else NT - 3) if special else it
        if not is_ctx:
            ld(kwt[b][:, :, 0:nwb * 128], kx[:, :, ext0 * 128:(ext0 + nwb) * 128], S("kwt")); ld(vwt[b][:, 0:nwb], vx[:, ext0:ext0 + nwb], S("vwt"))
        ld(qrt[b][:], qrT[:, it], S("qrt")); ld(krt[b][:], krT[:, it], S("krt"))
        ld(krm[b][:], kr[:, it, :], S("krm")); ld(vrm[b][:], vr[:, it, :], S("vrm")); ld(grm[b][:], gr[:, it, :], S("grm"))
        if is_ctx:
            ld(ppd[b][:], pxc[:, :, (it - NT) * 128:(it - NT) * 128 + 144], S("ppd"))
        else:
            ld(ppd[b][:], px[:, :, it * 128:it * 128 + 144], S("ppd"))

    loads(0)
    for it in range(NTT):
        b = it % 2
        is_ctx = it >= NT
        t0 = it * 128
        S = lambda n: "%s%d" % (n, b)
        special = (not is_ctx) and (it < 2 or it >= NT - 2)
        nwb = 7 if special else 5
        ext0 = (0 if it < 2 else NT - 3) if special else it
        if it + 1 < NTT:
            loads(it + 1)
        if stages & 1:
            if is_ctx:
                slot = None
            elif it == 0:
                slot = 0
            elif it == 1:
                slot = 1
            elif it == NT - 2:
                slot = 3
            elif it == NT - 1:
                slot = 4
            else:
                slot = 2
            for h in range(6):
                c, j = h // 2, h % 2
                pr = slice(j * 64, (j + 1) * 64)
                blocks = []
                if not is_ctx:
                    for bl in range(nwb):
                        blocks.append((kwt[b][pr, c, bl * 128:(bl + 1) * 128], vwt[b][:, bl, h, :], [S("kwt")], [S("vwt")]))
                for bl in range(2):
                    blocks.append((kTc_s[pr, c, bl * 128:(bl + 1) * 128], vaugc_s[:, bl, h, :], ["kTc_s"], ["vaugc_s"]))
                nb = len(blocks)
                for g0 in range(0, nb, 8):
                    g1 = min(nb, g0 + 8)
                    for bi in range(g0, g1):
                        kap, vap, kt_, vt_ = blocks[bi]
                        P.op("pe", "matmul", kt_ + [S("qTt")], ["pS"], out=pS[:, bi - g0, :], lhsT=kap, rhs=qTt[b][pr, c, :], start=True, stop=True)
                    P.op("act", "activation", ["pS"], ["pexp"], out=pexp[:, g0:g1, :], in_=pS[:, 0:g1 - g0, :], func=AF.Exp, scale=0.125)
                if not is_ctx:
                    P.op("dve", "tensor_tensor", ["pexp", "EB"], ["pexp"], out=pexp[:, 0:nwb, :], in0=pexp[:, 0:nwb, :],
                         in1=EB[:, slot, h, 0:nwb * 128].rearrange("p (k q) -> p k q", k=nwb), op=ALU.mult)
                for bi, (kap, vap, kt_, vt_) in enumerate(blocks):
                    P.op("pe", "matmul", vt_ + ["pexp"], ["po"], out=po[:, h, :], lhsT=pexp[:, bi, :], rhs=vap, start=(bi == 0), stop=(bi == nb - 1))
            P.op("dve", "reciprocal", ["po"], ["rcp"], out=rcp[:], in_=po[:, :, 64])
            P.op("dve", "tensor_tensor", ["po", "rcp"], ["mixtok_a"], out=mixtok[:, 0:384].rearrange("p (h e) -> p h e", h=6),
                 in0=po[:, :, 0:64], in1=rcp[:].unsqueeze(2).to_broadcast([128, 6, 64]), op=ALU.mult)
        if stages & 2:
            if it == NT and NC > 0:
                P.op("dve", "memset", [], ["Rf"], ap=Rf[:], constant=0.0)
            P.op("act", "copy", ["Rf"], ["Rfb"], out=Rfb[:], in_=Rf[:])
            for h in range(6):
                c, j = h // 2, h % 2
                pr = slice(j * 64, (j + 1) * 64)
                P.op("pe", "matmul", [S("krt"), S("qrt")], ["pS"], out=pS[:, j * 4 + c, :], lhsT=krt[b][pr, c, :], rhs=qrt[b][pr, c, :], start=True, stop=True)
            for j_ in range(2):
                P.op("dve", "tensor_tensor", ["pS"] + DT, ["SDf"], out=SDf[:, j_ * 3:j_ * 3 + 3, :], in0=pS[:, j_ * 4:j_ * 4 + 3, :], in1=DTf[:, j_ * 3:j_ * 3 + 3, :], op=ALU.mult)
                P.op("dve", "tensor_tensor", ["pS"] + DT, ["SDb"], out=SDb[:, j_ * 3:j_ * 3 + 3, :], in0=pS[:, j_ * 4:j_ * 4 + 3, :], in1=DTb[:, j_ * 3:j_ * 3 + 3, :], op=ALU.mult)
            P.op("dve", "tensor_tensor", [S("qrt"), "XI"], ["qxf"], out=qxf[:], in0=qrt[b][:], in1=XIf[:], op=ALU.mult)
            P.op("dve", "tensor_tensor", [S("qrt"), "XI"], ["qxb"], out=qxb[:], in0=qrt[b][:], in1=XIb[:], op=ALU.mult)
            for h in range(6):
                c, j = h // 2, h % 2
                pr = slice(j * 64, (j + 1) * 64)
                vh = vrm[b][:, h * 64:(h + 1) * 64]
                P.op("pe", "matmul", ["SDf", S("vrm")], ["py"], out=py[:, h, :], lhsT=SDf[:, sidx(h), :], rhs=vh, start=True, stop=False)
                P.op("pe", "matmul", ["SDb", S("vrm")], ["py"], out=py[:, h, :], lhsT=SDb[:, sidx(h), :], rhs=vh, start=False, stop=False)
                P.op("pe", "matmul", ["qxf", "Rfb"], ["py"], out=py[:, h, :], lhsT=qxf[pr, c, :], rhs=Rfb[pr, c, :], start=False, stop=False)
                P.op("pe", "matmul", ["qxb", "Rbs%d" % it], ["py"], out=py[:, h, :], lhsT=qxb[pr, c, :], rhs=Rbs[pr, it, c, :], start=False, stop=True)
            if it != NT - 1 and it != NTT - 1:
                P.op("dve", "tensor_tensor", [S("krm"), "ZF"], ["kz"], out=kz[:].rearrange("p (h e) -> p h e", h=6),
                     in0=krm[b][:].rearrange("p (h e) -> p h e", h=6), in1=ZF[:].unsqueeze(2).to_broadcast([128, 6, 64]), op=ALU.mult)
                state_update(Rf, kz, vrm[b], S("vrm"), 0, it)
            y3 = py[:, :, :]
            P.op("dve", "tensor_reduce", ["py"], ["ysum"], out=ysum[:], in_=y3, axis=AX.X, op=ALU.add)
            P.op("dve", "tensor_scalar", ["ysum"], ["ysum"], out=ysum[:], in0=ysum[:], scalar1=1.0 / 64, scalar2=None, op0=ALU.mult)
            P.op("dve", "tensor_tensor", ["py", "ysum"], ["yd"], out=yd[:].rearrange("p (h e) -> p h e", h=6), in0=y3,
                 in1=ysum[:].unsqueeze(2).to_broadcast([128, 6, 64]), op=ALU.subtract)
            P.op("dve", "tensor_tensor", ["yd"], ["yq"], out=yq[:], in0=yd[:], in1=yd[:], op=ALU.mult)
            P.op("dve", "tensor_reduce", ["yq"], ["yv"], out=yv[:], in_=yq[:].rearrange("p (h e) -> p h e", h=6), axis=AX.X, op=ALU.add)
            P.op("act", "activation", ["yv"], ["yv"], out=yv[:], in_=yv[:], func=AF.Sqrt, scale=1.0 / 64, bias=1e-6)
            P.op("dve", "reciprocal", ["yv"], ["yv"], out=yv[:], in_=yv[:])
            P.op("dve", "tensor_tensor", ["yd", "yv"], ["yd"], out=yd[:].rearrange("p (h e) -> p h e", h=6),
                 in0=yd[:].rearrange("p (h e) -> p h e", h=6), in1=yv[:].unsqueeze(2).to_broadcast([128, 6, 64]), op=ALU.mult)
            P.op("dve", "tensor_tensor", ["yd", "gnwb"], ["yd"], out=yd[:], in0=yd[:], in1=gnwb[:], op=ALU.mult)
            P.op("act", "activation", [S("grm")], ["sg"], out=sg[:], in_=grm[b][:], func=AF.Silu)
            P.op("dve", "tensor_tensor", ["yd", "sg"], ["mixtok_b"], out=mixtok[:, 384:768], in0=yd[:], in1=sg[:], op=ALU.mult)
        if stages & 4:
            pp = ppd[b]
            P.op("dve", "tensor_tensor", [S("ppd")], ["a2"], out=a2[:], in0=pp[:, :, 0:143], in1=pp[:, :, 1:144], op=ALU.add)
            P.op("dve", "tensor_tensor", ["a2"], ["a4"], out=a4[:], in0=a2[:, :, 0:141], in1=a2[:, :, 2:143], op=ALU.add)
            P.op("dve", "tensor_tensor", ["a4"], ["a8"], out=a8[:], in0=a4[:, :, 0:137], in1=a4[:, :, 4:141], op=ALU.add)
            P.op("dve", "tensor_tensor", ["a8"], ["a16"], out=a16[:], in0=a8[:, :, 0:128], in1=a8[:, :, 8:136], op=ALU.add)
            if is_ctx:
                isl = 3 + (it - NT)
            elif it == 0:
                isl = 0
            elif it == NT - 1:
                isl = 2
            else:
                isl = 1
            srcs = [(slice(0, 64), 0, a2[0:64, 0, 7:135]), (slice(64, 128), 0, a4[64:128, 0, 6:134]),
                    (slice(0, 64), 1, a8[0:64, 1, 4:132]), (slice(64, 128), 1, a16[64:128, 1, :])]
            for pr, c, wap in srcs:
                P.op("dve", "tensor_tensor", ["a2", "a4", "a8", "a16", "invcs"], ["pdm"], out=pdm[pr, c, :], in0=wap,
                     in1=invcs[pr, isl, c, :], op=ALU.mult)
                P.op("dve", "tensor_tensor", ["pdm", S("ppd")], ["pdf"], out=pdf[pr, c, :], in0=pdm[pr, c, :],
                     in1=pp[pr, c, 8:136], op=ALU.subtract)
            for c in range(2):
                P.op("pe", "matmul", ["pdf", "wpb"], ["pmi_p"], out=pmi[:, 256 + c * 128:256 + (c + 1) * 128], lhsT=wpb[:, c, :], rhs=pdf[:, c, :], start=True, stop=True)
                P.op("act", "activation", ["pmi_p", "psc"], ["mixT_p"], out=mixT[:, 6 + c, :], in_=pmi[:, 256 + c * 128:256 + (c + 1) * 128],
                     func=AF.Identity, scale=psc[:, c:c + 1])
        for c in range(6):
            P.op("pe", "transpose", ["mixtok_a", "mixtok_b", "identb"], ["ptr"], out=ptr[:, c, :], in_=mixtok[:, c * 128:(c + 1) * 128], identity=identb[:])
        P.op("act", "copy", ["ptr"], ["mixT_t"], out=mixT[:, 0:6, :], in_=ptr[:, 0:6, :])
        for hf in range(2):
            for c in range(8):
                P.op("pe", "matmul", ["mixT_t", "mixT_p"] + WO, ["pout"], out=pout[:, hf, :], lhsT=mixT[:, c, :], rhs=wo[:, c, hf * 512:(hf + 1) * 512],
                     start=(c == 0), stop=(c == 7))
        gg = g1c if is_ctx else g1x
        P.op("dve", "tensor_tensor", ["pout", "g1x", "g1c"], ["otmp"], out=otmp[:].rearrange("p (a n) -> p a n", a=2), in0=pout[:],
             in1=gg[:].rearrange("p (a n) -> p a n", a=2), op=ALU.mult)
        P.op("dve", "tensor_tensor", ["otmp", S("xt")], ["otmp"], out=otmp[:], in0=otmp[:], in1=xt[b][:], op=ALU.add)
        P.dop("sp", reads=["otmp"], out=x1[t0:t0 + 128, :], in_=otmp[:])
    P.end_phase()


def phase_B2(P, IO, NT, NC, final):
    NTT = NT + NC
    P.begin_phase()
    x1 = IO["x1_src"]; mod = IO["d_mod"]; n2w = IO["n2w"]; fnw = IO["fnw"]; wq = IO["wq"]; keysT = IO["keysT"]
    pu = IO["pu"]; pv = IO["pv"]; iota16 = IO["iota16"]; ident_d = IO["ident"]; x2 = IO["x2_dst"]

    identf = P.sb("identf", [128, 128]); identb = P.sb("identb", [128, 128], BF16)
    wqs = [P.sb("wqs%d" % i, [128, 1024]) for i in range(2)]
    wqb = P.sb("wqb", [128, 8, 2048], BF16)
    kTf = P.sb("kTf", [128, 16, 128]); kTb = P.sb("kTb", [128, 16, 128], BF16)
    io16 = P.sb("io16", [128, 16])
    w2x = P.sb("w2x", [128, D]); sh2x = P.sb("sh2x", [128, D]); g2x = P.sb("g2x", [128, D])
    if NC > 0:
        w2c = P.sb("w2c", [128, D]); sh2c = P.sb("sh2c", [128, D]); g2c = P.sb("g2c", [128, D])
    n2b = P.sb("n2b", [128, D])
    if final:
        fnb = P.sb("fnb", [128, D])

    xt = [P.sb("xt%d" % i, [128, D]) for i in range(2)]
    junk = P.sb("junk", [128, D]); ssq = P.sb("ssq", [128, 1]); rstd = P.sb("rstd", [128, 1])
    h2 = P.sb("h2", [128, D]); h2b2 = [P.sb("h2b%d" % i, [128, D], BF16) for i in range(2)]
    h2T = P.sb("h2T", [128, 8, 128], BF16)
    qTs = P.sb("qTs", [128, 16, 128], BF16)
    ssb = P.sb("ssb", [128, 16, 128]); wk = P.sb("wk", [128, 16, 128])
    va = P.sb("va", [128, 16, 16]); ia = P.sb("ia", [128, 16, 16], U32); iaf = P.sb("iaf", [128, 16, 16])
    cand = P.sb("cand", [128, 8, 256])
    sc = P.sb("sc", [128, 8, 16]); ci = P.sb("ci", [128, 8, 16], U32)
    rk = P.sb("rk", [128, 8, 16], U32); ck = P.sb("ck", [128, 8, 16], U32)
    rkf = P.sb("rkf", [128, 8, 16]); ckf = P.sb("ckf", [128, 8, 16])
    oh = P.sb("oh", [128, 8, 16, 16])
    iak = P.sb("iak", [128, 8, 16]); ibk = P.sb("ibk", [128, 8, 16])
    idxf = P.sb("idxf", [128, 128]); idx2 = [P.sb("idx%d" % i, [128, 128], I32) for i in range(2)]
    ex = P.sb("ex", [128, 8, 16]); zs = P.sb("zs", [128, 8]); gate2 = [P.sb("gate%d" % i, [128, 128]) for i in range(2)]
    aa = P.sb("aa", [128, 128]); coef = P.sb("coef", [128, 128])
    ring = [P.sb("ring%d" % i, [128, D], BF16) for i in range(NRING)]
    otmp = P.sb("otmp", [128, D]); dg = [P.sb("dg%d" % i, [128, 128], BF16) for i in range(4)]
    xo = [P.sb("xo%d" % i, [128, D]) for i in range(2)]

    pT = P.ps("pT", [128, 8, 128], BF16)
    pq = P.ps("pq", [128, 16, 128])
    pacc = P.ps("pacc", [128, 2, 512])

    ld = lambda out, in_, w, r=(): P.dop("sp", reads=list(r), writes=[w], out=out, in_=in_)
    ld(identf[:], ident_d, "identf")
    P.op("dve", "tensor_copy", ["identf"], ["identb"], out=identb[:], in_=identf[:])
    ld(kTf[:], keysT, "kTf"); P.op("dve", "tensor_copy", ["kTf"], ["kTb"], out=kTb[:], in_=kTf[:])
    ld(io16[:], iota16, "io16")
    ld(n2b[:], n2w.partition_broadcast(128), "n2b")
    rows = [(0, w2x, sh2x, g2x, "x")]
    if NC > 0:
        rows.append((1, w2c, sh2c, g2c, "c"))
    for r, w2_, sh2_, g2_, nm in rows:
        ld(sh2_[:], mod[r:r + 1, 3 * D:4 * D].partition_broadcast(128), "sh2" + nm)
        ld(w2_[:], mod[r:r + 1, 4 * D:5 * D].partition_broadcast(128), "w2" + nm)
        ld(g2_[:], mod[r:r + 1, 5 * D:6 * D].partition_broadcast(128), "g2" + nm)
        P.op("dve", "scalar_tensor_tensor", ["w2" + nm, "n2b"], ["w2" + nm], out=w2_[:], in0=w2_[:], scalar=1.0, in1=n2b[:], op0=ALU.add, op1=ALU.mult)
    if final:
        ld(fnb[:], fnw.partition_broadcast(128), "fnb")
    for c in range(8):
        for hf in range(2):
            ld(wqs[hf][:], wq[c * 128:(c + 1) * 128, hf * 1024:(hf + 1) * 1024], "wqs%d" % hf)
            P.op("dve", "tensor_copy", ["wqs%d" % hf], ["wqb%d" % c], out=wqb[:, c, hf * 1024:(hf + 1) * 1024], in_=wqs[hf][:])
    WQ = ["wqb%d" % c for c in range(8)]

    def top16(vals_tok, vals, work, outv, outi, wtok):
        P.op("dve", "max", [vals_tok], [wtok + "v"], out=outv[:, 0:8], in_=vals)
        P.op("dve", "max_index", [vals_tok, wtok + "v"], [wtok + "i"], out=outi[:, 0:8], in_max=outv[:, 0:8], in_values=vals)
        P.op("dve", "match_replace", [vals_tok, wtok + "v"], [wtok + "w"], out=work, in_to_replace=outv[:, 0:8], in_values=vals, imm_value=-1e30)
        P.op("dve", "max", [wtok + "w"], [wtok + "v"], out=outv[:, 8:16], in_=work)
        P.op("dve", "max_index", [wtok + "w", wtok + "v"], [wtok + "i"], out=outi[:, 8:16], in_max=outv[:, 8:16], in_values=work)

    ring_i = 0
    def prologue1(it):
            b = it % 2
            is_ctx = it >= NT
            t0 = it * 128
            S = lambda n: "%s%d" % (n, b)
            w2_, sh2_, g2_, nm = (w2c, sh2c, g2c, "c") if is_ctx else (w2x, sh2x, g2x, "x")
            ld(xt[b][:], x1[t0:t0 + 128, :], S("xt"))
            P.op("act", "activation", [S("xt")], ["junk", "ssq"], out=junk[:], in_=xt[b][:], func=AF.Square, accum_out=ssq[:])
            P.op("act", "activation", ["ssq"], ["rstd"], out=rstd[:], in_=ssq[:], func=AF.Sqrt, scale=1.0 / D, bias=1e-6)
            P.op("dve", "reciprocal", ["rstd"], ["rstd"], out=rstd[:], in_=rstd[:])
            P.op("dve", "scalar_tensor_tensor", [S("xt"), "rstd", "w2" + nm], ["h2"], out=h2[:], in0=xt[b][:], scalar=rstd[:, 0:1], in1=w2_[:], op0=ALU.mult, op1=ALU.mult)
            P.op("dve", "tensor_tensor", ["h2", "sh2" + nm], ["h2"], out=h2[:], in0=h2[:], in1=sh2_[:], op=ALU.add)
            P.op("act", "copy", ["h2"], [S("h2b")], out=h2b2[b][:], in_=h2[:])
            for c in range(8):
                P.op("pe", "transpose", [S("h2b"), "identb"], ["pT"], out=pT[:, c, :], in_=h2b2[b][:, c * 128:(c + 1) * 128], identity=identb[:])
            P.op("act", "copy", ["pT"], ["h2T"], out=h2T[:], in_=pT[:])
            for n in range(16):
                for c in range(8):
                    P.op("pe", "matmul", ["h2T"] + WQ, ["pq"], out=pq[:, n, :], lhsT=wqb[:, c, n * 128:(n + 1) * 128], rhs=h2T[:, c, :], start=(c == 0), stop=(c == 7))
            P.op("act", "copy", ["pq"], ["qTs"], out=qTs[:], in_=pq[:])
            for n in range(16):
                P.op("pe", "matmul", ["qTs", "kTb"], ["pq"], out=pq[:, n, :], lhsT=qTs[:, n, :], rhs=kTb[:, n, :], start=True, stop=True)
            P.op("act", "copy", ["pq"], ["ssb"], out=ssb[:], in_=pq[:])

    def prologue2(it):
            b = it % 2
            is_ctx = it >= NT
            t0 = it * 128
            S = lambda n: "%s%d" % (n, b)
            w2_, sh2_, g2_, nm = (w2c, sh2c, g2c, "c") if is_ctx else (w2x, sh2x, g2x, "x")
            for g in range(16):
                top16("ssb", ssb[:, g, :], wk[:, g, :], va[:, g, :], ia[:, g, :], "t%d" % g)
            TV = ["t%dv" % g for g in range(16)]
            TI = ["t%di" % g for g in range(16)]
            va4 = va[:].rearrange("p (h s) r -> p h s r", s=2)
            P.op("dve", "tensor_tensor", TV + TI, ["cand"], out=cand[:].rearrange("p h (r c) -> p h r c", r=16),
                 in0=va4[:, :, 0, :].unsqueeze(3).to_broadcast([128, 8, 16, 16]),
                 in1=va4[:, :, 1, :].unsqueeze(2).to_broadcast([128, 8, 16, 16]), op=ALU.add)
            for h in range(8):
                top16("cand", cand[:, h, :], wk[:, 2 * h:2 * h + 2, :].rearrange("p a n -> p (a n)"), sc[:, h, :], ci[:, h, :], "c%d" % h)
            CV = ["c%dv" % h for h in range(8)]
            CI = ["c%di" % h for h in range(8)]
            P.op("dve", "tensor_single_scalar", CI, ["rk"], out=rk[:], in_=ci[:], scalar=4, op=ALU.logical_shift_right)
            P.op("dve", "tensor_single_scalar", CI, ["ck"], out=ck[:], in_=ci[:], scalar=15, op=ALU.bitwise_and)
            P.op("dve", "tensor_copy", ["rk"], ["rkf"], out=rkf[:], in_=rk[:])
            P.op("dve", "tensor_copy", ["ck"], ["ckf"], out=ckf[:], in_=ck[:])
            P.op("dve", "tensor_copy", TI, ["iaf"], out=iaf[:], in_=ia[:])
            iaf4 = iaf[:].rearrange("p (h s) r -> p h s r", s=2)
            io_b = io16[:].unsqueeze(1).unsqueeze(1).to_broadcast([128, 8, 16, 16])
            for side, (kf, ktok, dst, dtok) in enumerate([(rkf, "rkf", iak, "iak"), (ckf, "ckf", ibk, "ibk")]):
                P.op("dve", "tensor_tensor", [ktok, "io16"], ["oh"], out=oh[:], in0=io_b,
                     in1=kf[:].unsqueeze(3).to_broadcast([128, 8, 16, 16]), op=ALU.is_equal)
                P.op("dve", "tensor_tensor", ["oh", "iaf"], ["oh"], out=oh[:], in0=oh[:],
                     in1=iaf4[:, :, side, :].unsqueeze(2).to_broadcast([128, 8, 16, 16]), op=ALU.mult)
                P.op("dve", "tensor_reduce", ["oh"], [dtok], out=dst[:], in_=oh[:], axis=AX.X, op=ALU.add)
            P.op("dve", "scalar_tensor_tensor", ["iak", "ibk"], ["idxf"], out=idxf[:], in0=iak[:].rearrange("p h k -> p (h k)"), scalar=128.0,
                 in1=ibk[:].rearrange("p h k -> p (h k)"), op0=ALU.mult, op1=ALU.add)
            P.op("dve", "tensor_copy", ["idxf"], [S("idx")], out=idx2[b][:], in_=idxf[:])
            P.op("dve", "tensor_tensor", CV, ["ex"], out=ex[:], in0=sc[:], in1=sc[:, :, 0:1].to_broadcast([128, 8, 16]), op=ALU.subtract)
            P.op("act", "activation", ["ex"], ["ex"], out=ex[:], in_=ex[:], func=AF.Exp)
            P.op("dve", "tensor_reduce", ["ex"], ["zs"], out=zs[:], in_=ex[:], axis=AX.X, op=ALU.add)
            P.op("dve", "reciprocal", ["zs"], ["zs"], out=zs[:], in_=zs[:])
            P.op("dve", "tensor_tensor", ["ex", "zs"], [S("gate")], out=gate2[b][:].rearrange("p (h k) -> p h k", h=8), in0=ex[:],
                 in1=zs[:].unsqueeze(2).to_broadcast([128, 8, 16]), op=ALU.mult)


    def uphase(it):
            nonlocal ring_i
            b = it % 2
            is_ctx = it >= NT
            t0 = it * 128
            S = lambda n: "%s%d" % (n, b)
            w2_, sh2_, g2_, nm = (w2c, sh2c, g2c, "c") if is_ctx else (w2x, sh2x, g2x, "x")
            for kk in range(128):
                rg = ring[ring_i % NRING]; rtok = "ring%d" % (ring_i % NRING); ring_i += 1
                P.dop("pool", reads=[S("idx")], writes=[rtok], method="indirect_dma_start", out=rg[:], out_offset=None, in_=pu,
                      in_offset=bass.IndirectOffsetOnAxis(ap=idx2[b][:, kk:kk + 1], axis=0))
                P.op("dve", "tensor_tensor", [rtok, S("h2b")], [rtok], out=rg[:], in0=rg[:], in1=h2b2[b][:], op=ALU.mult)
                P.op("act", "activation", [rtok], [rtok, "aa%d" % kk], out=rg[:], in_=rg[:], func=AF.Identity, accum_out=aa[:, kk:kk + 1])
            P.op("act", "activation", ["aa%d" % kk for kk in range(128)], ["coef"], out=coef[:], in_=aa[:], func=AF.Gelu)
            P.op("dve", "tensor_tensor", ["coef", S("gate")], ["coef"], out=coef[:], in0=coef[:], in1=gate2[b][:], op=ALU.mult)

    def vphase(it):
            nonlocal ring_i
            b = it % 2
            is_ctx = it >= NT
            t0 = it * 128
            S = lambda n: "%s%d" % (n, b)
            w2_, sh2_, g2_, nm = (w2c, sh2c, g2c, "c") if is_ctx else (w2x, sh2x, g2x, "x")
            for kk in range(128):
                rg = ring[ring_i % NRING]; rtok = "ring%d" % (ring_i % NRING); ring_i += 1
                P.dop("pool", reads=[S("idx")], writes=[rtok], method="indirect_dma_start", out=rg[:], out_offset=None, in_=pv,
                      in_offset=bass.IndirectOffsetOnAxis(ap=idx2[b][:, kk:kk + 1], axis=0))
                dgi = kk % 4
                P.op("act", "activation", ["coef", "identb"], ["dg%d" % dgi], out=dg[dgi][:], in_=identb[:], func=AF.Identity, scale=coef[:, kk:kk + 1])
                for hf in range(2):
                    P.op("pe", "matmul", ["dg%d" % dgi, rtok], ["pacc"], out=pacc[:, hf, :], lhsT=dg[dgi][:], rhs=rg[:, hf * 512:(hf + 1) * 512],
                         start=(kk == 0), stop=(kk == 127))

    def epilogue(it):
            b = it % 2
            is_ctx = it >= NT
            t0 = it * 128
            S = lambda n: "%s%d" % (n, b)
            w2_, sh2_, g2_, nm = (w2c, sh2c, g2c, "c") if is_ctx else (w2x, sh2x, g2x, "x")
            P.op("dve", "tensor_tensor", ["pacc", "g2" + nm], ["otmp"], out=otmp[:].rearrange("p (a n) -> p a n", a=2), in0=pacc[:],
                 in1=g2_[:].rearrange("p (a n) -> p a n", a=2), op=ALU.mult)
            P.op("dve", "tensor_tensor", ["otmp", S("xt")], [S("xo")], out=xo[b][:], in0=otmp[:], in1=xt[b][:], op=ALU.add)
            if final and not is_ctx:
                P.op("act", "activation", [S("xo")], ["junk", "ssq"], out=junk[:], in_=xo[b][:], func=AF.Square, accum_out=ssq[:])
                P.op("act", "activation", ["ssq"], ["rstd"], out=rstd[:], in_=ssq[:], func=AF.Sqrt, scale=1.0 / D, bias=1e-6)
                P.op("dve", "reciprocal", ["rstd"], ["rstd"], out=rstd[:], in_=rstd[:])
                P.op("dve", "scalar_tensor_tensor", [S("xo"), "rstd", "fnb"], [S("xo")], out=xo[b][:], in0=xo[b][:], scalar=rstd[:, 0:1], in1=fnb[:],
                     op0=ALU.mult, op1=ALU.mult)
            P.dop("sp", reads=[S("xo")], out=x2[t0:t0 + 128, :], in_=xo[b][:])


    prologue1(0)
    prologue2(0)
    for it in range(NTT):
        uphase(it)
        if it + 1 < NTT:
            prologue1(it + 1)
        vphase(it)
        if it + 1 < NTT:
            prologue2(it + 1)
        epilogue(it)
    P.end_phase()


import numpy as np
import ml_dtypes

BF = ml_dtypes.bfloat16
D = 1024
GRID_W = 64
POOL_W = (2, 4, 8, 16)


def rope_tables(S):
    t = np.arange(S)
    row = (t // GRID_W).astype(np.float32)
    col = (t % GRID_W).astype(np.float32)
    inv = (10000.0 ** (-np.arange(16, dtype=np.float32) / 16)).astype(np.float32)
    ang = np.concatenate([row[:, None] * inv, col[:, None] * inv], axis=-1)
    return np.cos(ang).astype(np.float32), np.sin(ang).astype(np.float32)


def na_slot_tables(rpb, half, NT):
    NB = 2 * NT
    rows = 2 * NB
    locs = [0, 1, 2, NT - 2, NT - 1]
    G = np.zeros((128, 5, 6, 640), np.float32)
    M = np.zeros((128, 5, 640), np.float32)
    k = np.arange(128)[:, None, None]
    blk = np.arange(5)[None, :, None]
    q = np.arange(128)[None, None, :]
    for s, ml in enumerate(locs):
        m = half * NT + ml
        bs = int(np.clip(m - 2, 0, NB - 5))
        kr = 2 * (bs + blk) + k // 64
        ck = k % 64
        qr = 2 * m + q // 64
        cq = q % 64
        r_start = np.clip(qr - 4, 0, rows - 8)
        valid_r = (kr >= r_start) & (kr < r_start + 8)
        roff = np.clip(kr - qr + 7, 0, 14)
        c_start = np.clip(cq - 8, 0, GRID_W - 16)
        valid_c = (ck >= c_start) & (ck < c_start + 16)
        coff = np.clip(ck - cq + 15, 0, 30)
        valid = np.broadcast_to(valid_r & valid_c, (128, 5, 128))
        roff_b = np.broadcast_to(roff, (128, 5, 128))
        coff_b = np.broadcast_to(coff, (128, 5, 128))
        M[:, s, :] = valid.reshape(128, 640)
        for h in range(6):
            g = rpb[h][roff_b, coff_b]
            G[:, s, h, :] = np.where(valid, g, np.float32(0)).reshape(128, 640)
    return G, M


def window_start(m, NT):
    return int(np.clip(m - 2, 0, 2 * NT - 5))


def ret_consts():
    m = np.arange(128)[:, None]
    n = np.arange(128)[None, :]
    cst = np.zeros((128, 6, 128), np.float32)
    cst[:, 0] = np.maximum(n - m, 0)
    cst[:, 1] = 0.125 * (n >= m)
    cst[:, 2] = np.maximum(m - n, 0)
    cst[:, 3] = 0.125 * (m >= n)
    cst[:, 4] = np.broadcast_to(n + 1, (128, 128))
    cst[:, 5] = np.broadcast_to(128 - n, (128, 128))
    pm = np.stack([127 - np.arange(128), np.arange(128)], 1).astype(np.float32)
    return cst, pm


def inv_counts(half, NT):
    S = 2 * NT * 128
    out = np.zeros((128, 5, 2, 128), np.float32)
    specs = [(half * NT * 128, S), (half * NT * 128 + 128, S), (half * NT * 128 + (NT - 1) * 128, S), (0, 256), (128, 256)]
    for s, (tstart, Tseq) in enumerate(specs):
        t = tstart + np.arange(128)
        for c in range(2):
            for gi in range(2):
                w = POOL_W[2 * c + gi]
                lo = np.clip(t - w // 2, 0, Tseq)
                hi = np.clip(t + w // 2, 0, Tseq)
                out[gi * 64:(gi + 1) * 64, s, c, :] = (1.0 / (hi - lo).astype(np.float32))[None, :]
    return out


def pair_pack(st):
    a = st.reshape(64, 4, 3, 2, 64)
    return np.ascontiguousarray(a.transpose(3, 0, 1, 2, 4).reshape(128, 4, 3, 64))


def phase_C(P, tables):
    P.begin_phase()
    stg = [P.sb("cstg%d" % i, [128, 4096]) for i in range(2)]
    stb = [P.sb("cstb%d" % i, [128, 4096], BF16) for i in range(2)]
    n = 0
    for src, dst in tables:
        sv = src.rearrange("(p j) d -> p (j d)", p=128)
        dv = dst.rearrange("(p j) d -> p (j d)", p=128)
        for ch in range(32):
            i = n % 2
            n += 1
            P.dop("sp", writes=["cstg%d" % i], out=stg[i][:], in_=sv[:, ch * 4096:(ch + 1) * 4096])
            if i == 0:
                P.op("dve", "tensor_copy", ["cstg%d" % i], ["cstb%d" % i], out=stb[i][:], in_=stg[i][:])
            else:
                P.op("act", "copy", ["cstg%d" % i], ["cstb%d" % i], out=stb[i][:], in_=stg[i][:])
            P.dop("sp", reads=["cstb%d" % i], out=dv[:, ch * 4096:(ch + 1) * 4096], in_=stb[i][:])
    P.end_phase()


def build_fused(NT, ncores=8):
    NC = 2
    NTT = NT + NC
    P = Prog()
    di = lambda n, s, dt=F32: P.dram(n, s, dt, "ExternalInput")
    xcat = di("xcat", [NTT * 128, D]); cvT = di("cvT", [128, 8, 2])
    w_ada = di("w_ada", [2, D, 6 * D]); b_ada = di("b_ada", [2, 1, 6 * D]); n1w = di("n1w", [2, 8, 128]); w_in = di("w_in", [2, D, DPROJ])
    ropec = di("ropec", [NTT * 128, 32]); ropes = di("ropes", [NTT * 128, 32]); dec = di("dec", [2, 128, 12])
    posf = di("posf", [128, NTT]); posb = di("posb", [128, NTT]); ident = di("ident", [128, 128])
    G = di("G", [2, 128, 5, 6, 896]); M01 = di("M01", [128, 5, 896]); npow = di("npow", [128, 2]); sel = di("sel", [128, 4])
    dec_col = di("dec_col", [2, 128, 6]); cst = di("cst", [128, 6, 128]); pm = di("pm", [128, 2]); gnw = di("gnw", [2, 1, 384])
    invc = di("invc", [128, 5, 2, 128]); wpool = di("wpool", [2, 128, 2, 128]); pscale = di("pscale", [2, 128, 2]); w_out = di("w_out", [2, D, D])
    n2w = di("n2w", [2, 1, D]); fnw = di("fnw", [1, D]); wq = di("wq", [2, D, 2048]); keysT = di("keysT", [2, 128, 16, 128])
    pu = [di("pu%d" % l_, [16384, D]) for l_ in range(2)]; pv = [di("pv%d" % l_, [16384, D]) for l_ in range(2)]; iota16 = di("iota16", [128, 16])
    out = P.dram("out", [NT * 128, D], F32, "ExternalOutput")

    sc = {}
    def mk(name, shape, dt=F32):
        sc["t_" + name] = P.scratch("s_" + name, shape, dt)
        sc["d_" + name] = sc["t_" + name].ap()
    mk("mod", [2, 6 * D]); mk("qT", [128, NTT, 3, 128], BF16); mk("qrT", [128, NTT, 3, 128], BF16); mk("krT", [128, NTT, 3, 128], BF16)
    mk("kx", [128, 3, (NT + 4) * 128], BF16); mk("kTc", [128, 3, 256], BF16); mk("vx", [128, NT + 4, 6, 65], BF16); mk("vaugc", [128, 2, 6, 65], BF16)
    mk("kr", [128, NTT, 384], BF16); mk("vr", [128, NTT, 384], BF16); mk("gr", [128, NTT, 384]); mk("px", [128, 2, NT * 128 + 16]); mk("pxc", [128, 2, 272])
    mk("stpp", [128, 4, 3, 64]); mk("xb", [128, 3096], BF16); mk("xf", [128, 416]); mk("gb", [256, 3096], BF16); mk("gf", [256, 416])
    mk("x1", [NTT * 128, D]); mk("x2", [NTT * 128, D])
    for l_ in range(2):
        mk("pub%d" % l_, [16384, D], BF16); mk("pvb%d" % l_, [16384, D], BF16)
    phase_C(P, [(pu[0], sc["d_pub0"]), (pv[0], sc["d_pvb0"]), (pu[1], sc["d_pub1"]), (pv[1], sc["d_pvb1"])])

    for l in range(2):
        last = l == 1
        NCB = 0 if last else NC
        src = xcat if l == 0 else sc["d_x2"]
        IO = dict(sc)
        IO.update(x_src=src[0:NT * 128, :], ctx_src=src[NT * 128:NTT * 128, :], cvT=cvT, w_ada=w_ada[l], b_ada=b_ada[l], n1w=n1w[l], w_in=w_in[l],
                  ropec=ropec, ropes=ropes, dec=dec[l], posf=posf, posb=posb, ident=ident)
        phase_A(P, IO, NT, NC)
        IO = dict(sc); IO.update(sel=sel)
        phase_X(P, IO, NT, ncores)
        IO = dict(sc)
        IO.update(x_src=src, G=G[l], M01=M01, npow=npow, sel=sel, dec_row=dec[l], dec_col=dec_col[l], cst=cst, pm=pm, ident=ident, gnw=gnw[l],
                  invc=invc, wpool=wpool[l], pscale=pscale[l], w_out=w_out[l], x1_dst=sc["d_x1"])
        phase_B1(P, IO, NT, NCB)
        IO = dict(sc)
        IO.update(x1_src=sc["d_x1"], n2w=n2w[l], fnw=fnw, wq=wq[l], keysT=keysT[l], pu=sc["d_pub%d" % l], pv=sc["d_pvb%d" % l], iota16=iota16, ident=ident,
                  x2_dst=out if last else sc["d_x2"])
        phase_B2(P, IO, NT, NCB, last)
    P.es.close()
    return P.nc


def na_slot_tables_fused(rpb, half, NT):
    NB = 2 * NT
    rows = 2 * NB
    specs = [(0, 0, 7), (1, 0, 7), (2, 2, 5), (NT - 2, NT - 3, 7), (NT - 1, NT - 3, 7)]
    G = np.zeros((128, 5, 6, 896), np.float32)
    M = np.zeros((128, 5, 896), np.float32)
    k = np.arange(128)[:, None, None]
    q = np.arange(128)[None, None, :]
    for s, (ml, ext0, nb) in enumerate(specs):
        m = half * NT + ml
        e = np.arange(nb)[None, :, None]
        g = half * NT + ext0 + e - 2
        inseq = (g >= 0) & (g < NB)
        kr = 2 * g + k // 64
        ck = k % 64
        qr = 2 * m + q // 64
        cq = q % 64
        r_start = np.clip(qr - 4, 0, rows - 8)
        valid_r = (kr >= r_start) & (kr < r_start + 8) & inseq
        roff = np.clip(kr - qr + 7, 0, 14)
        c_start = np.clip(cq - 8, 0, GRID_W - 16)
        valid_c = (ck >= c_start) & (ck < c_start + 16)
        coff = np.clip(ck - cq + 15, 0, 30)
        valid = np.broadcast_to(valid_r & valid_c, (128, nb, 128))
        roff_b = np.broadcast_to(roff, (128, nb, 128))
        coff_b = np.broadcast_to(coff, (128, nb, 128))
        M[:, s, :nb * 128] = valid.reshape(128, nb * 128)
        for h in range(6):
            gg = rpb[h][roff_b, coff_b]
            G[:, s, h, :nb * 128] = np.where(valid, gg, np.float32(0)).reshape(128, nb * 128)
    return G, M


def fused_inputs(inp, B, NT):
    S = 2 * NT * 128
    NTT = NT + 2
    own_T = NT * 128
    ident = np.eye(128, dtype=np.float32)
    cos, sin = rope_tables(S)
    cst, pm = ret_consts()
    iota16 = np.broadcast_to(np.arange(16, dtype=np.float32), (128, 16)).copy()
    DEPTH = 2
    dec = np.stack([np.tile(np.concatenate([inp["ret_decay_fwd"][l], inp["ret_decay_bwd"][l]])[None, :], (128, 1)) for l in range(DEPTH)]).astype(np.float32)
    dec_col = np.zeros((DEPTH, 128, 6), np.float32)
    wpool = np.zeros((DEPTH, 128, 2, 128), np.float32)
    for l in range(DEPTH):
        for cc in range(3):
            for j in range(2):
                dec_col[l, j * 64:(j + 1) * 64, cc] = inp["ret_decay_fwd"][l][2 * cc + j]
                dec_col[l, j * 64:(j + 1) * 64, 3 + cc] = inp["ret_decay_bwd"][l][2 * cc + j]
        for cc in range(2):
            for gi in range(2):
                wpool[l, gi * 64:(gi + 1) * 64, cc, gi * 64:(gi + 1) * 64] = inp["pool_w"][l][2 * cc + gi]
    pscale = np.ascontiguousarray(inp["pool_scale"].reshape(DEPTH, 2, 128).transpose(0, 2, 1))
    keysT = np.ascontiguousarray(inp["peer_keys"].transpose(0, 4, 2, 1, 3).reshape(DEPTH, 128, 16, 128))
    shared = {
        "w_ada": inp["w_ada"], "b_ada": inp["b_ada"][:, None, :], "n1w": inp["norm1_w"].reshape(DEPTH, 8, 128), "w_in": inp["w_in"],
        "dec": dec, "ident": ident, "dec_col": dec_col, "cst": cst, "pm": pm, "gnw": inp["ret_gn_w"][:, None, :], "wpool": wpool,
        "pscale": pscale, "w_out": inp["w_out"], "n2w": inp["norm2_w"][:, None, :], "fnw": inp["final_norm_w"][None, :], "wq": inp["peer_wq"],
        "keysT": keysT, "pu0": inp["peer_u"][0], "pu1": inp["peer_u"][1], "pv0": inp["peer_v"][0], "pv1": inp["peer_v"][1], "iota16": iota16}
    t = np.arange(NTT * 128)
    own = t < own_T
    posf = np.where(own, own_T - 1 - t, 255 - (t - own_T)).astype(np.float32)
    posb = np.where(own, t, t - own_T).astype(np.float32)
    posf = np.ascontiguousarray(posf.reshape(NTT, 128).T); posb = np.ascontiguousarray(posb.reshape(NTT, 128).T)
    tabs = {}
    for half in range(2):
        GM = [na_slot_tables_fused(inp["na_rpb"][l], half, NT) for l in range(DEPTH)]
        tabs[half] = (np.stack([g for g, _ in GM]), GM[0][1], inv_counts(half, NT))
    maps = []
    for c in range(2 * B):
        b, half = c // 2, c % 2
        cv = np.stack([inp["c"][b], inp["c_ctx"]], 0)
        m = dict(shared)
        m["xcat"] = np.concatenate([inp["x"][b, half * own_T:(half + 1) * own_T], inp["ctx"][b]], 0)
        m["cvT"] = np.ascontiguousarray(cv.reshape(2, 8, 128).transpose(2, 1, 0))
        m["ropec"] = np.concatenate([cos[half * own_T:(half + 1) * own_T], np.ones((256, 32), np.float32)], 0)
        m["ropes"] = np.concatenate([sin[half * own_T:(half + 1) * own_T], np.zeros((256, 32), np.float32)], 0)
        m["posf"] = posf; m["posb"] = posb
        m["G"], m["M01"], m["invc"] = tabs[half]
        npow = np.zeros((128, 2), np.float32); npow[:, 0] = own_T if half == 1 else 0; npow[:, 1] = own_T if half == 0 else 0
        m["npow"] = npow
        selv = np.zeros((128, 4), np.float32)
        selv[:, 0] = half; selv[:, 1] = 1 - half; selv[:, 2] = half; selv[:, 3] = 1 - half
        m["sel"] = selv
        maps.append(m)
    return maps


from concourse.bass_utils import run_bass_kernel_spmd


def kernel(**inputs):
    inp = {k: np.asarray(v) for k, v in inputs.items()}
    B, S, _ = inp["x"].shape
    NT = S // 256
    maps = fused_inputs(inp, B, NT)
    nc = build_fused(NT, len(maps))
    res = run_bass_kernel_spmd(nc, maps, core_ids=list(range(len(maps)))).results
    out = np.zeros((B, S, D), np.float32)
    for c in range(len(maps)):
        out[c // 2, (c % 2) * NT * 128:(c % 2 + 1) * NT * 128] = np.asarray(res[c]["out"])
    return out
```
